# Optimizing a Trainium2 kernel written in Bass

```python
import math
import jax, jax.numpy as jnp
from jax import lax
import numpy as np

D_MODEL = 1024
BATCH = 16
SEQ = 2048
DEPTH = 1

CONV_CH = 512
CONV_KERNEL = 31
ATTN_HEADS = 4
ATTN_HEAD_DIM = 64
ATTN_V_DIM = 2 * ATTN_HEAD_DIM
ATTN_WIDTH = ATTN_HEADS * ATTN_V_DIM
MIX_WIDTH = CONV_CH + ATTN_WIDTH
IN_COLS = 2 * CONV_CH + 3 * ATTN_WIDTH
N_EXPERTS = 32
TOP_K = 4
D_FF = D_MODEL
SWIGLU_LIMIT = 7.0
SWIGLU_ALPHA = 1.702
Q_BLOCK = 128
ROW_BLOCK = 256
N_MOD = 6
EPS = 1e-5

kernel_name = "hymba_conformer_diffattn_moe_adaln"


def rms_norm(x, g):
    xf = x.astype(jnp.float32)
    y = xf * lax.rsqrt(jnp.mean(xf * xf, axis=-1, keepdims=True) + EPS)
    return (y * g.astype(jnp.float32)).astype(x.dtype)


def layer_norm(x, g, b):
    xf = x.astype(jnp.float32)
    mu = jnp.mean(xf, axis=-1, keepdims=True)
    xc = xf - mu
    y = xc * lax.rsqrt(jnp.mean(xc * xc, axis=-1, keepdims=True) + EPS)
    return (y * g.astype(jnp.float32) + b.astype(jnp.float32)).astype(x.dtype)


def alibi_slopes(n_heads):
    return jnp.exp2(-8.0 * jnp.arange(1, n_heads + 1, dtype=jnp.float32) / n_heads)


def conformer_conv(u, conv_w, conv_b, ln_g, ln_b):
    a, g = jnp.split(u, 2, axis=-1)
    h = a * jax.nn.sigmoid(g)
    h = lax.conv_general_dilated(
        h, conv_w[:, None, :].astype(h.dtype), window_strides=(1,),
        padding=[(CONV_KERNEL // 2, CONV_KERNEL // 2)],
        dimension_numbers=("NWC", "WIO", "NWC"),
        feature_group_count=CONV_CH) + conv_b
    h = layer_norm(h, ln_g, ln_b)
    return jax.nn.silu(h)


def diff_attention(q, k, v, lam, subln_g, lam_init):
    B, S = q.shape[0], q.shape[1]
    n_blocks = S // Q_BLOCK
    scale = ATTN_HEAD_DIM ** -0.5
    slopes = alibi_slopes(ATTN_HEADS)[None, :, None, None, None]
    kpos = jnp.arange(S, dtype=jnp.float32)
    qb = q.reshape(B, n_blocks, Q_BLOCK, ATTN_HEADS, 2, ATTN_HEAD_DIM).transpose(1, 0, 2, 3, 4, 5)

    def block(args):
        q_blk, j = args
        s = jnp.einsum("bqhcd,bkhcd->bhcqk", q_blk, k).astype(jnp.float32) * scale
        qpos = (j * Q_BLOCK + jnp.arange(Q_BLOCK)).astype(jnp.float32)
        dist = jnp.abs(qpos[:, None] - kpos[None, :])
        p = jax.nn.softmax(s - slopes * dist, axis=-1)
        p = p[:, :, 0] - lam * p[:, :, 1]
        return jnp.einsum("bhqk,bkhe->bqhe", p.astype(v.dtype), v)

    o = lax.map(block, (qb, jnp.arange(n_blocks)))
    o = o.transpose(1, 0, 2, 3, 4).reshape(B, S, ATTN_HEADS, ATTN_V_DIM)
    o = rms_norm(o, subln_g) * (1.0 - lam_init)
    return o.reshape(B, S, ATTN_WIDTH)


def routed_experts(h, w_router, b_router, w_gate_up, b_gate_up, w_down, b_down):
    B, S, D = h.shape
    N = B * S
    t = h.reshape(N, D)
    logits = (t @ w_router + b_router).astype(jnp.float32)
    top_v, top_i = lax.top_k(logits, TOP_K)
    gates = jax.nn.softmax(top_v, axis=-1)
    A = N * TOP_K
    e_flat = top_i.reshape(A)
    order = jnp.argsort(e_flat)
    e_sorted = e_flat[order]
    tok_sorted = order // TOP_K
    gate_sorted = gates.reshape(A)[order]
    counts = jnp.bincount(e_flat, length=N_EXPERTS)
    padded = (counts + ROW_BLOCK - 1) // ROW_BLOCK * ROW_BLOCK
    start = jnp.cumsum(counts) - counts
    pend = jnp.cumsum(padded)
    pstart = pend - padded
    dest = pstart[e_sorted] + jnp.arange(A) - start[e_sorted]
    n_blocks = -(-A // ROW_BLOCK) + N_EXPERTS
    P = n_blocks * ROW_BLOCK
    buf_tok = jnp.zeros((P,), jnp.int32).at[dest].set(tok_sorted.astype(jnp.int32))
    buf_gate = jnp.zeros((P,), jnp.float32).at[dest].set(gate_sorted)
    blk_expert = jnp.minimum(
        jnp.searchsorted(pend, jnp.arange(n_blocks) * ROW_BLOCK, side="right"), N_EXPERTS - 1)

    def expert_block(args):
        tok, e = args
        xb = t[tok]
        gu = xb @ w_gate_up[e] + b_gate_up[e]
        g, u = jnp.split(gu, 2, axis=-1)
        g = jnp.minimum(g, SWIGLU_LIMIT)
        u = jnp.clip(u, -SWIGLU_LIMIT, SWIGLU_LIMIT)
        y = (u + 1.0) * (g * jax.nn.sigmoid(SWIGLU_ALPHA * g))
        return y @ w_down[e] + b_down[e]

    out = lax.map(expert_block, (buf_tok.reshape(n_blocks, ROW_BLOCK), blk_expert))
    out = out.reshape(P, D) * buf_gate[:, None].astype(out.dtype)
    y = jax.ops.segment_sum(out, buf_tok, num_segments=N)
    return y.reshape(B, S, D)


def setup_inputs(seed: int = 0) -> dict:
    key = jax.random.key(seed)
    ks = jax.random.split(key, 24)
    L, D, E = DEPTH, D_MODEL, N_EXPERTS
    n = lambda k, shape, s: jax.random.normal(k, shape, jnp.float32) * s
    return {
        "x": n(ks[0], (BATCH, SEQ, D), 1.0),
        "c": n(ks[1], (BATCH, D), 1.0),
        "w_ada": n(ks[2], (L, D, N_MOD * D), 0.5 * D ** -0.5),
        "b_ada": n(ks[3], (L, N_MOD * D), 0.02),
        "norm1_g": 1.0 + n(ks[4], (L, D), 0.02),
        "w_in": n(ks[5], (L, D, IN_COLS), D ** -0.5),
        "q_norm_g": 1.0 + n(ks[6], (L, ATTN_HEAD_DIM), 0.02),
        "k_norm_g": 1.0 + n(ks[7], (L, ATTN_HEAD_DIM), 0.02),
        "lambda_q1": n(ks[8], (L, ATTN_HEAD_DIM), 0.1),
        "lambda_k1": n(ks[9], (L, ATTN_HEAD_DIM), 0.1),
        "lambda_q2": n(ks[10], (L, ATTN_HEAD_DIM), 0.1),
        "lambda_k2": n(ks[11], (L, ATTN_HEAD_DIM), 0.1),
        "subln_g": 1.0 + n(ks[12], (L, ATTN_V_DIM), 0.02),
        "conv_w": n(ks[13], (L, CONV_KERNEL, CONV_CH), CONV_KERNEL ** -0.5),
        "conv_b": n(ks[14], (L, CONV_CH), 0.02),
        "conv_ln_g": 1.0 + n(ks[15], (L, CONV_CH), 0.02),
        "conv_ln_b": n(ks[16], (L, CONV_CH), 0.02),
        "w_out": n(ks[17], (L, MIX_WIDTH, D), MIX_WIDTH ** -0.5),
        "norm2_g": 1.0 + n(ks[18], (L, D), 0.02),
        "w_router": n(ks[19], (L, D, E), D ** -0.5),
        "b_router": n(ks[20], (L, E), 0.01),
        "w_gate_up": n(ks[21], (L, E, D, 2 * D_FF), D ** -0.5),
        "b_gate_up": n(ks[22], (L, E, 2 * D_FF), 0.02),
        "w_down": n(ks[23], (L, E, D_FF, D), D_FF ** -0.5),
        "b_down": n(jax.random.fold_in(key, 99), (L, E, D), 0.02),
    }


def reference(x, c, w_ada, b_ada, norm1_g, w_in, q_norm_g, k_norm_g, lambda_q1, lambda_k1,
              lambda_q2, lambda_k2, subln_g, conv_w, conv_b, conv_ln_g, conv_ln_b, w_out,
              norm2_g, w_router, b_router, w_gate_up, b_gate_up, w_down, b_down):
    B, S, D = x.shape
    for l in range(DEPTH):
        mod = jax.nn.silu(c) @ w_ada[l] + b_ada[l]
        shift1, scale1, gate1, shift2, scale2, gate2 = [m[:, None, :] for m in jnp.split(mod, N_MOD, axis=-1)]

        h = rms_norm(x, norm1_g[l]) * (1.0 + scale1) + shift1
        proj = h @ w_in[l]
        u_conv = proj[..., :2 * CONV_CH]
        q, k, v = jnp.split(proj[..., 2 * CONV_CH:], 3, axis=-1)
        q = rms_norm(q.reshape(B, S, ATTN_HEADS, 2, ATTN_HEAD_DIM), q_norm_g[l])
        k = rms_norm(k.reshape(B, S, ATTN_HEADS, 2, ATTN_HEAD_DIM), k_norm_g[l])
        v = v.reshape(B, S, ATTN_HEADS, ATTN_V_DIM)
        lam_init = 0.8 - 0.6 * math.exp(-0.3 * l)
        lam = (jnp.exp(jnp.sum(lambda_q1[l] * lambda_k1[l]).astype(jnp.float32))
               - jnp.exp(jnp.sum(lambda_q2[l] * lambda_k2[l]).astype(jnp.float32)) + lam_init)
        attn_out = diff_attention(q, k, v, lam, subln_g[l], lam_init)
        conv_out = conformer_conv(u_conv, conv_w[l], conv_b[l], conv_ln_g[l], conv_ln_b[l])
        mix = jnp.concatenate([conv_out, attn_out], axis=-1) @ w_out[l]
        x = x + gate1 * mix

        h2 = rms_norm(x, norm2_g[l]) * (1.0 + scale2) + shift2
        y = routed_experts(h2, w_router[l], b_router[l], w_gate_up[l], b_gate_up[l], w_down[l], b_down[l])
        x = x + gate2 * y
    return x
```

```python
import math
from contextlib import ExitStack
import numpy as np
import concourse.bass as bass
import concourse.mybir as mybir
from concourse.bass_utils import run_bass_kernel_spmd

F32 = mybir.dt.float32
BF16 = mybir.dt.bfloat16
I32 = mybir.dt.int32
AF = mybir.ActivationFunctionType
ALU = mybir.AluOpType
AX = mybir.AxisListType

NCORES = 8
D = 1024
S = 2048
NSEQ = 2
NT = NSEQ * S // 128
E = 32
RB = 512
NSTEP = (NT * 128 * 4) // RB + E
EPS = 1e-5
LAM_INIT = 0.8 - 0.6 * math.exp(0.0)
NDQ = 12


class _Op:
    __slots__ = ("eng", "fn", "dma", "deps", "milestone", "sem", "val", "know")


class Prog:
    ENGS = ("pe", "act", "dve", "pool", "sp")

    def __init__(self, nc, es):
        self.nc = nc
        self.ops = []
        self.emitted = 0
        self.last_w = {}
        self.readers = {}
        self.esem = {e: es.enter_context(nc.semaphore("tl_" + e)) for e in self.ENGS}
        self.dsem = {e: [es.enter_context(nc.semaphore("dq_%s%d" % (e, i))) for i in range(NDQ)]
                     for e in ("sp", "act", "pool")}
        self.ecount = {e: 0 for e in self.ENGS}
        self.dcount = {e: 0 for e in self.dsem}
        self.dhist = {e: [] for e in self.dsem}
        self.know = {e: {} for e in self.ENGS}
        self.live_dma = []
        self.last_real = {}

    def op(self, eng, fn, r=(), w=(), dma=False, extra=()):
        o = _Op()
        o.eng, o.fn, o.dma = eng, fn, dma
        deps = set(extra)
        for x in r:
            if x in self.last_w:
                deps.add(self.last_w[x])
        for x in w:
            if x in self.last_w:
                deps.add(self.last_w[x])
            for rd in self.readers.get(x, ()):
                deps.add(rd)
        idx = len(self.ops)
        o.deps = deps
        o.milestone = False
        o.know = None
        self.ops.append(o)
        for x in r:
            self.readers.setdefault(x, []).append(idx)
        for x in w:
            self.last_w[x] = idx
            self.readers[x] = []
        if dma:
            self.live_dma.append(idx)
        elif fn is not None:
            self.last_real[eng] = idx
        return idx

    def barrier(self):
        firsts = []
        for e in self.ENGS:
            ex = list(self.live_dma) if e == "sp" else []
            if e in self.last_real:
                ex.append(self.last_real[e])
            firsts.append(self.op(e, None, w=[("bar", e)], extra=ex))
        self.live_dma = []
        self.last_real = {}
        for e in self.ENGS:
            self.op(e, None, r=[("bar", x) for x in self.ENGS], w=[("bar2", e)])
        self.last_w = {k: v for k, v in self.last_w.items() if k[0] == "bar2"}
        self.readers = {}

    def emit(self):
        nc = self.nc
        ops = self.ops
        start = self.emitted
        for o in ops[start:]:
            for d in o.deps:
                ops[d].milestone = True
        plan = {e: [] for e in self.ENGS}
        for i in range(start, len(ops)):
            o = ops[i]
            e = o.eng
            know = self.know[e]
            waits = []
            if o.dma:
                j = self.dcount[e]
                self.dcount[e] += 1
                o.sem = self.dsem[e][j % NDQ]
                o.val = 16 * (j // NDQ + 1)
                if j >= NDQ:
                    o.deps.add(self.dhist[e][j - NDQ])
                self.dhist[e].append(i)
            for d in sorted(o.deps):
                p = ops[d]
                if (not p.dma) and p.eng == "pe" and e == "pe" and not o.dma and o.fn is not None:
                    continue
                assert p.sem is not None, "dependency on op that was never made a milestone"
                key = id(p.sem)
                if know.get(key, (None, 0))[1] >= p.val:
                    continue
                waits.append((p.sem, p.val))
                if p.know:
                    for k2, v2 in p.know.items():
                        if know.get(k2, (None, 0))[1] < v2[1]:
                            know[k2] = v2
                know[key] = (p.sem, p.val)
            if o.dma:
                o.know = dict(know)
            elif o.milestone or o.fn is None:
                self.ecount[e] += 1
                o.sem = self.esem[e]
                o.val = self.ecount[e]
                o.milestone = True
                snap = dict(know)
                snap[id(o.sem)] = (o.sem, o.val)
                o.know = snap
            else:
                o.sem = None
                o.val = 0
            plan[e].append((o, waits))
        self.emitted = len(ops)
        attr = {"pe": "tensor", "act": "scalar", "dve": "vector", "pool": "gpsimd", "sp": "sync"}

        def run(engname, eng):
            for o, waits in plan[engname]:
                best = {}
                for sem, val in waits:
                    k = id(sem)
                    if k not in best or best[k][1] < val:
                        best[k] = (sem, val)
                for sem, val in best.values():
                    eng.wait_ge(sem, val)
                if o.fn is None:
                    eng.sem_inc(o.sem, 1)
                    continue
                ins = o.fn(eng)
                if o.dma:
                    ins.then_inc(o.sem, 16)
                elif o.milestone:
                    ins.then_inc(o.sem, 1)

        with nc.Block() as block:
            for engname in self.ENGS:
                if not plan[engname]:
                    continue
                deco = getattr(block, attr[engname])

                def body(eng, engname=engname):
                    run(engname, eng)
                deco(body)


def _ap(base, dims, off=0):
    return bass.AP(tensor=base.tensor, offset=base.offset + off, ap=[list(d) for d in dims])


def build_program(debug=False, nseq=NSEQ, do_moe=True, stop=None):
    nc = bass.Bass("TRN2", target_bir_lowering=False)
    NTOK = nseq * S
    NTL = NTOK // 128
    nstep = (NTOK * 4) // RB + E
    NSLOT = nstep * RB

    def din(name, shape, dt=F32):
        return nc.dram_tensor(name, list(shape), dt, kind="ExternalInput")

    x_d = din("x", [NTOK, D])
    cT_d = din("cT", [128, 8, nseq])
    wada_d = din("w_ada", [D, 6 * D])
    bada_d = din("b_ada", [1, 6 * D])
    n1g_d = din("norm1_g", [1, D])
    n2g_d = din("norm2_g", [1, D])
    win_d = din("w_in", [D, 2560])
    gq_d = din("gq", [128, 1])
    gk_d = din("gk", [128, 1])
    lamv_d = din("lamv", [4, 64])
    subg_d = din("subln_g", [1, 128])
    cw_d = din("conv_wT", [128, 4, 31])
    cb_d = din("conv_b", [128, 4])
    clg_d = din("conv_ln_g", [128, 4])
    clb_d = din("conv_ln_b", [128, 4])
    wout_d = din("w_out", [D, D])
    wr_d = din("w_router", [D, E])
    br_d = din("b_router", [1, E])
    wgu_d = din("w_gate_up", [E, D, 2 * D])
    bgu_d = din("b_gate_up", [E, 2 * D])
    wd_d = din("w_down", [E, D, D])
    bd_d = din("b_down", [E, D])
    out_d = nc.dram_tensor("out", [NTOK, D], F32, kind="ExternalOutput")
    X1_d = nc.dram_tensor("X1s", [NTOK, D], F32)
    H2_d = nc.dram_tensor("H2s", [NTOK, D], BF16)
    XB_d = nc.dram_tensor("XBs", [NSLOT, D], BF16)
    OB_d = nc.dram_tensor("OBs", [NSLOT, D], BF16)
    dbg = {}
    if debug:
        dbg["x1"] = nc.dram_tensor("dbg_x1", [NTOK, D], F32, kind="ExternalOutput")
        dbg["lg"] = nc.dram_tensor("dbg_lg", [128, NTL, E], F32, kind="ExternalOutput")
        dbg["slot"] = nc.dram_tensor("dbg_slot", [128, NTL, 4], F32, kind="ExternalOutput")
        dbg["g4"] = nc.dram_tensor("dbg_g4", [128, NTL, 4], F32, kind="ExternalOutput")
        dbg["be"] = nc.dram_tensor("dbg_be", [128, nstep], F32, kind="ExternalOutput")

    es = ExitStack()
    with es:
        P = Prog(nc, es)

        _cnt = [0]

        def sb(name, shape, dt, stack=es):
            _cnt[0] += 1
            return stack.enter_context(nc.sbuf_tensor("s%d_%s" % (_cnt[0], name), list(shape), dt))

        ps = [es.enter_context(nc.psum_tensor("ps%d" % i, [128, 512], F32)) for i in range(8)]

        def PSR(i):
            return ("ps", i)

        def psb(i):
            return ps[i][:].bitcast(BF16)

        ident = sb("ident", [128, 128], BF16)
        ones_bf = sb("ones_bf", [128, 512], BF16)
        zero_bf = sb("zero_bf", [128, 512], BF16)
        lg_all = sb("lg_all", [128, NTL, E], F32)
        mx8 = sb("mx8", [128, NTL, 8], F32)
        Mb = sb("Mb", [128, NTL * E], BF16)
        g4 = sb("g4", [128, NTL, 4], F32)
        nmx = sb("nmx", [128, NTL], F32)
        gsm = sb("gsm", [128, NTL], F32)
        iota_p = sb("iota_p", [128, 1], F32)
        iota_f = sb("iota_f", [128, 128], F32)

        def dve(fn, r=(), w=()):
            return P.op("dve", fn, r, w)

        def act(fn, r=(), w=()):
            return P.op("act", fn, r, w)

        def pool(fn, r=(), w=()):
            return P.op("pool", fn, r, w)

        def pe(fn, r=(), w=()):
            return P.op("pe", fn, r, w)

        def dma(eng, out, in_, r=(), w=(), **kw):
            return P.op(eng, lambda q: q.dma_start(out=out, in_=in_, **kw), r, w, dma=True)

        zf_state = [0]
        ZF_TOTAL = (NSLOT // 128) * 2

        def zero_fill(n):
            if not do_moe:
                return
            for _ in range(n):
                i = zf_state[0]
                if i >= ZF_TOTAL:
                    return
                zf_state[0] += 1
                r0, hf = (i // 2) * 128, i % 2
                dma("pool", XB_d[r0:r0 + 128, hf * 512:(hf + 1) * 512], zero_bf[:], r=["zero_bf"], w=[("XBz", i)])

        MOD_d = nc.dram_tensor("MODs", [nseq, 6, 128, D], F32)
        x_tiles = x_d.ap().rearrange("(t p) d -> t p d", p=128)
        X1_tiles = X1_d.ap().rearrange("(t p) d -> t p d", p=128)
        H2_tiles = H2_d.ap().rearrange("(t p) d -> t p d", p=128)
        slopes = [2.0 ** (-8.0 * (h + 1) / 4) for h in range(4)]

        with ExitStack() as cs:
            it_i = sb("it_i", [128, 128], I32, cs)
            ip_i = sb("ip_i", [128, 1], I32, cs)
            cT = sb("cT", [128, 8, nseq], F32, cs)
            scT = sb("scT", [128, 8, nseq], F32, cs)
            bcl = sb("bcl", [128, 8, 128], BF16, cs)
            wa_st = [sb("wa_st%d" % i, [128, 8, 512], BF16, cs) for i in range(6)]
            bada_b = sb("bada_b", [128, 512], F32, cs)
            g1b = sb("g1b", [128, D], F32, cs)
            g2b = sb("g2b", [128, D], F32, cs)
            modt = [sb("modt%d" % i, [128, D], F32, cs) for i in range(6)]
            pool(lambda g: g.iota(it_i[:], pattern=[[1, 128]], base=0, channel_multiplier=0), w=["it_i"])
            pool(lambda g: g.iota(ip_i[:], pattern=[[1, 1]], base=0, channel_multiplier=1), w=["ip_i"])
            dve(lambda v: v.tensor_copy(out=iota_f[:], in_=it_i[:]), r=["it_i"], w=["iota_f"])
            dve(lambda v: v.tensor_copy(out=iota_p[:], in_=ip_i[:]), r=["ip_i"], w=["iota_p"])
            dve(lambda v: v.tensor_scalar(out=ident[:], in0=iota_f[:], scalar1=iota_p[:, 0:1], scalar2=None,
                                          op0=ALU.is_equal), r=["iota_f", "iota_p"], w=["ident"])
            dve(lambda v: v.memset(ones_bf[:], 1.0), w=["ones_bf"])
            dve(lambda v: v.memset(zero_bf[:], 0.0), w=["zero_bf"])
            dma("sp", cT[:], cT_d.ap(), w=["cT"])
            dma("sp", g1b[:], _ap(n1g_d.ap(), [[0, 128], [1, D]]), w=["g1b"])
            dma("sp", g2b[:], _ap(n2g_d.ap(), [[0, 128], [1, D]]), w=["g2b"])
            act(lambda a: a.activation(out=scT[:], in_=cT[:], func=AF.Silu), r=["cT"], w=["scT"])
            for b in range(nseq):
                dve(lambda v, b=b: v.tensor_copy(out=bcl[:], in_=_ap(scT[:], [[8 * nseq, 128], [nseq, 8], [0, 128]], off=b)),
                    r=["scT"], w=["bcl"])
                for cc in range(12):
                    st = wa_st[cc % 6]
                    dma("pool", st[:], wada_d.ap().rearrange("(kc p) n -> p kc n", p=128)[:, :, cc * 512:(cc + 1) * 512],
                        w=[("wa_st", cc % 6)])
                    dma("sp", bada_b[:], _ap(bada_d.ap(), [[0, 128], [1, 512]], off=cc * 512), w=["bada_b"])
                    bank = cc % 2
                    for kc in range(8):
                        pe(lambda t, kc=kc, st=st, bank=bank: t.matmul(ps[bank][:], lhsT=bcl[:, kc, :], rhs=st[:, kc, :],
                                                                        start=(kc == 0), stop=(kc == 7)),
                           r=["bcl", ("wa_st", cc % 6)], w=[PSR(bank)])
                    which, half = cc // 2, cc % 2
                    dve(lambda v, bank=bank, which=which, half=half: v.tensor_tensor(
                        out=modt[which][:, half * 512:(half + 1) * 512], in0=ps[bank][:], in1=bada_b[:], op=ALU.add),
                        r=[PSR(bank), "bada_b"], w=[("modt", which)])
                for (which, gx, gname) in ((1, g1b, "g1b"), (4, g2b, "g2b")):
                    dve(lambda v, which=which, gx=gx: v.scalar_tensor_tensor(out=modt[which][:], in0=modt[which][:], scalar=1.0,
                                                                            in1=gx[:], op0=ALU.add, op1=ALU.mult),
                        r=[("modt", which), gname], w=[("modt", which)])
                for which in range(6):
                    dma("sp", MOD_d[b, which], modt[which][:], r=[("modt", which)], w=[("MODd", b, which)])
            if stop == "ada" and debug:
                dma("sp", dbg["x1"].ap()[0:128, :], modt[1][:], r=[("modt", 1)], w=["dbgada"])
            P.barrier()
            P.emit()
            if stop == "ada":
                return nc

        with ExitStack() as pa:
            W_r = sb("W_r", [128, 8, E], BF16, pa)
            brb = sb("brb", [128, E], F32, pa)
            gq = sb("gq", [128, 1], F32, pa)
            gk = sb("gk", [128, 1], F32, pa)
            lams = sb("lams", [128, 2], F32, pa)
            neglam = sb("neglam", [128, 1], F32, pa)
            cw = sb("cw", [128, 4, 31], F32, pa)
            cwb = sb("cwb", [128, 4, 31], BF16, pa)
            cb = sb("cb", [128, 4], F32, pa)
            clg = sb("clg", [128, 4], F32, pa)
            clb = sb("clb", [128, 4], F32, pa)
            blk1 = sb("blk1", [128, 128], BF16, pa)
            om512 = sb("om512", [128, 128], BF16, pa)
            eps_c = sb("eps_c", [128, 1], F32, pa)
            hbuf = sb("hbuf", [128, 4, S + 32], BF16, pa)
            qT = sb("qT", [128, 4, S], BF16, pa)
            kT = sb("kT", [128, 4, S], BF16, pa)
            vS = sb("vS", [128, 16, 4, 128], BF16, pa)
            xs = [sb("xs%d" % i, [128, D], F32, pa) for i in range(2)]
            tmp1 = sb("tmp1", [128, D], F32, pa)
            htok = [sb("htok%d" % i, [128, D], BF16, pa) for i in range(2)]
            ss = sb("ss", [128, 4], F32, pa)
            sig = [sb("sig%d" % i, [128, 512], BF16, pa) for i in range(2)]
            sq = [sb("sq%d" % i, [128, 512], BF16, pa) for i in range(2)]
            rs = [sb("rs%d" % i, [128, 512], F32, pa) for i in range(2)]
            Eb = [sb("Eb%d" % i, [128, 512], BF16, pa) for i in range(3)]
            Pt = [sb("Pt%d" % i, [128, 512], BF16, pa) for i in range(5)]
            qz = [[sb("qz%d%d" % (i, c_), [128, 512], BF16, pa) for c_ in range(2)] for i in range(2)]
            oT = [sb("oT%d" % i, [128, 512], F32, pa) for i in range(2)]
            om128 = sb("om128", [128, 128], BF16, pa)
            subgc = sb("subgc", [128, 1], F32, pa)
            sm = sb("sm", [128, 8], F32, pa)
            tmp_scope = ExitStack()
            tmp_scope.__enter__()
            pge = sb("pge", [128, 1], F32, tmp_scope)
            lamb = sb("lamb", [128, 4, 64], F32, tmp_scope)
            lamt = sb("lamt", [128, 2, 64], F32, tmp_scope)
            o1 = sb("o1", [128, 128], F32, tmp_scope)

            dma("pool", W_r[:], wr_d.ap().rearrange("(kc p) n -> p kc n", p=128), w=["W_r"])
            dma("sp", brb[:], _ap(br_d.ap(), [[0, 128], [1, E]]), w=["brb"])
            dma("sp", gq[:], gq_d.ap(), w=["gq"])
            dma("sp", gk[:], gk_d.ap(), w=["gk"])
            dma("sp", lamb[:], _ap(lamv_d.ap(), [[0, 128], [64, 4], [1, 64]]), w=["lamb"])
            dma("sp", cw[:], cw_d.ap(), w=["cw"])
            dma("sp", cb[:], cb_d.ap(), w=["cb"])
            dma("sp", clg[:], clg_d.ap(), w=["clg"])
            dma("sp", clb[:], clb_d.ap(), w=["clb"])
            dve(lambda v: v.tensor_scalar(out=gq[:], in0=gq[:], scalar1=0.125, scalar2=None, op0=ALU.mult),
                r=["gq"], w=["gq"])
            dve(lambda v: v.tensor_tensor(out=lamt[:], in0=lamb[:, 0:2, :], in1=lamb[:, 2:4, :], op=ALU.mult),
                r=["lamb"], w=["lamt"])
            dve(lambda v: v.tensor_reduce(out=lams[:], in_=lamt[:], axis=AX.X, op=ALU.add), r=["lamt"], w=["lams"])
            act(lambda a: a.activation(out=lams[:], in_=lams[:], func=AF.Exp), r=["lams"], w=["lams"])
            dve(lambda v: v.tensor_tensor(out=neglam[:], in0=lams[:, 1:2], in1=lams[:, 0:1], op=ALU.subtract),
                r=["lams"], w=["neglam"])
            dve(lambda v: v.tensor_scalar(out=neglam[:], in0=neglam[:], scalar1=-LAM_INIT, scalar2=None, op0=ALU.add),
                r=["neglam"], w=["neglam"])
            dma("sp", subgc[:], _ap(subg_d.ap(), [[1, 128], [1, 1]]), w=["subgc"])
            dve(lambda v: v.tensor_scalar(out=subgc[:], in0=subgc[:], scalar1=(1.0 - LAM_INIT), scalar2=None,
                                          op0=ALU.mult), r=["subgc"], w=["subgc"])
            dve(lambda v: v.memset(om128[:], 1.0 / 128), w=["om128"])
            dve(lambda v: v.tensor_copy(out=cwb[:], in_=cw[:]), r=["cw"], w=["cwb"])
            for i_ in range(2):
                for c_ in range(2):
                    dve(lambda v, i_=i_, c_=c_: v.memset(qz[i_][c_][:], 0.0), w=[("qz", i_)])
            dve(lambda v: v.tensor_scalar(out=o1[:], in0=iota_f[:], scalar1=64.0, scalar2=None, op0=ALU.is_ge),
                r=["iota_f"], w=["o1"])
            dve(lambda v: v.tensor_scalar(out=pge[:], in0=iota_p[:], scalar1=64.0, scalar2=None, op0=ALU.is_ge),
                r=["iota_p"], w=["pge"])
            dve(lambda v: v.tensor_scalar(out=blk1[:], in0=o1[:], scalar1=pge[:, 0:1], scalar2=1.0 / 64,
                                          op0=ALU.is_equal, op1=ALU.mult), r=["o1", "pge"], w=["blk1"])
            dve(lambda v: v.memset(om512[:], 1.0 / 512), w=["om512"])
            dve(lambda v: v.memset(eps_c[:], EPS), w=["eps_c"])
            dve(lambda v: v.memset(hbuf[:], 0.0), w=[("hbuf", i) for i in range(4)])
            dve(lambda v: v.memset(vS[:], 1.0), w=[("vS", t) for t in range(16)])
            P.barrier()
            P.emit()
            tmp_scope.__exit__(None, None, None)
            if stop == "consts":
                return nc

            for b in range(nseq):
                with ExitStack() as p1:
                    W_in = sb("W_in", [128, 8, 2560], BF16, p1)
                    hT2 = [sb("hT%d" % i_, [128, 8, 512], BF16, p1) for i_ in range(2)]
                    S1 = sb("S1", [128, D], F32, p1)
                    H1 = sb("H1", [128, D], F32, p1)
                    dma("pool", W_in[:], win_d.ap().rearrange("(kc p) n -> p kc n", p=128), w=["W_in"])
                    dma("sp", H1[:], MOD_d[b, 0], w=["H1"])
                    dma("sp", S1[:], MOD_d[b, 1], w=["S1"])
                    def chain(w_, tl):
                        t = b * 16 + w_ * 4 + tl
                        sl = t % 2
                        zero_fill(-(-ZF_TOTAL // (2 * nseq * 16)))
                        dma("sp", xs[sl][:], x_tiles[t], w=[("xs", sl)])
                        act(lambda a, sl=sl: a.activation(out=htok[sl][:], in_=xs[sl][:], func=AF.Square, scale=D ** -0.5,
                                                          accum_out=ss[:, 0:1]),
                            r=[("xs", sl)], w=[("htok", sl), ("ss", 0)])
                        act(lambda a: a.activation(out=ss[:, 1:2], in_=ss[:, 0:1], func=AF.Ln, bias=eps_c[:, 0:1]),
                            r=[("ss", 0), "eps_c"], w=[("ss", 1)])
                        act(lambda a: a.activation(out=ss[:, 1:2], in_=ss[:, 1:2], func=AF.Exp, scale=-0.5),
                            r=[("ss", 1)], w=[("ss", 1)])
                        dve(lambda v, sl=sl: v.scalar_tensor_tensor(out=tmp1[:], in0=xs[sl][:], scalar=ss[:, 1:2], in1=S1[:],
                                                                    op0=ALU.mult, op1=ALU.mult),
                            r=[("xs", sl), ("ss", 1), "S1"], w=["tmp1"])
                        dve(lambda g, sl=sl: g.tensor_tensor(out=htok[sl][:], in0=tmp1[:], in1=H1[:], op=ALU.add),
                            r=["tmp1", "H1"], w=[("htok", sl)])

                    def trans(w_, tl):
                        t = b * 16 + w_ * 4 + tl
                        sl = t % 2
                        bank = tl % 2
                        hTw = hT2[w_ % 2]
                        for kc in range(8):
                            pe(lambda tt, kc=kc, sl=sl, bank=bank: tt.transpose(psb(bank)[:, kc * 128:(kc + 1) * 128],
                                                                               htok[sl][:, kc * 128:(kc + 1) * 128], ident[:]),
                               r=[("htok", sl), "ident"], w=[PSR(bank)])
                        act(lambda a, bank=bank, tl=tl, hTw=hTw: a.activation(
                            out=hTw[:, :, tl * 128:(tl + 1) * 128],
                            in_=psb(bank).rearrange("p (k t) -> p k t", k=8), func=AF.Copy),
                            r=[PSR(bank)], w=[("hT", w_ % 2)])

                    def glu_chunk(w_, i):
                        hTw = hT2[w_ % 2]
                        hres = ("hT", w_ % 2)
                        tok0 = w_ * 512
                        ba, bg = 2 + 2 * (i % 2), 3 + 2 * (i % 2)
                        for (bank, oc) in ((ba, i), (bg, 4 + i)):
                            for kc in range(8):
                                pe(lambda tt, kc=kc, bank=bank, oc=oc, hTw=hTw: tt.matmul(
                                    ps[bank][:], lhsT=W_in[:, kc, oc * 128:(oc + 1) * 128], rhs=hTw[:, kc, :],
                                    start=(kc == 0), stop=(kc == 7)), r=["W_in", hres], w=[PSR(bank)])
                        act(lambda a, bg=bg, i=i: a.activation(out=sig[i % 2][:], in_=ps[bg][:], func=AF.Sigmoid),
                            r=[PSR(bg)], w=[("sig", i % 2)])
                        dve(lambda v, ba=ba, i=i, tok0=tok0: v.tensor_tensor(
                            out=hbuf[:, i, 15 + tok0:15 + tok0 + 512], in0=ps[ba][:], in1=sig[i % 2][:], op=ALU.mult),
                            r=[PSR(ba), ("sig", i % 2)], w=[("hbuf", i)])

                    def qk_chunk(w_, qi):
                        hTw = hT2[w_ % 2]
                        hres = ("hT", w_ % 2)
                        tok0 = w_ * 512
                        hh = qi % 4
                        isq = qi < 4
                        oc = (8 + hh) if isq else (12 + hh)
                        bd_, bs_ = 2 + 2 * (qi % 2), 3 + 2 * (qi % 2)
                        for kc in range(8):
                            pe(lambda tt, kc=kc, bd_=bd_, oc=oc, hTw=hTw: tt.matmul(
                                ps[bd_][:], lhsT=W_in[:, kc, oc * 128:(oc + 1) * 128], rhs=hTw[:, kc, :],
                                start=(kc == 0), stop=(kc == 7)), r=["W_in", hres], w=[PSR(bd_)])
                        act(lambda a, bd_=bd_, qi=qi: a.activation(out=sq[qi % 2][:], in_=ps[bd_][:], func=AF.Square),
                            r=[PSR(bd_)], w=[("sq", qi % 2)])
                        return (qi, hh, isq, bd_, bs_, tok0)

                    def qk_finish(state):
                        qi, hh, isq, bd_, bs_, tok0 = state
                        pe(lambda tt, bs_=bs_, qi=qi: tt.matmul(ps[bs_][:], lhsT=blk1[:], rhs=sq[qi % 2][:], start=True, stop=True),
                           r=["blk1", ("sq", qi % 2)], w=[PSR(bs_)])
                        act(lambda a, bs_=bs_, qi=qi: a.activation(out=rs[qi % 2][:], in_=ps[bs_][:], func=AF.Ln, bias=eps_c[:, 0:1]),
                            r=[PSR(bs_), "eps_c"], w=[("rs", qi % 2)])
                        act(lambda a, qi=qi: a.activation(out=rs[qi % 2][:], in_=rs[qi % 2][:], func=AF.Exp, scale=-0.5),
                            r=[("rs", qi % 2)], w=[("rs", qi % 2)])
                        dstT = qT if isq else kT
                        gcol = gq if isq else gk
                        dve(lambda v, bd_=bd_, qi=qi, dstT=dstT, gcol=gcol, hh=hh, tok0=tok0: v.scalar_tensor_tensor(
                            out=dstT[:, hh, tok0:tok0 + 512], in0=ps[bd_][:], scalar=gcol[:, 0:1], in1=rs[qi % 2][:],
                            op0=ALU.mult, op1=ALU.mult),
                            r=[PSR(bd_), ("rs", qi % 2), "gq", "gk"], w=[("qk", isq, hh)])

                    def v_tile(w_, tl):
                        hTw = hT2[w_ % 2]
                        hres = ("hT", w_ % 2)
                        bank = 6 + tl % 2
                        kt = w_ * 4 + tl
                        for kc in range(8):
                            pe(lambda tt, kc=kc, bank=bank, tl=tl, hTw=hTw: tt.matmul(
                                ps[bank][:], lhsT=hTw[:, kc, tl * 128:(tl + 1) * 128], rhs=W_in[:, kc, 2048:2560],
                                start=(kc == 0), stop=(kc == 7)), r=["W_in", hres], w=[PSR(bank)])
                        act(lambda a, bank=bank, kt=kt: a.activation(
                            out=vS[:, kt, :, 0:128], in_=ps[bank][:].rearrange("p (h e) -> p h e", h=4), func=AF.Copy),
                            r=[PSR(bank)], w=[("vS", kt)])

                    for tl in range(4):
                        chain(0, tl)
                        trans(0, tl)
                    for w_ in range(4):
                        nxt = w_ + 1 < 4
                        for q in range(4):
                            if nxt:
                                chain(w_ + 1, q)
                            if q == 0:
                                glu_chunk(w_, 0); glu_chunk(w_, 1)
                            elif q == 1:
                                glu_chunk(w_, 2); glu_chunk(w_, 3)
                            elif q == 2:
                                prev = None
                                for qi in range(4):
                                    st_ = qk_chunk(w_, qi)
                                    if prev is not None:
                                        qk_finish(prev)
                                    prev = st_
                                qk_finish(prev)
                            else:
                                prev = None
                                for qi in range(4, 8):
                                    st_ = qk_chunk(w_, qi)
                                    if prev is not None:
                                        qk_finish(prev)
                                    prev = st_
                                v_tile(w_, 0)
                                qk_finish(prev)
                                for tl in range(1, 4):
                                    v_tile(w_, tl)
                            if nxt:
                                trans(w_ + 1, q)
                    P.barrier()
                    P.emit()
                    if stop == "p1":
                        return nc

                with ExitStack() as p2:
                    W_out = sb("W_out", [128, 8, D], BF16, p2)
                    tA_i = sb("tA_i", [128, 4096], I32, p2)
                    Tdec = sb("Tdec", [128, 2432], BF16, p2)
                    diag = [sb("diag%d" % i, [128, 31, 128], BF16, p2) for i in range(2)]
                    catT = sb("catT", [128, 8, 512], BF16, p2)
                    cvv = sb("cvv", [128, 4, 512], F32, p2)
                    cst = sb("cst", [128, 2, 512], F32, p2)
                    x1t = sb("x1t", [128, D], F32, p2)
                    h2T = sb("h2T", [128, 8, 128], BF16, p2)
                    G1 = sb("G1", [128, D], F32, p2)
                    S2 = sb("S2", [128, D], F32, p2)
                    H2 = sb("H2", [128, D], F32, p2)
                    dma("pool", W_out[:], wout_d.ap().rearrange("(kc p) n -> p kc n", p=128), w=["W_out"])
                    dma("sp", G1[:], MOD_d[b, 2], w=["G1"])
                    dma("sp", H2[:], MOD_d[b, 3], w=["H2"])
                    dma("sp", S2[:], MOD_d[b, 4], w=["S2"])
                    pool(lambda g: g.iota(tA_i[:], pattern=[[1, 4096]], base=-2048, channel_multiplier=-1), w=["tA"])
                    dve(lambda v: v.scalar_tensor_tensor(out=tA_i[:], in0=tA_i[:], scalar=-1.0, in1=tA_i[:], op0=ALU.mult, op1=ALU.max),
                        r=["tA"], w=["tA"])
                    QKR = [("qk", a_, h_) for a_ in (True, False) for h_ in range(4)]
                    VR = [("vS", t_) for t_ in range(16)]
                    nd = 0

                    def build_diag(i, slot):
                        dg_ = diag[slot]
                        dve(lambda g, i=i, dg_=dg_: g.tensor_tensor(out=dg_[:], in0=_ap(ident[:], [[128, 128], [0, 31], [1, 128]]),
                                                                    in1=_ap(cwb[:], [[124, 128], [1, 31], [0, 128]], off=i * 31), op=ALU.mult),
                            r=["ident", "cwb"], w=[("diag", slot)])

                    build_diag(0, 0)
                    pending = []
                    stats_pending = []
                    for w_ in range(4):
                        tok0 = w_ * 512
                        for i in range(4):
                            dg = diag[nd % 2]
                            dgr = ("diag", nd % 2)
                            nd += 1
                            if not (w_ == 3 and i == 3):
                                build_diag((i + 1) % 4, nd % 2)
                            bank = i % 2
                            for j in range(31):
                                pe(lambda tt, i=i, j=j, bank=bank, tok0=tok0, dg=dg: tt.matmul(
                                    ps[bank][:], lhsT=dg[:, j, :], rhs=hbuf[:, i, tok0 + j:tok0 + j + 512],
                                    start=(j == 0), stop=(j == 30)), r=[dgr, ("hbuf", i)], w=[PSR(bank)])
                            while stats_pending:
                                stats_pending.pop(0)()
                            act(lambda a, i=i, bank=bank: a.activation(out=cvv[:, i, :], in_=ps[bank][:], func=AF.Identity,
                                                                        bias=cb[:, i:i + 1]), r=[PSR(bank), "cb"], w=[("cvv", i)])
                            dve(lambda v, i=i: v.tensor_copy(out=sig[i % 2][:], in_=cvv[:, i, :]),
                                r=[("cvv", i)], w=[("sig", i % 2)])
                            act(lambda a, i=i: a.activation(out=sq[i % 2][:], in_=cvv[:, i, :], func=AF.Square),
                                r=[("cvv", i)], w=[("sq", i % 2)])
                            def _stats(i=i):
                                pe(lambda tt, i=i: tt.matmul(ps[2][:], lhsT=om512[:], rhs=sig[i % 2][:],
                                                             start=(i == 0), stop=(i == 3)), r=["om512", ("sig", i % 2)], w=[PSR(2)])
                                pe(lambda tt, i=i: tt.matmul(ps[3][:], lhsT=om512[:], rhs=sq[i % 2][:], start=(i == 0), stop=(i == 3)),
                                   r=["om512", ("sq", i % 2)], w=[PSR(3)])
                            stats_pending.append(_stats)
                        while stats_pending:
                            stats_pending.pop(0)()
                        act(lambda a: a.activation(out=cst[:, 0, :], in_=ps[2][:], func=AF.Copy), r=[PSR(2)], w=[("cst", 0)])
                        dve(lambda v: v.tensor_tensor(out=rs[0][:], in0=cst[:, 0, :], in1=cst[:, 0, :], op=ALU.mult),
                            r=[("cst", 0)], w=[("rs", 0)])
                        dve(lambda v: v.tensor_tensor(out=cst[:, 1, :], in0=ps[3][:], in1=rs[0][:], op=ALU.subtract),
                            r=[PSR(3), ("rs", 0)], w=[("cst", 1)])
                        act(lambda a: a.activation(out=cst[:, 1, :], in_=cst[:, 1, :], func=AF.Ln, bias=eps_c[:, 0:1]),
                            r=[("cst", 1), "eps_c"], w=[("cst", 1)])
                        act(lambda a: a.activation(out=cst[:, 1, :], in_=cst[:, 1, :], func=AF.Exp, scale=-0.5),
                            r=[("cst", 1)], w=[("cst", 1)])
                        for i in range(4):
                            dve(lambda v, i=i: v.tensor_tensor(out=cvv[:, i, :], in0=cvv[:, i, :], in1=cst[:, 0, :], op=ALU.subtract),
                                r=[("cvv", i), ("cst", 0)], w=[("cvv", i)])
                            dve(lambda v, i=i: v.tensor_tensor(out=cvv[:, i, :], in0=cvv[:, i, :], in1=cst[:, 1, :], op=ALU.mult),
                                r=[("cvv", i), ("cst", 1)], w=[("cvv", i)])
                            act(lambda a, i=i: a.activation(out=catT[:, i, :], in_=cvv[:, i, :], func=AF.Silu,
                                                            bias=clb[:, i:i + 1], scale=clg[:, i:i + 1]),
                                r=[("cvv", i), "clb", "clg"], w=[("catT", i)])
                        lo = tok0 + 128
                        items = [(hh, c, kt) for hh in range(4) for kt in range(16) for c in range(2)]

                        def stage1(g):
                            hh, c, kt = items[g]
                            if c == 0 and kt == 0:
                                act(lambda a, hh=hh, lo=lo: a.activation(out=Tdec[:], in_=tA_i[:, lo:lo + 2432], func=AF.Exp,
                                                                         scale=-slopes[hh]), r=["tA"], w=["Tdec"])
                                pool(lambda gp, hh=hh, tok0=tok0: gp.tensor_copy(out=qz[hh % 2][0][0:64, :], in_=qT[0:64, hh, tok0:tok0 + 512]),
                                     r=QKR, w=[("qz", hh % 2)])
                                pool(lambda gp, hh=hh, tok0=tok0: gp.tensor_copy(out=qz[hh % 2][1][64:128, :], in_=qT[64:128, hh, tok0:tok0 + 512]),
                                     r=QKR + [("qz", hh % 2)], w=[("qz", hh % 2)])
                            sbk = 3 + g % 3
                            pe(lambda tt, hh=hh, c=c, kt=kt, sbk=sbk: tt.matmul(
                                ps[sbk][:], lhsT=kT[:, hh, kt * 128:(kt + 1) * 128],
                                rhs=qz[hh % 2][c][:], start=True, stop=True),
                               r=QKR + [("qz", hh % 2)], w=[PSR(sbk)])
                            act(lambda a, sbk=sbk, g=g: a.activation(out=Eb[g % 3][:], in_=ps[sbk][:], func=AF.Exp),
                                r=[PSR(sbk)], w=[("Eb", g % 3)])
                            i0 = tok0 - kt * 128 + 2048 - lo
                            dve(lambda v, g=g, i0=i0: v.tensor_tensor(out=Pt[g % 5][:], in0=Eb[g % 3][:],
                                                                      in1=Tdec[:, i0:i0 + 512], op=ALU.mult),
                                r=[("Eb", g % 3), "Tdec"], w=[("Pt", g % 5)])

                        def stage2(g):
                            hh, c, kt = items[g]
                            bankA, bankB = (6, 7) if c == 0 else (0, 1)
                            pe(lambda tt, kt=kt, bankA=bankA, hh=hh, g=g: tt.matmul(
                                ps[bankA][:], lhsT=vS[:, kt, hh, 0:128], rhs=Pt[g % 5][:], start=(kt == 0), stop=(kt == 15)),
                               r=[("Pt", g % 5)] + VR, w=[PSR(bankA)])
                            pe(lambda tt, kt=kt, bankB=bankB, g=g: tt.matmul(
                                ps[bankB][:], lhsT=ones_bf[:, 0:128], rhs=Pt[g % 5][:], start=(kt == 0), stop=(kt == 15)),
                               r=[("Pt", g % 5), "ones_bf"], w=[PSR(bankB)])
                            if kt != 15:
                                return
                            act(lambda a, bankB=bankB, c=c: a.activation(out=rs[c][:], in_=ps[bankB][:], func=AF.Ln), r=[PSR(bankB)], w=[("rs", c)])
                            act(lambda a, c=c: a.activation(out=rs[c][:], in_=rs[c][:], func=AF.Exp, scale=-1.0), r=[("rs", c)], w=[("rs", c)])
                            dve(lambda v, bankA=bankA, c=c: v.tensor_tensor(out=oT[c][:], in0=ps[bankA][:], in1=rs[c][:], op=ALU.mult),
                                r=[PSR(bankA), ("rs", c)], w=[("oT", c)])
                            if c == 1:
                                dve(lambda v: v.scalar_tensor_tensor(out=oT[1][:], in0=oT[1][:], scalar=neglam[:, 0:1], in1=oT[0][:],
                                                                     op0=ALU.mult, op1=ALU.add),
                                    r=[("oT", 0), ("oT", 1), "neglam"], w=[("oT", 1)])
                                dve(lambda v: v.tensor_tensor(out=sq[0][:], in0=oT[1][:], in1=oT[1][:], op=ALU.mult),
                                    r=[("oT", 1)], w=[("sq", 0)])

                                def _fin(hh=hh):
                                    pe(lambda tt: tt.matmul(ps[2][:], lhsT=om128[:], rhs=sq[0][:], start=True, stop=True),
                                       r=["om128", ("sq", 0)], w=[PSR(2)])
                                    act(lambda a: a.activation(out=rs[0][:], in_=ps[2][:], func=AF.Ln, bias=eps_c[:, 0:1]),
                                        r=[PSR(2), "eps_c"], w=[("rs", 0)])
                                    act(lambda a: a.activation(out=rs[0][:], in_=rs[0][:], func=AF.Exp, scale=-0.5),
                                        r=[("rs", 0)], w=[("rs", 0)])
                                    dve(lambda v, hh=hh: v.scalar_tensor_tensor(out=catT[:, 4 + hh, :], in0=oT[1][:], scalar=subgc[:, 0:1],
                                                                                in1=rs[0][:], op0=ALU.mult, op1=ALU.mult),
                                        r=[("oT", 1), ("rs", 0), "subgc"], w=[("catT", 4 + hh)])
                                pending.append(_fin)

                        DEPTH = 4
                        for g in range(len(items)):
                            stage1(g)
                            if g >= DEPTH:
                                stage2(g - DEPTH)
                            if items[g][2] == 9 and items[g][1] == 1 and pending:
                                pending.pop(0)()
                        for g in range(len(items) - DEPTH, len(items)):
                            stage2(g)
                        while pending:
                            pending.pop(0)()
                        CATR = [("catT", i) for i in range(8)]

                        def stA(u):
                            t = b * 16 + w_ * 4 + u
                            sl = t % 2
                            zero_fill(-(-ZF_TOTAL // (2 * nseq * 16)))
                            dma("sp", xs[sl][:], x_tiles[t], w=[("xs", sl)])
                            for half in range(2):
                                bank = 4 + half
                                for kc in range(8):
                                    pe(lambda tt, kc=kc, u=u, half=half, bank=bank: tt.matmul(
                                        ps[bank][:], lhsT=catT[:, kc, u * 128:(u + 1) * 128],
                                        rhs=W_out[:, kc, half * 512:(half + 1) * 512], start=(kc == 0), stop=(kc == 7)),
                                       r=CATR + ["W_out"], w=[PSR(bank)])
                                dve(lambda v, half=half, bank=bank: v.tensor_tensor(
                                    out=tmp1[:, half * 512:(half + 1) * 512], in0=ps[bank][:], in1=G1[:, half * 512:(half + 1) * 512],
                                    op=ALU.mult), r=[PSR(bank), "G1"], w=[("tmp1h", half)])

                        def stB(u):
                            t = b * 16 + w_ * 4 + u
                            sl = t % 2
                            dve(lambda g, sl=sl: g.tensor_tensor(out=x1t[:], in0=tmp1[:], in1=xs[sl][:], op=ALU.add),
                                r=[("tmp1h", 0), ("tmp1h", 1), ("xs", sl)], w=["x1t", "tmp1"])
                            dma("sp", X1_tiles[t], x1t[:], r=["x1t"], w=[("X1", t)])
                            if debug:
                                dma("sp", dbg["x1"].ap().rearrange("(t p) d -> t p d", p=128)[t], x1t[:],
                                    r=["x1t"], w=[("dbgx1", t)])
                            act(lambda a, sl=sl: a.activation(out=htok[sl][:], in_=x1t[:], func=AF.Square, scale=D ** -0.5, accum_out=ss[:, 2:3]),
                                r=["x1t"], w=[("htok", sl), ("ss", 2)])
                            act(lambda a: a.activation(out=ss[:, 3:4], in_=ss[:, 2:3], func=AF.Ln, bias=eps_c[:, 0:1]),
                                r=[("ss", 2), "eps_c"], w=[("ss", 3)])
                            act(lambda a: a.activation(out=ss[:, 3:4], in_=ss[:, 3:4], func=AF.Exp, scale=-0.5),
                                r=[("ss", 3)], w=[("ss", 3)])
                            dve(lambda v: v.scalar_tensor_tensor(out=tmp1[:], in0=x1t[:], scalar=ss[:, 3:4], in1=S2[:],
                                                                 op0=ALU.mult, op1=ALU.mult),
                                r=["x1t", ("ss", 3), "S2"], w=["tmp1", ("tmp1h", 0), ("tmp1h", 1)])
                            dve(lambda g, sl=sl: g.tensor_tensor(out=htok[sl][:], in0=tmp1[:], in1=H2[:], op=ALU.add),
                                r=["tmp1", ("tmp1h", 0), ("tmp1h", 1), "H2"], w=[("htok", sl)])
                            dma("sp", H2_tiles[t], htok[sl][:], r=[("htok", sl)], w=[("H2d", t)])

                        def stC(u):
                            t = b * 16 + w_ * 4 + u
                            sl = t % 2
                            for kc in range(8):
                                pe(lambda tt, kc=kc, sl=sl: tt.transpose(psb(2)[:, kc * 128:(kc + 1) * 128],
                                                                         htok[sl][:, kc * 128:(kc + 1) * 128], ident[:]),
                                   r=[("htok", sl), "ident"], w=[PSR(2)])
                            act(lambda a: a.activation(out=h2T[:], in_=psb(2).rearrange("p (k t) -> p k t", k=8), func=AF.Copy),
                                r=[PSR(2)], w=["h2T"])
                            for kc in range(8):
                                pe(lambda tt, kc=kc: tt.matmul(ps[3][:, 0:E], lhsT=h2T[:, kc, :], rhs=W_r[:, kc, :],
                                                               start=(kc == 0), stop=(kc == 7)), r=["h2T", "W_r"], w=[PSR(3)])
                            dve(lambda v, t=t: v.tensor_tensor(out=lg_all[:, t, :], in0=ps[3][:, 0:E], in1=brb[:], op=ALU.add),
                                r=[PSR(3), "brb"], w=[("lg", t)])
                            dve(lambda v, t=t: v.max(out=mx8[:, t, :], in_=lg_all[:, t, :]), r=[("lg", t)], w=[("mx8", t)])
                            dve(lambda v, t=t: v.tensor_scalar(out=Mb[:, t * E:(t + 1) * E], in0=lg_all[:, t, :], scalar1=mx8[:, t, 3:4],
                                                               scalar2=None, op0=ALU.is_ge), r=[("lg", t), ("mx8", t)], w=[("Mb", t)])
                            dve(lambda v, t=t: v.tensor_scalar(out=nmx[:, t:t + 1], in0=mx8[:, t, 0:1], scalar1=-1.0, scalar2=None,
                                                               op0=ALU.mult), r=[("mx8", t)], w=[("nmx", t)])
                            act(lambda a, t=t: a.activation(out=g4[:, t, :], in_=mx8[:, t, 0:4], func=AF.Exp, bias=nmx[:, t:t + 1],
                                                            accum_out=gsm[:, t:t + 1]), r=[("mx8", t), ("nmx", t)], w=[("g4", t), ("gsm", t)])
                            dve(lambda v, t=t: v.reciprocal(out=gsm[:, t:t + 1], in_=gsm[:, t:t + 1]), r=[("gsm", t)], w=[("gsm", t)])
                            dve(lambda v, t=t: v.tensor_scalar(out=g4[:, t, :], in0=g4[:, t, :], scalar1=gsm[:, t:t + 1], scalar2=None,
                                                               op0=ALU.mult), r=[("g4", t), ("gsm", t)], w=[("g4", t)])

                        stA(0)
                        stB(0)
                        for u in range(1, 4):
                            stA(u)
                            stC(u - 1)
                            stB(u)
                        stC(3)
                    P.barrier()
                    P.emit()
                    if stop == "p2":
                        return nc

        with ExitStack() as pb:
            sloti = sb("sloti", [128, NTL * 4], I32, pb)
            bei = sb("bei", [128, nstep], I32, pb)
            widx = sb("widx", [128, nstep * 8], I32, pb)
            bsel = sb("bsel", [128, 16, nstep], F32, pb)
            ohT = sb("ohT", [128, nstep], F32, pb)
            bgu = sb("bgu", [E, 2 * D], BF16, pb)
            bdn = sb("bdn", [128, D], BF16, pb)
            rt_scope = ExitStack()
            rt_scope.__enter__()
            Lst = sb("Lst", [128, 128], BF16, rt_scope)
            onesq = sb("onesq", [128, 128], BF16, rt_scope)
            pre = sb("pre", [128, NTL, E], F32, rt_scope)
            csb = sb("csb", [128, NTL, E], F32, rt_scope)
            off = sb("off", [128, NTL, E], F32, rt_scope)
            cnt = sb("cnt", [128, E], F32, rt_scope)
            pad = sb("pad", [128, E], F32, rt_scope)
            pst = sb("pst", [128, E], F32, rt_scope)
            pend = sb("pend", [128, E], F32, rt_scope)
            big = sb("big", [128, NTL, 4, E], F32, rt_scope)
            slotf = sb("slotf", [128, NTL, 4], F32, rt_scope)
            bthr = sb("bthr", [128, nstep], F32, rt_scope)
            bthr_i = sb("bthr_i", [128, nstep], I32, rt_scope)
            cmpb = sb("cmpb", [128, nstep, E], F32, rt_scope)
            bef = sb("bef", [128, nstep], F32, rt_scope)
            bgu_f = sb("bgu_f", [E, 2 * D], F32, rt_scope)
            bd_f = sb("bd_f", [E, D], F32, rt_scope)
            cmp8 = sb("cmp8", [128, E, 16], F32, rt_scope)
            kcp_i = sb("kcp_i", [128, 8], I32, rt_scope)
            kcp = sb("kcp", [128, 8], F32, rt_scope)
            widx_f = sb("widx_f", [128, nstep, 8], F32, rt_scope)
            bef1k = sb("bef1k", [128, nstep], F32, rt_scope)
            inact = sb("inact", [128, nstep], F32, rt_scope)
            ohTb = sb("ohTb", [E, nstep], BF16, rt_scope)

            LGR = [("lg", t) for t in range(NTL)]
            dve(lambda v: v.tensor_scalar(out=Lst[:], in0=iota_f[:], scalar1=iota_p[:, 0:1], scalar2=None, op0=ALU.is_gt),
                r=["iota_f", "iota_p"], w=["Lst"])
            dve(lambda v: v.memset(onesq[:], 1.0), w=["onesq"])
            pool(lambda g: g.iota(bthr_i[:], pattern=[[RB, nstep]], base=0, channel_multiplier=0), w=["bthr_i"])
            dve(lambda v: v.tensor_copy(out=bthr[:], in_=bthr_i[:]), r=["bthr_i"], w=["bthr"])
            dma("sp", bgu_f[:], bgu_d.ap(), w=["bgu_f"])
            dma("sp", bd_f[:], bd_d.ap(), w=["bd_f"])
            act(lambda a: a.activation(out=bgu[:], in_=bgu_f[:], func=AF.Copy), r=["bgu_f"], w=["bgu"])
            dve(lambda v: v.memset(bdn[:], 0.0), w=["bdn"])
            act(lambda a: a.activation(out=bdn[0:E, :], in_=bd_f[:], func=AF.Copy), r=["bd_f", "bdn"], w=["bdn"])
            MXR = []
            G4R = []
            nh = (NTL * E) // 512
            for hf in range(nh):
                pe(lambda tt, hf=hf: tt.matmul(ps[hf][:], lhsT=Lst[:], rhs=Mb[:, hf * 512:(hf + 1) * 512], start=True, stop=True),
                   r=["Lst"], w=[PSR(hf)])
                pe(lambda tt, hf=hf: tt.matmul(ps[2 + hf][:], lhsT=onesq[:], rhs=Mb[:, hf * 512:(hf + 1) * 512], start=True, stop=True),
                   r=["onesq"], w=[PSR(2 + hf)])
                act(lambda a, hf=hf: a.activation(out=pre[:].rearrange("p t e -> p (t e)")[:, hf * 512:(hf + 1) * 512],
                                                  in_=ps[hf][:], func=AF.Copy), r=[PSR(hf)], w=[("pre", hf)])
                act(lambda a, hf=hf: a.activation(out=csb[:].rearrange("p t e -> p (t e)")[:, hf * 512:(hf + 1) * 512],
                                                  in_=ps[2 + hf][:], func=AF.Copy), r=[PSR(2 + hf)], w=[("csb", hf)])
            PRER = [("pre", hf) for hf in range(nh)]
            CSR = [("csb", hf) for hf in range(nh)]
            dve(lambda v: v.memset(off[:, 0, :], 0.0), w=["off"])
            for t in range(1, NTL):
                dve(lambda v, t=t: v.tensor_tensor(out=off[:, t, :], in0=off[:, t - 1, :], in1=csb[:, t - 1, :], op=ALU.add),
                    r=["off"] + CSR, w=["off"])
            dve(lambda v: v.tensor_tensor(out=cnt[:], in0=off[:, NTL - 1, :], in1=csb[:, NTL - 1, :], op=ALU.add),
                r=["off"] + CSR, w=["cnt"])
            nmx_b = NTOK // RB + 1
            assert nmx_b <= 16
            dve(lambda v: v.tensor_tensor(out=cmp8[:, :, 0:nmx_b], in0=_ap(cnt[:], [[E, 128], [1, E], [0, nmx_b]]),
                                          in1=_ap(bthr[:], [[nstep, 128], [0, E], [1, nmx_b]]), op=ALU.is_gt),
                r=["cnt", "bthr"], w=["cmp8"])
            dve(lambda v: v.tensor_reduce(out=pad[:], in_=cmp8[:, :, 0:nmx_b], axis=AX.X, op=ALU.add), r=["cmp8"], w=["pad"])
            dve(lambda v: v.tensor_scalar(out=pad[:], in0=pad[:], scalar1=float(RB), scalar2=None, op0=ALU.mult),
                r=["pad"], w=["pad"])
            dve(lambda v: v.memset(pst[:, 0:1], 0.0), w=["pst"])
            for e in range(1, E):
                dve(lambda v, e=e: v.tensor_tensor(out=pst[:, e:e + 1], in0=pst[:, e - 1:e], in1=pad[:, e - 1:e], op=ALU.add),
                    r=["pst", "pad"], w=["pst"])
            dve(lambda v: v.tensor_tensor(out=pend[:], in0=pst[:], in1=pad[:], op=ALU.add), r=["pst", "pad"], w=["pend"])
            dve(lambda v: v.tensor_tensor(out=pre[:], in0=pre[:], in1=off[:], op=ALU.add), r=PRER + ["off"], w=["dest"])
            dve(lambda v: v.tensor_tensor(out=pre[:], in0=pre[:], in1=_ap(pst[:], [[E, 128], [0, NTL], [1, E]]), op=ALU.add),
                r=["dest", "pst"], w=["dest"])
            dve(lambda v: v.tensor_tensor(out=big[:], in0=_ap(lg_all[:], [[NTL * E, 128], [E, NTL], [0, 4], [1, E]]),
                                          in1=_ap(mx8[:], [[NTL * 8, 128], [8, NTL], [1, 4], [0, E]]), op=ALU.is_equal),
                r=LGR + MXR, w=["big"])
            dve(lambda v: v.tensor_tensor(out=big[:], in0=big[:], in1=_ap(pre[:], [[NTL * E, 128], [E, NTL], [0, 4], [1, E]]),
                                          op=ALU.mult), r=["big", "dest"], w=["big"])
            dve(lambda v: v.tensor_reduce(out=slotf[:], in_=big[:], axis=AX.X, op=ALU.add), r=["big"], w=["slotf"])
            dve(lambda v: v.tensor_copy(out=sloti[:], in_=slotf[:].rearrange("p t k -> p (t k)")), r=["slotf"], w=["sloti"])
            dve(lambda v: v.tensor_tensor(out=cmpb[:], in0=_ap(pend[:], [[E, 128], [0, nstep], [1, E]]),
                                          in1=_ap(bthr[:], [[nstep, 128], [1, nstep], [0, E]]), op=ALU.is_le),
                r=["pend", "bthr"], w=["cmpb"])
            dve(lambda v: v.tensor_reduce(out=bef[:], in_=cmpb[:], axis=AX.X, op=ALU.add), r=["cmpb"], w=["bef"])
            dve(lambda v: v.tensor_scalar(out=bef[:], in0=bef[:], scalar1=float(E - 1), scalar2=None, op0=ALU.min),
                r=["bef"], w=["bef"])
            dve(lambda v: v.tensor_copy(out=bei[:], in_=bef[:]), r=["bef"], w=["bei"])
            dve(lambda v: v.tensor_scalar(out=ohT[:], in0=bef[:, :], scalar1=iota_p[:, 0:1], scalar2=None, op0=ALU.is_equal),
                r=["bef", "iota_p"], w=["ohT"])
            pool(lambda g: g.iota(kcp_i[:], pattern=[[128, 8]], base=0, channel_multiplier=1), w=["kcp_i"])
            dve(lambda v: v.tensor_copy(out=kcp[:], in_=kcp_i[:]), r=["kcp_i"], w=["kcp"])
            dve(lambda v: v.tensor_scalar(out=bef1k[:], in0=bef[:], scalar1=float(D), scalar2=None, op0=ALU.mult), r=["bef"], w=["bef1k"])
            dve(lambda v: v.tensor_scalar(out=inact[:], in0=bthr[:], scalar1=pend[:, E - 1:E], scalar2=None, op0=ALU.is_ge),
                r=["bthr", "pend"], w=["inact"])
            dve(lambda v: v.scalar_tensor_tensor(out=bef1k[:], in0=inact[:], scalar=1.0e6, in1=bef1k[:], op0=ALU.mult, op1=ALU.add),
                r=["inact", "bef1k"], w=["bef1k"])
            dve(lambda v: v.tensor_copy(out=ohTb[:], in_=ohT[0:E, :]), r=["ohT"], w=["ohTb"])
            for c16 in range(16):
                bk = 4 + c16 // 8
                pe(lambda tt, c16=c16, bk=bk: tt.matmul(ps[bk][:, (c16 % 8) * nstep:(c16 % 8 + 1) * nstep],
                                                        lhsT=bgu[:, c16 * 128:(c16 + 1) * 128], rhs=ohTb[:], start=True, stop=True),
                   r=["bgu", "ohTb"], w=[PSR(bk)])
            act(lambda a: a.activation(out=bsel[:, 0:8, :], in_=ps[4][:, 0:8 * nstep].rearrange("p (c b) -> p c b", c=8), func=AF.Copy),
                r=[PSR(4)], w=["bsel"])
            dve(lambda v: v.tensor_scalar(out=bsel[:, 8:16, :], in0=ps[5][:, 0:8 * nstep].rearrange("p (c b) -> p c b", c=8),
                                          scalar1=1.0, scalar2=None, op0=ALU.add), r=[PSR(5), "bsel"], w=["bsel"])
            dve(lambda v: v.tensor_tensor(out=widx_f[:], in0=_ap(bef1k[:], [[nstep, 128], [1, nstep], [0, 8]]),
                                          in1=_ap(kcp[:], [[8, 128], [0, nstep], [1, 8]]), op=ALU.add),
                r=["bef1k", "kcp"], w=["widx_f"])
            dve(lambda v: v.tensor_copy(out=widx[:], in_=widx_f[:].rearrange("p b c -> p (b c)")), r=["widx_f"], w=["widx"])
            if debug:
                dma("sp", dbg["lg"].ap(), lg_all[:], r=LGR, w=["dbg_lg"])
                dma("sp", dbg["slot"].ap(), slotf[:], r=["slotf"], w=["dbg_slot"])
                dma("sp", dbg["g4"].ap(), g4[:], r=G4R, w=["dbg_g4"])
                dma("sp", dbg["be"].ap(), bef[:], r=["bef"], w=["dbg_be"])

            P.barrier()
            P.emit()
            rt_scope.__exit__(None, None, None)
            G4R = [("g4", t) for t in range(NTL)]
            st_scope = ExitStack()
            st_scope.__enter__()
            if do_moe:
                bcreg = nc.gpsimd.alloc_register("bcreg")
                ohb = [sb("ohb%d" % i, [128, 128], BF16, st_scope) for i in range(2)]
                Wgu = [sb("Wgu%d" % i, [128, 8, 2 * D], BF16, st_scope) for i in range(2)]
                Wdn = [sb("Wdn%d" % i, [128, 8, D], BF16, st_scope) for i in range(2)]
                xr = [sb("xr%d" % i, [128, D], BF16, st_scope) for i in range(8)]
                xT = [sb("xT%d" % i, [128, 8, 512], BF16, st_scope) for i in range(2)]
                actT = sb("actT", [128, 8, 512], BF16, st_scope)
                gm = [sb("gm%d" % i, [128, 512], F32, st_scope) for i in range(2)]
                sg = [sb("sg%d" % i, [128, 512], F32, st_scope) for i in range(2)]
                uc = [sb("uc%d" % i, [128, 512], F32, st_scope) for i in range(2)]
                ost = [sb("ost%d" % i, [128, D], BF16, st_scope) for i in range(2)]
            if do_moe:
                H2_tiles = H2_d.ap().rearrange("(t p) d -> t p d", p=128)
                X1_tiles = X1_d.ap().rearrange("(t p) d -> t p d", p=128)
                out_tiles = out_d.ap().rearrange("(t p) d -> t p d", p=128)
                wgu2d = wgu_d.ap().rearrange("e k n -> (e k) n")
                wd2d = wd_d.ap().rearrange("e k n -> (e k) n")
                wreg = nc.gpsimd.alloc_register("wreg")

                def load_weights(bstep):
                    par = bstep % 2
                    for kc in range(8):
                        def fn(g, kc=kc):
                            if bstep == 0 and kc == 0:
                                g.reg_mov(wreg, E * D - 1)
                            return g.indirect_dma_start(
                                out=Wgu[par][:, kc, :], out_offset=None, in_=wgu2d,
                                in_offset=bass.IndirectOffsetOnAxis(ap=widx[:, bstep * 8 + kc:bstep * 8 + kc + 1], axis=0),
                                bounds_check=wreg, oob_is_err=False)
                        P.op("pool", fn, r=["widx"], w=[("Wgu", par, kc)], dma=True)
                    for kc in range(8):
                        P.op("pool", lambda g, kc=kc: g.indirect_dma_start(
                            out=Wdn[par][:, kc, :], out_offset=None, in_=wd2d,
                            in_offset=bass.IndirectOffsetOnAxis(ap=widx[:, bstep * 8 + kc:bstep * 8 + kc + 1], axis=0),
                            bounds_check=wreg, oob_is_err=False), r=["widx"], w=[("Wdn", par, kc)], dma=True)

                load_weights(0)
                for t in range(NTL):
                    sl = t % 4
                    dma("sp", xr[sl][:], H2_tiles[t], w=[("xr", 0, sl)])
                    for k in range(4):
                        def _scat(g, sl=sl, t=t, k=k):
                            if t == 0 and k == 0:
                                g.reg_mov(bcreg, NSLOT - 1)
                            return g.indirect_dma_start(
                                out=XB_d[:, :], out_offset=bass.IndirectOffsetOnAxis(ap=sloti[:, t * 4 + k:t * 4 + k + 1], axis=0),
                                in_=xr[sl][:], in_offset=None, bounds_check=bcreg, oob_is_err=False)
                        P.op("pool", _scat,
                            r=[("xr", 0, sl), "sloti"], w=[("XBs", t, k)], dma=True)
                XBS = [("XBs", t, k) for t in range(NTL) for k in range(4)]
                P.op("sp", None, r=XBS, w=["XBjoin"])

                def emit_xload(bs):
                    for rt in range(4):
                        dma("sp", xr[(bs % 2) * 4 + rt][:], XB_d[bs * RB + rt * 128: bs * RB + (rt + 1) * 128, :],
                            r=["XBjoin"], w=[("xr", bs % 2, rt)])

                def emit_xT(bs):
                    xTs = xT[bs % 2]
                    for rt in range(4):
                        bank = rt % 2
                        src_t = xr[(bs % 2) * 4 + rt]
                        for kc in range(8):
                            pe(lambda tt, kc=kc, src_t=src_t, bank=bank: tt.transpose(psb(bank)[:, kc * 128:(kc + 1) * 128],
                                                                                     src_t[:, kc * 128:(kc + 1) * 128], ident[:]),
                               r=[("xr", bs % 2, rt), "ident"], w=[PSR(bank)])
                        act(lambda a, bank=bank, rt=rt, xTs=xTs: a.activation(
                            out=xTs[:, :, rt * 128:(rt + 1) * 128], in_=psb(bank).rearrange("p (k t) -> p k t", k=8), func=AF.Copy),
                            r=[PSR(bank)], w=[("xT", bs % 2)])

                emit_xload(0)
                emit_xT(0)
                if nstep > 1:
                    emit_xload(1)
                for bstep in range(nstep):
                    par = bstep % 2
                    if bstep + 1 < nstep:
                        load_weights(bstep + 1)
                    WG = [("Wgu", par, q) for q in range(8)]
                    WD = [("Wdn", par, q) for q in range(8)]
                    xTs = xT[par]
                    dve(lambda v, bstep=bstep, par=par: v.tensor_copy(out=ohb[par][:], in_=_ap(ohT[:], [[nstep, 128], [0, 128]], off=bstep)),
                        r=["ohT"], w=[("ohb", par)])
                    for fc in range(8):
                        bg_, bu_ = 2 + 2 * (fc % 2), 3 + 2 * (fc % 2)
                        for (bank, col0) in ((bg_, fc * 128), (bu_, D + fc * 128)):
                            for kc in range(8):
                                pe(lambda tt, kc=kc, bank=bank, col0=col0, par=par, xTs=xTs: tt.matmul(
                                    ps[bank][:], lhsT=Wgu[par][:, kc, col0:col0 + 128], rhs=xTs[:, kc, :],
                                    start=(kc == 0), stop=(kc == 7)), r=WG + [("xT", par)], w=[PSR(bank)])
                        s2 = fc % 2
                        dve(lambda v, bg_=bg_, s2=s2, fc=fc, bstep=bstep: v.tensor_scalar(
                            out=gm[s2][:], in0=ps[bg_][:], scalar1=bsel[:, fc, bstep:bstep + 1], scalar2=7.0, op0=ALU.add, op1=ALU.min),
                            r=[PSR(bg_), "bsel"], w=[("gm", s2)])
                        act(lambda a, s2=s2: a.activation(out=sg[s2][:], in_=gm[s2][:], func=AF.Sigmoid, scale=1.702),
                            r=[("gm", s2)], w=[("sg", s2)])
                        dve(lambda v, bu_=bu_, s2=s2, fc=fc, bstep=bstep: v.tensor_scalar(
                            out=uc[s2][:], in0=ps[bu_][:], scalar1=bsel[:, 8 + fc, bstep:bstep + 1], scalar2=8.0, op0=ALU.add, op1=ALU.min),
                            r=[PSR(bu_), "bsel"], w=[("uc", s2)])
                        dve(lambda v, s2=s2: v.tensor_tensor(out=gm[s2][:], in0=gm[s2][:], in1=sg[s2][:], op=ALU.mult),
                            r=[("gm", s2), ("sg", s2)], w=[("gm", s2)])
                        dve(lambda v, s2=s2, fc=fc: v.scalar_tensor_tensor(out=actT[:, fc, :], in0=uc[s2][:], scalar=-6.0, in1=gm[s2][:],
                                                                           op0=ALU.max, op1=ALU.mult),
                            r=[("uc", s2), ("gm", s2)], w=[("actT", fc)])
                    if bstep + 1 < nstep:
                        emit_xT(bstep + 1)
                    if bstep + 2 < nstep:
                        emit_xload(bstep + 2)
                    ACTR = [("actT", fc) for fc in range(8)]
                    for rt in range(4):
                        o_ = ost[rt % 2]
                        for half in range(2):
                            bank = 6 + half
                            for fc in range(8):
                                pe(lambda tt, fc=fc, rt=rt, half=half, bank=bank, par=par: tt.matmul(
                                    ps[bank][:], lhsT=actT[:, fc, rt * 128:(rt + 1) * 128],
                                    rhs=Wdn[par][:, fc, half * 512:(half + 1) * 512], start=(fc == 0), stop=False),
                                   r=ACTR + WD, w=[PSR(bank)])
                            pe(lambda tt, half=half, bank=bank, par=par: tt.matmul(
                                ps[bank][:], lhsT=ohb[par][:, 0:128], rhs=bdn[:, half * 512:(half + 1) * 512], start=False, stop=True),
                               r=["bdn", ("ohb", par)], w=[PSR(bank)])
                            if False:
                                pass
                            else:
                                dve(lambda v, half=half, bank=bank, o_=o_: v.tensor_copy(out=o_[:, half * 512:(half + 1) * 512], in_=ps[bank][:]),
                                    r=[PSR(bank)], w=[("ost", rt % 2, half)])
                        dma("sp", OB_d[bstep * RB + rt * 128: bstep * RB + (rt + 1) * 128, :], o_[:],
                            r=[("ost", rt % 2, 0), ("ost", rt % 2, 1)], w=[("OB", bstep, rt)])
                OBR = [("OB", bs_, rt) for bs_ in range(nstep) for rt in range(4)]
                P.op("sp", None, r=OBR, w=["OBjoin"])
                P.barrier()
                P.emit()
                st_scope.__exit__(None, None, None)
                st_scope = ExitStack()
                st_scope.__enter__()
                NG = 3
                gr = [[sb("gr%d_%d" % (j_, i), [128, D], BF16, st_scope) for i in range(4)] for j_ in range(NG)]
                acc = [sb("acc%d" % i, [128, D], F32, st_scope) for i in range(2)]
                x1r = [sb("x1r%d" % i, [128, D], F32, st_scope) for i in range(3)]
                G2 = sb("G2", [128, nseq, D], F32, st_scope)
                for b_ in range(nseq):
                    dma("sp", G2[:, b_, :], MOD_d[b_, 5], w=[("G2", b_, 0), ("G2", b_, 1)])
                for t in range(NTL):
                    b = t // 16
                    sl = t % 3
                    gs = t % NG
                    ac = acc[t % 2]
                    acr = ("acc", t % 2)
                    dma("sp", x1r[sl][:], X1_tiles[t], w=[("x1r", sl)])
                    for k in range(4):
                        def _gath(g, t=t, k=k, gs=gs):
                            if t == 0 and k == 0:
                                g.reg_mov(bcreg, NSLOT - 1)
                            return g.indirect_dma_start(
                                out=gr[gs][k][:], out_offset=None, in_=OB_d[:, :],
                                in_offset=bass.IndirectOffsetOnAxis(ap=sloti[:, t * 4 + k:t * 4 + k + 1], axis=0),
                                bounds_check=bcreg, oob_is_err=False)
                        P.op("pool", _gath, r=["OBjoin", "sloti"], w=[("gr", gs, k)], dma=True)
                    act(lambda a, t=t, gs=gs, ac=ac: a.activation(out=ac[:], in_=gr[gs][0][:], func=AF.Copy, scale=g4[:, t, 0:1]),
                        r=[("gr", gs, 0)] + G4R, w=[acr])
                    for k in range(1, 4):
                        dve(lambda v, t=t, k=k, gs=gs, ac=ac: v.scalar_tensor_tensor(out=ac[:], in0=gr[gs][k][:], scalar=g4[:, t, k:k + 1], in1=ac[:],
                                                                                    op0=ALU.mult, op1=ALU.add), r=[("gr", gs, k), acr] + G4R, w=[acr])
                    dve(lambda g, b=b, ac=ac: g.tensor_tensor(out=ac[:], in0=ac[:], in1=G2[:, b, :], op=ALU.mult),
                        r=[acr, ("G2", b, 0), ("G2", b, 1)], w=[acr])
                    dve(lambda g, sl=sl, ac=ac: g.tensor_tensor(out=x1r[sl][:], in0=ac[:], in1=x1r[sl][:], op=ALU.add),
                        r=[acr, ("x1r", sl)], w=[("x1r", sl), acr])
                    dma("sp", out_tiles[t], x1r[sl][:], r=[("x1r", sl)], w=[("out", t)])
            else:
                x1r = [sb("x1r%d" % i, [128, D], F32, st_scope) for i in range(2)]
                X1_tiles = X1_d.ap().rearrange("(t p) d -> t p d", p=128)
                out_tiles = out_d.ap().rearrange("(t p) d -> t p d", p=128)
                for t in range(NTL):
                    sl = t % 2
                    dma("sp", x1r[sl][:], X1_tiles[t], w=[("x1r", sl)])
                    dma("sp", out_tiles[t], x1r[sl][:], r=[("x1r", sl)], w=[("out", t)])
            P.barrier()
            P.emit()
            st_scope.__exit__(None, None, None)
    return nc


def make_in_maps(inputs, ncores=NCORES, nseq=NSEQ):
    f = lambda a: np.ascontiguousarray(np.asarray(a, dtype=np.float32))
    x = f(inputs["x"])
    c = f(inputs["c"])
    shared = {
        "w_ada": f(inputs["w_ada"][0]),
        "b_ada": f(inputs["b_ada"][0]).reshape(1, -1),
        "norm1_g": f(inputs["norm1_g"][0]).reshape(1, -1),
        "norm2_g": f(inputs["norm2_g"][0]).reshape(1, -1),
        "w_in": f(inputs["w_in"][0]),
        "gq": f(np.tile(np.asarray(inputs["q_norm_g"][0]), 2).reshape(128, 1)),
        "gk": f(np.tile(np.asarray(inputs["k_norm_g"][0]), 2).reshape(128, 1)),
        "lamv": f(np.stack([inputs["lambda_q1"][0], inputs["lambda_q2"][0], inputs["lambda_k1"][0], inputs["lambda_k2"][0]])),
        "subln_g": f(inputs["subln_g"][0]).reshape(1, -1),
        "conv_wT": f(np.asarray(inputs["conv_w"][0]).reshape(31, 4, 128).transpose(2, 1, 0)),
        "conv_b": f(np.asarray(inputs["conv_b"][0]).reshape(4, 128).T),
        "conv_ln_g": f(np.asarray(inputs["conv_ln_g"][0]).reshape(4, 128).T),
        "conv_ln_b": f(np.asarray(inputs["conv_ln_b"][0]).reshape(4, 128).T),
        "w_out": f(inputs["w_out"][0]),
        "w_router": f(inputs["w_router"][0]),
        "b_router": f(inputs["b_router"][0]).reshape(1, -1),
        "w_gate_up": f(inputs["w_gate_up"][0]),
        "b_gate_up": f(inputs["b_gate_up"][0]),
        "w_down": f(inputs["w_down"][0]),
        "b_down": f(inputs["b_down"][0]),
    }
    maps = []
    for i in range(ncores):
        m = dict(shared)
        m["x"] = np.ascontiguousarray(x[i * nseq:(i + 1) * nseq].reshape(nseq * S, D))
        cc = c[i * nseq:(i + 1) * nseq]
        m["cT"] = np.ascontiguousarray(cc.reshape(nseq, 8, 128).transpose(2, 1, 0))
        maps.append(m)
    return maps


_NC_CACHE = {}


def kernel(**inputs):
    if "nc" not in _NC_CACHE:
        _NC_CACHE["nc"] = build_program()
    nc = _NC_CACHE["nc"]
    maps = make_in_maps(inputs)
    res = run_bass_kernel_spmd(nc, maps, core_ids=list(range(NCORES)))
    outs = [np.asarray(r["out"]).reshape(NSEQ, S, D) for r in res.results]
    return np.concatenate(outs, axis=0).astype(np.float32)
```

```python
import math
from contextlib import ExitStack
import numpy as np
import concourse.bass as bass
import concourse.mybir as mybir
from concourse.bass_utils import run_bass_kernel_spmd

F32 = mybir.dt.float32
BF16 = mybir.dt.bfloat16
I32 = mybir.dt.int32
AF = mybir.ActivationFunctionType
ALU = mybir.AluOpType
AX = mybir.AxisListType

NCORES = 8
D = 1024
S = 2048
NSEQ = 2
NT = NSEQ * S // 128
E = 32
RB = 512
NSTEP = (NT * 128 * 4) // RB + E
EPS = 1e-5
LAM_INIT = 0.8 - 0.6 * math.exp(0.0)
NDQ = 12


class _Op:
    __slots__ = ("eng", "fn", "dma", "deps", "milestone", "sem", "val", "know")


class Prog:
    ENGS = ("pe", "act", "dve", "pool", "sp")

    def __init__(self, nc, es):
        self.nc = nc
        self.ops = []
        self.emitted = 0
        self.last_w = {}
        self.readers = {}
        self.esem = {e: es.enter_context(nc.semaphore("tl_" + e)) for e in self.ENGS}
        self.dsem = {e: [es.enter_context(nc.semaphore("dq_%s%d" % (e, i))) for i in range(NDQ)]
                     for e in ("sp", "act", "pool")}
        self.ecount = {e: 0 for e in self.ENGS}
        self.dcount = {e: 0 for e in self.dsem}
        self.dhist = {e: [] for e in self.dsem}
        self.know = {e: {} for e in self.ENGS}
        self.live_dma = []
        self.last_real = {}

    def op(self, eng, fn, r=(), w=(), dma=False, extra=()):
        o = _Op()
        o.eng, o.fn, o.dma = eng, fn, dma
        deps = set(extra)
        for x in r:
            if x in self.last_w:
                deps.add(self.last_w[x])
        for x in w:
            if x in self.last_w:
                deps.add(self.last_w[x])
            for rd in self.readers.get(x, ()):
                deps.add(rd)
        idx = len(self.ops)
        o.deps = deps
        o.milestone = False
        o.know = None
        self.ops.append(o)
        for x in r:
            self.readers.setdefault(x, []).append(idx)
        for x in w:
            self.last_w[x] = idx
            self.readers[x] = []
        if dma:
            self.live_dma.append(idx)
        elif fn is not None:
            self.last_real[eng] = idx
        return idx

    def barrier(self):
        firsts = []
        for e in self.ENGS:
            ex = list(self.live_dma) if e == "sp" else []
            if e in self.last_real:
                ex.append(self.last_real[e])
            firsts.append(self.op(e, None, w=[("bar", e)], extra=ex))
        self.live_dma = []
        self.last_real = {}
        for e in self.ENGS:
            self.op(e, None, r=[("bar", x) for x in self.ENGS], w=[("bar2", e)])
        self.last_w = {k: v for k, v in self.last_w.items() if k[0] == "bar2"}
        self.readers = {}

    def emit(self):
        nc = self.nc
        ops = self.ops
        start = self.emitted
        for o in ops[start:]:
            for d in o.deps:
                ops[d].milestone = True
        plan = {e: [] for e in self.ENGS}
        for i in range(start, len(ops)):
            o = ops[i]
            e = o.eng
            know = self.know[e]
            waits = []
            if o.dma:
                j = self.dcount[e]
                self.dcount[e] += 1
                o.sem = self.dsem[e][j % NDQ]
                o.val = 16 * (j // NDQ + 1)
                if j >= NDQ:
                    o.deps.add(self.dhist[e][j - NDQ])
                self.dhist[e].append(i)
            for d in sorted(o.deps):
                p = ops[d]
                if (not p.dma) and p.eng == "pe" and e == "pe" and not o.dma and o.fn is not None:
                    continue
                assert p.sem is not None, "dependency on op that was never made a milestone"
                key = id(p.sem)
                if know.get(key, (None, 0))[1] >= p.val:
                    continue
                waits.append((p.sem, p.val))
                if p.know:
                    for k2, v2 in p.know.items():
                        if know.get(k2, (None, 0))[1] < v2[1]:
                            know[k2] = v2
                know[key] = (p.sem, p.val)
            if o.dma:
                o.know = dict(know)
            elif o.milestone or o.fn is None:
                self.ecount[e] += 1
                o.sem = self.esem[e]
                o.val = self.ecount[e]
                o.milestone = True
                snap = dict(know)
                snap[id(o.sem)] = (o.sem, o.val)
                o.know = snap
            else:
                o.sem = None
                o.val = 0
            plan[e].append((o, waits))
        self.emitted = len(ops)
        attr = {"pe": "tensor", "act": "scalar", "dve": "vector", "pool": "gpsimd", "sp": "sync"}

        def run(engname, eng):
            for o, waits in plan[engname]:
                best = {}
                for sem, val in waits:
                    k = id(sem)
                    if k not in best or best[k][1] < val:
                        best[k] = (sem, val)
                for sem, val in best.values():
                    eng.wait_ge(sem, val)
                if o.fn is None:
                    eng.sem_inc(o.sem, 1)
                    continue
                ins = o.fn(eng)
                if o.dma:
                    ins.then_inc(o.sem, 16)
                elif o.milestone:
                    ins.then_inc(o.sem, 1)

        with nc.Block() as block:
            for engname in self.ENGS:
                if not plan[engname]:
                    continue
                deco = getattr(block, attr[engname])

                def body(eng, engname=engname):
                    run(engname, eng)
                deco(body)


def _ap(base, dims, off=0):
    return bass.AP(tensor=base.tensor, offset=base.offset + off, ap=[list(d) for d in dims])


def build_program(debug=False, nseq=NSEQ, do_moe=True, stop=None):
    nc = bass.Bass("TRN2", target_bir_lowering=False)
    NTOK = nseq * S
    NTL = NTOK // 128
    nstep = (NTOK * 4) // RB + E
    NSLOT = nstep * RB

    def din(name, shape, dt=F32):
        return nc.dram_tensor(name, list(shape), dt, kind="ExternalInput")

    x_d = din("x", [NTOK, D])
    cT_d = din("cT", [128, 8, nseq])
    wada_d = din("w_ada", [D, 6 * D])
    bada_d = din("b_ada", [1, 6 * D])
    n1g_d = din("norm1_g", [1, D])
    n2g_d = din("norm2_g", [1, D])
    win_d = din("w_in", [D, 2560])
    gq_d = din("gq", [128, 1])
    gk_d = din("gk", [128, 1])
    lamv_d = din("lamv", [4, 64])
    subg_d = din("subln_g", [1, 128])
    cw_d = din("conv_wT", [128, 4, 31])
    cb_d = din("conv_b", [128, 4])
    clg_d = din("conv_ln_g", [128, 4])
    clb_d = din("conv_ln_b", [128, 4])
    wout_d = din("w_out", [D, D])
    wr_d = din("w_router", [D, E])
    br_d = din("b_router", [1, E])
    wgu_d = din("w_gate_up", [E, D, 2 * D])
    bgu_d = din("b_gate_up", [E, 2 * D])
    wd_d = din("w_down", [E, D, D])
    bd_d = din("b_down", [E, D])
    out_d = nc.dram_tensor("out", [NTOK, D], F32, kind="ExternalOutput")
    X1_d = nc.dram_tensor("X1s", [NTOK, D], F32)
    H2_d = nc.dram_tensor("H2s", [NTOK, D], BF16)
    XB_d = nc.dram_tensor("XBs", [NSLOT, D], BF16)
    OB_d = nc.dram_tensor("OBs", [NSLOT, D], BF16)
    dbg = {}
    if debug:
        dbg["x1"] = nc.dram_tensor("dbg_x1", [NTOK, D], F32, kind="ExternalOutput")
        dbg["lg"] = nc.dram_tensor("dbg_lg", [128, NTL, E], F32, kind="ExternalOutput")
        dbg["slot"] = nc.dram_tensor("dbg_slot", [128, NTL, 4], F32, kind="ExternalOutput")
        dbg["g4"] = nc.dram_tensor("dbg_g4", [128, NTL, 4], F32, kind="ExternalOutput")
        dbg["be"] = nc.dram_tensor("dbg_be", [128, nstep], F32, kind="ExternalOutput")

    es = ExitStack()
    with es:
        P = Prog(nc, es)

        _cnt = [0]

        def sb(name, shape, dt, stack=es):
            _cnt[0] += 1
            return stack.enter_context(nc.sbuf_tensor("s%d_%s" % (_cnt[0], name), list(shape), dt))

        ps = [es.enter_context(nc.psum_tensor("ps%d" % i, [128, 512], F32)) for i in range(8)]

        def PSR(i):
            return ("ps", i)

        def psb(i):
            return ps[i][:].bitcast(BF16)

        ident = sb("ident", [128, 128], BF16)
        ones_bf = sb("ones_bf", [128, 512], BF16)
        zero_bf = sb("zero_bf", [128, 512], BF16)
        lg_all = sb("lg_all", [128, NTL, E], F32)
        mx8 = sb("mx8", [128, NTL, 8], F32)
        Mb = sb("Mb", [128, NTL * E], BF16)
        g4 = sb("g4", [128, NTL, 4], F32)
        nmx = sb("nmx", [128, NTL], F32)
        gsm = sb("gsm", [128, NTL], F32)
        iota_p = sb("iota_p", [128, 1], F32)
        iota_f = sb("iota_f", [128, 128], F32)

        def dve(fn, r=(), w=()):
            return P.op("dve", fn, r, w)

        def act(fn, r=(), w=()):
            return P.op("act", fn, r, w)

        def pool(fn, r=(), w=()):
            return P.op("pool", fn, r, w)

        def pe(fn, r=(), w=()):
            return P.op("pe", fn, r, w)

        def dma(eng, out, in_, r=(), w=(), **kw):
            return P.op(eng, lambda q: q.dma_start(out=out, in_=in_, **kw), r, w, dma=True)

        zf_state = [0]
        ZF_TOTAL = (NSLOT // 128) * 2

        def zero_fill(n):
            if not do_moe:
                return
            for _ in range(n):
                i = zf_state[0]
                if i >= ZF_TOTAL:
                    return
                zf_state[0] += 1
                r0, hf = (i // 2) * 128, i % 2
                dma("pool", XB_d[r0:r0 + 128, hf * 512:(hf + 1) * 512], zero_bf[:], r=["zero_bf"], w=[("XBz", i)])

        MOD_d = nc.dram_tensor("MODs", [nseq, 6, 128, D], F32)
        x_tiles = x_d.ap().rearrange("(t p) d -> t p d", p=128)
        X1_tiles = X1_d.ap().rearrange("(t p) d -> t p d", p=128)
        H2_tiles = H2_d.ap().rearrange("(t p) d -> t p d", p=128)
        slopes = [2.0 ** (-8.0 * (h + 1) / 4) for h in range(4)]

        with ExitStack() as cs:
            it_i = sb("it_i", [128, 128], I32, cs)
            ip_i = sb("ip_i", [128, 1], I32, cs)
            cT = sb("cT", [128, 8, nseq], F32, cs)
            scT = sb("scT", [128, 8, nseq], F32, cs)
            bcl2 = [sb("bcl%d" % i, [128, 8, 128], BF16, cs) for i in range(nseq)]
            wa_st = [sb("wa_st%d" % i, [128, 8, 512], BF16, cs) for i in range(6)]
            bada_b = [sb("bada_b%d" % i, [128, 512], F32, cs) for i in range(2)]
            g1b = sb("g1b", [128, D], F32, cs)
            g2b = sb("g2b", [128, D], F32, cs)
            modt2 = [[sb("modt%d_%d" % (b_i, i), [128, D], F32, cs) for i in range(6)] for b_i in range(nseq)]
            pool(lambda g: g.iota(it_i[:], pattern=[[1, 128]], base=0, channel_multiplier=0), w=["it_i"])
            pool(lambda g: g.iota(ip_i[:], pattern=[[1, 1]], base=0, channel_multiplier=1), w=["ip_i"])
            dve(lambda v: v.tensor_copy(out=iota_f[:], in_=it_i[:]), r=["it_i"], w=["iota_f"])
            dve(lambda v: v.tensor_copy(out=iota_p[:], in_=ip_i[:]), r=["ip_i"], w=["iota_p"])
            dve(lambda v: v.tensor_scalar(out=ident[:], in0=iota_f[:], scalar1=iota_p[:, 0:1], scalar2=None,
                                          op0=ALU.is_equal), r=["iota_f", "iota_p"], w=["ident"])
            dve(lambda v: v.memset(ones_bf[:], 1.0), w=["ones_bf"])
            dve(lambda v: v.memset(zero_bf[:], 0.0), w=["zero_bf"])
            dma("sp", cT[:], cT_d.ap(), w=["cT"])
            dma("sp", g1b[:], _ap(n1g_d.ap(), [[0, 128], [1, D]]), w=["g1b"])
            dma("sp", g2b[:], _ap(n2g_d.ap(), [[0, 128], [1, D]]), w=["g2b"])
            act(lambda a: a.activation(out=scT[:], in_=cT[:], func=AF.Silu), r=["cT"], w=["scT"])
            for b in range(nseq):
                dve(lambda v, b=b: v.tensor_copy(out=bcl2[b][:], in_=_ap(scT[:], [[8 * nseq, 128], [nseq, 8], [0, 128]], off=b)),
                    r=["scT"], w=[("bcl", b)])
            for cc in range(12):
                st = wa_st[cc % 6]
                dma("pool", st[:], wada_d.ap().rearrange("(kc p) n -> p kc n", p=128)[:, :, cc * 512:(cc + 1) * 512],
                    w=[("wa_st", cc % 6)])
                dma("sp", bada_b[cc % 2][:], _ap(bada_d.ap(), [[0, 128], [1, 512]], off=cc * 512), w=[("bada_b", cc % 2)])
                which, half = cc // 2, cc % 2
                for b in range(nseq):
                    bank = (cc * nseq + b) % 4
                    for kc in range(8):
                        pe(lambda t, kc=kc, st=st, bank=bank, b=b: t.matmul(ps[bank][:], lhsT=bcl2[b][:, kc, :], rhs=st[:, kc, :],
                                                                             start=(kc == 0), stop=(kc == 7)),
                           r=[("bcl", b), ("wa_st", cc % 6)], w=[PSR(bank)])
                    dve(lambda v, bank=bank, which=which, half=half, b=b, cc=cc: v.tensor_tensor(
                        out=modt2[b][which][:, half * 512:(half + 1) * 512], in0=ps[bank][:], in1=bada_b[cc % 2][:], op=ALU.add),
                        r=[PSR(bank), ("bada_b", cc % 2)], w=[("modt", b, which)])
            for b in range(nseq):
                for (which, gx, gname) in ((1, g1b, "g1b"), (4, g2b, "g2b")):
                    dve(lambda v, which=which, gx=gx, b=b: v.scalar_tensor_tensor(out=modt2[b][which][:], in0=modt2[b][which][:], scalar=1.0,
                                                                                 in1=gx[:], op0=ALU.add, op1=ALU.mult),
                        r=[("modt", b, which), gname], w=[("modt", b, which)])
                for which in range(6):
                    dma("sp", MOD_d[b, which], modt2[b][which][:], r=[("modt", b, which)], w=[("MODd", b, which)])
            if stop == "ada" and debug:
                dma("sp", dbg["x1"].ap()[0:128, :], modt2[0][1][:], r=[("modt", 0, 1)], w=["dbgada"])
            P.barrier()
            P.emit()
            if stop == "ada":
                return nc

        with ExitStack() as pa:
            W_r = sb("W_r", [128, 8, E], BF16, pa)
            brb = sb("brb", [128, E], F32, pa)
            gq = sb("gq", [128, 1], F32, pa)
            gk = sb("gk", [128, 1], F32, pa)
            lams = sb("lams", [128, 2], F32, pa)
            neglam = sb("neglam", [128, 1], F32, pa)
            cw = sb("cw", [128, 4, 31], F32, pa)
            cwb = sb("cwb", [128, 4, 31], BF16, pa)
            cb = sb("cb", [128, 4], F32, pa)
            clg = sb("clg", [128, 4], F32, pa)
            clb = sb("clb", [128, 4], F32, pa)
            blk1 = sb("blk1", [128, 128], BF16, pa)
            om512 = sb("om512", [128, 128], BF16, pa)
            eps_c = sb("eps_c", [128, 1], F32, pa)
            hbuf = sb("hbuf", [128, 4, S + 32], BF16, pa)
            qT = sb("qT", [128, 4, S], BF16, pa)
            kT = sb("kT", [128, 4, S], BF16, pa)
            vS = sb("vS", [128, 16, 4, 128], BF16, pa)
            xs = [sb("xs%d" % i, [128, D], F32, pa) for i in range(2)]
            tmp1 = sb("tmp1", [128, D], F32, pa)
            htok = [sb("htok%d" % i, [128, D], BF16, pa) for i in range(2)]
            ss = sb("ss", [128, 4], F32, pa)
            sig = [sb("sig%d" % i, [128, 512], BF16, pa) for i in range(2)]
            sq = [sb("sq%d" % i, [128, 512], BF16, pa) for i in range(2)]
            rs = [sb("rs%d" % i, [128, 512], F32, pa) for i in range(2)]
            Eb = [sb("Eb%d" % i, [128, 512], BF16, pa) for i in range(3)]
            Pt = [sb("Pt%d" % i, [128, 512], BF16, pa) for i in range(5)]
            qz = [[sb("qz%d%d" % (i, c_), [128, 512], BF16, pa) for c_ in range(2)] for i in range(2)]
            oT = [sb("oT%d" % i, [128, 512], F32, pa) for i in range(2)]
            om128 = sb("om128", [128, 128], BF16, pa)
            subgc = sb("subgc", [128, 1], F32, pa)
            sm = sb("sm", [128, 8], F32, pa)
            tmp_scope = ExitStack()
            tmp_scope.__enter__()
            pge = sb("pge", [128, 1], F32, tmp_scope)
            lamb = sb("lamb", [128, 4, 64], F32, tmp_scope)
            lamt = sb("lamt", [128, 2, 64], F32, tmp_scope)
            o1 = sb("o1", [128, 128], F32, tmp_scope)

            dma("pool", W_r[:], wr_d.ap().rearrange("(kc p) n -> p kc n", p=128), w=["W_r"])
            dma("sp", brb[:], _ap(br_d.ap(), [[0, 128], [1, E]]), w=["brb"])
            dma("sp", gq[:], gq_d.ap(), w=["gq"])
            dma("sp", gk[:], gk_d.ap(), w=["gk"])
            dma("sp", lamb[:], _ap(lamv_d.ap(), [[0, 128], [64, 4], [1, 64]]), w=["lamb"])
            dma("sp", cw[:], cw_d.ap(), w=["cw"])
            dma("sp", cb[:], cb_d.ap(), w=["cb"])
            dma("sp", clg[:], clg_d.ap(), w=["clg"])
            dma("sp", clb[:], clb_d.ap(), w=["clb"])
            dve(lambda v: v.tensor_scalar(out=gq[:], in0=gq[:], scalar1=0.125, scalar2=None, op0=ALU.mult),
                r=["gq"], w=["gq"])
            dve(lambda v: v.tensor_tensor(out=lamt[:], in0=lamb[:, 0:2, :], in1=lamb[:, 2:4, :], op=ALU.mult),
                r=["lamb"], w=["lamt"])
            dve(lambda v: v.tensor_reduce(out=lams[:], in_=lamt[:], axis=AX.X, op=ALU.add), r=["lamt"], w=["lams"])
            act(lambda a: a.activation(out=lams[:], in_=lams[:], func=AF.Exp), r=["lams"], w=["lams"])
            dve(lambda v: v.tensor_tensor(out=neglam[:], in0=lams[:, 1:2], in1=lams[:, 0:1], op=ALU.subtract),
                r=["lams"], w=["neglam"])
            dve(lambda v: v.tensor_scalar(out=neglam[:], in0=neglam[:], scalar1=-LAM_INIT, scalar2=None, op0=ALU.add),
                r=["neglam"], w=["neglam"])
            dma("sp", subgc[:], _ap(subg_d.ap(), [[1, 128], [1, 1]]), w=["subgc"])
            dve(lambda v: v.tensor_scalar(out=subgc[:], in0=subgc[:], scalar1=(1.0 - LAM_INIT), scalar2=None,
                                          op0=ALU.mult), r=["subgc"], w=["subgc"])
            dve(lambda v: v.memset(om128[:], 1.0 / 128), w=["om128"])
            dve(lambda v: v.tensor_copy(out=cwb[:], in_=cw[:]), r=["cw"], w=["cwb"])
            for i_ in range(2):
                for c_ in range(2):
                    dve(lambda v, i_=i_, c_=c_: v.memset(qz[i_][c_][:], 0.0), w=[("qz", i_)])
            dve(lambda v: v.tensor_scalar(out=o1[:], in0=iota_f[:], scalar1=64.0, scalar2=None, op0=ALU.is_ge),
                r=["iota_f"], w=["o1"])
            dve(lambda v: v.tensor_scalar(out=pge[:], in0=iota_p[:], scalar1=64.0, scalar2=None, op0=ALU.is_ge),
                r=["iota_p"], w=["pge"])
            dve(lambda v: v.tensor_scalar(out=blk1[:], in0=o1[:], scalar1=pge[:, 0:1], scalar2=1.0 / 64,
                                          op0=ALU.is_equal, op1=ALU.mult), r=["o1", "pge"], w=["blk1"])
            dve(lambda v: v.memset(om512[:], 1.0 / 512), w=["om512"])
            dve(lambda v: v.memset(eps_c[:], EPS), w=["eps_c"])
            dve(lambda v: v.memset(hbuf[:], 0.0), w=[("hbuf", i) for i in range(4)])
            dve(lambda v: v.memset(vS[:], 1.0), w=[("vS", t) for t in range(16)])
            P.barrier()
            P.emit()
            tmp_scope.__exit__(None, None, None)
            if stop == "consts":
                return nc

            for b in range(nseq):
                seq_scope = ExitStack()
                seq_scope.__enter__()
                W_out = sb("W_out", [128, 8, D], BF16, seq_scope)
                G1 = sb("G1", [128, D], F32, seq_scope)
                dma("pool", W_out[:], wout_d.ap().rearrange("(kc p) n -> p kc n", p=128), w=["W_out"])
                dma("sp", G1[:], MOD_d[b, 2], w=["G1"])
                with ExitStack() as p1:
                    W_in = sb("W_in", [128, 8, 2560], BF16, p1)
                    hT2 = [sb("hT%d" % i_, [128, 8, 512], BF16, p1) for i_ in range(2)]
                    S1 = sb("S1", [128, D], F32, p1)
                    H1 = sb("H1", [128, D], F32, p1)
                    dma("pool", W_in[:], win_d.ap().rearrange("(kc p) n -> p kc n", p=128), w=["W_in"])
                    dma("sp", H1[:], MOD_d[b, 0], w=["H1"])
                    dma("sp", S1[:], MOD_d[b, 1], w=["S1"])
                    def chain(w_, tl):
                        t = b * 16 + w_ * 4 + tl
                        sl = t % 2
                        zero_fill(-(-ZF_TOTAL // (2 * nseq * 16)))
                        dma("sp", xs[sl][:], x_tiles[t], w=[("xs", sl)])
                        act(lambda a, sl=sl: a.activation(out=htok[sl][:], in_=xs[sl][:], func=AF.Square, scale=D ** -0.5,
                                                          accum_out=ss[:, 0:1]),
                            r=[("xs", sl)], w=[("htok", sl), ("ss", 0)])
                        act(lambda a: a.activation(out=ss[:, 1:2], in_=ss[:, 0:1], func=AF.Ln, bias=eps_c[:, 0:1]),
                            r=[("ss", 0), "eps_c"], w=[("ss", 1)])
                        act(lambda a: a.activation(out=ss[:, 1:2], in_=ss[:, 1:2], func=AF.Exp, scale=-0.5),
                            r=[("ss", 1)], w=[("ss", 1)])
                        dve(lambda v, sl=sl: v.scalar_tensor_tensor(out=tmp1[:], in0=xs[sl][:], scalar=ss[:, 1:2], in1=S1[:],
                                                                    op0=ALU.mult, op1=ALU.mult),
                            r=[("xs", sl), ("ss", 1), "S1"], w=["tmp1"])
                        dve(lambda g, sl=sl: g.tensor_tensor(out=htok[sl][:], in0=tmp1[:], in1=H1[:], op=ALU.add),
                            r=["tmp1", "H1"], w=[("htok", sl)])

                    def trans(w_, tl):
                        t = b * 16 + w_ * 4 + tl
                        sl = t % 2
                        bank = tl % 2
                        hTw = hT2[w_ % 2]
                        for kc in range(8):
                            pe(lambda tt, kc=kc, sl=sl, bank=bank: tt.transpose(psb(bank)[:, kc * 128:(kc + 1) * 128],
                                                                               htok[sl][:, kc * 128:(kc + 1) * 128], ident[:]),
                               r=[("htok", sl), "ident"], w=[PSR(bank)])
                        act(lambda a, bank=bank, tl=tl, hTw=hTw: a.activation(
                            out=hTw[:, :, tl * 128:(tl + 1) * 128],
                            in_=psb(bank).rearrange("p (k t) -> p k t", k=8), func=AF.Copy),
                            r=[PSR(bank)], w=[("hT", w_ % 2)])

                    def glu_chunk(w_, i):
                        hTw = hT2[w_ % 2]
                        hres = ("hT", w_ % 2)
                        tok0 = w_ * 512
                        ba, bg = 2 + 2 * (i % 2), 3 + 2 * (i % 2)
                        for (bank, oc) in ((ba, i), (bg, 4 + i)):
                            for kc in range(8):
                                pe(lambda tt, kc=kc, bank=bank, oc=oc, hTw=hTw: tt.matmul(
                                    ps[bank][:], lhsT=W_in[:, kc, oc * 128:(oc + 1) * 128], rhs=hTw[:, kc, :],
                                    start=(kc == 0), stop=(kc == 7)), r=["W_in", hres], w=[PSR(bank)])
                        act(lambda a, bg=bg, i=i: a.activation(out=sig[i % 2][:], in_=ps[bg][:], func=AF.Sigmoid),
                            r=[PSR(bg)], w=[("sig", i % 2)])
                        dve(lambda v, ba=ba, i=i, tok0=tok0: v.tensor_tensor(
                            out=hbuf[:, i, 15 + tok0:15 + tok0 + 512], in0=ps[ba][:], in1=sig[i % 2][:], op=ALU.mult),
                            r=[PSR(ba), ("sig", i % 2)], w=[("hbuf", i)])

                    def qk_chunk(w_, qi):
                        hTw = hT2[w_ % 2]
                        hres = ("hT", w_ % 2)
                        tok0 = w_ * 512
                        hh = qi % 4
                        isq = qi < 4
                        oc = (8 + hh) if isq else (12 + hh)
                        bd_, bs_ = 2 + 2 * (qi % 2), 3 + 2 * (qi % 2)
                        for kc in range(8):
                            pe(lambda tt, kc=kc, bd_=bd_, oc=oc, hTw=hTw: tt.matmul(
                                ps[bd_][:], lhsT=W_in[:, kc, oc * 128:(oc + 1) * 128], rhs=hTw[:, kc, :],
                                start=(kc == 0), stop=(kc == 7)), r=["W_in", hres], w=[PSR(bd_)])
                        act(lambda a, bd_=bd_, qi=qi: a.activation(out=sq[qi % 2][:], in_=ps[bd_][:], func=AF.Square),
                            r=[PSR(bd_)], w=[("sq", qi % 2)])
                        return (qi, hh, isq, bd_, bs_, tok0)

                    def qk_finish(state):
                        qi, hh, isq, bd_, bs_, tok0 = state
                        pe(lambda tt, bs_=bs_, qi=qi: tt.matmul(ps[bs_][:], lhsT=blk1[:], rhs=sq[qi % 2][:], start=True, stop=True),
                           r=["blk1", ("sq", qi % 2)], w=[PSR(bs_)])
                        act(lambda a, bs_=bs_, qi=qi: a.activation(out=rs[qi % 2][:], in_=ps[bs_][:], func=AF.Ln, bias=eps_c[:, 0:1]),
                            r=[PSR(bs_), "eps_c"], w=[("rs", qi % 2)])
                        act(lambda a, qi=qi: a.activation(out=rs[qi % 2][:], in_=rs[qi % 2][:], func=AF.Exp, scale=-0.5),
                            r=[("rs", qi % 2)], w=[("rs", qi % 2)])
                        dstT = qT if isq else kT
                        gcol = gq if isq else gk
                        dve(lambda v, bd_=bd_, qi=qi, dstT=dstT, gcol=gcol, hh=hh, tok0=tok0: v.scalar_tensor_tensor(
                            out=dstT[:, hh, tok0:tok0 + 512], in0=ps[bd_][:], scalar=gcol[:, 0:1], in1=rs[qi % 2][:],
                            op0=ALU.mult, op1=ALU.mult),
                            r=[PSR(bd_), ("rs", qi % 2), "gq", "gk"], w=[("qk", isq, hh)])

                    def v_tile(w_, tl):
                        hTw = hT2[w_ % 2]
                        hres = ("hT", w_ % 2)
                        bank = 6 + tl % 2
                        kt = w_ * 4 + tl
                        for kc in range(8):
                            pe(lambda tt, kc=kc, bank=bank, tl=tl, hTw=hTw: tt.matmul(
                                ps[bank][:], lhsT=hTw[:, kc, tl * 128:(tl + 1) * 128], rhs=W_in[:, kc, 2048:2560],
                                start=(kc == 0), stop=(kc == 7)), r=["W_in", hres], w=[PSR(bank)])
                        act(lambda a, bank=bank, kt=kt: a.activation(
                            out=vS[:, kt, :, 0:128], in_=ps[bank][:].rearrange("p (h e) -> p h e", h=4), func=AF.Copy),
                            r=[PSR(bank)], w=[("vS", kt)])

                    for tl in range(4):
                        chain(0, tl)
                        trans(0, tl)
                    for w_ in range(4):
                        nxt = w_ + 1 < 4
                        for q in range(4):
                            if nxt:
                                chain(w_ + 1, q)
                            if q == 0:
                                glu_chunk(w_, 0); glu_chunk(w_, 1)
                            elif q == 1:
                                glu_chunk(w_, 2); glu_chunk(w_, 3)
                            elif q == 2:
                                prev = None
                                for qi in range(4):
                                    st_ = qk_chunk(w_, qi)
                                    if prev is not None:
                                        qk_finish(prev)
                                    prev = st_
                                qk_finish(prev)
                            else:
                                prev = None
                                for qi in range(4, 8):
                                    st_ = qk_chunk(w_, qi)
                                    if prev is not None:
                                        qk_finish(prev)
                                    prev = st_
                                v_tile(w_, 0)
                                qk_finish(prev)
                                for tl in range(1, 4):
                                    v_tile(w_, tl)
                            if nxt:
                                trans(w_ + 1, q)
                    P.barrier()
                    P.emit()
                    if stop == "p1":
                        return nc

                with ExitStack() as p2:
                    tA_i = sb("tA_i", [128, 4096], I32, p2)
                    Tdec = sb("Tdec", [128, 2432], BF16, p2)
                    diag = [sb("diag%d" % i, [128, 31, 128], BF16, p2) for i in range(2)]
                    catT = sb("catT", [128, 8, 512], BF16, p2)
                    cvv = sb("cvv", [128, 4, 512], F32, p2)
                    cst = sb("cst", [128, 2, 512], F32, p2)
                    x1t = sb("x1t", [128, D], F32, p2)
                    h2T = sb("h2T", [128, 8, 128], BF16, p2)
                    S2 = sb("S2", [128, D], F32, p2)
                    H2 = sb("H2", [128, D], F32, p2)
                    dma("sp", H2[:], MOD_d[b, 3], w=["H2"])
                    dma("sp", S2[:], MOD_d[b, 4], w=["S2"])
                    pool(lambda g: g.iota(tA_i[:], pattern=[[1, 4096]], base=-2048, channel_multiplier=-1), w=["tA"])
                    dve(lambda v: v.scalar_tensor_tensor(out=tA_i[:], in0=tA_i[:], scalar=-1.0, in1=tA_i[:], op0=ALU.mult, op1=ALU.max),
                        r=["tA"], w=["tA"])
                    QKR = [("qk", a_, h_) for a_ in (True, False) for h_ in range(4)]
                    VR = [("vS", t_) for t_ in range(16)]
                    nd = 0

                    def build_diag(i, slot):
                        dg_ = diag[slot]
                        dve(lambda g, i=i, dg_=dg_: g.tensor_tensor(out=dg_[:], in0=_ap(ident[:], [[128, 128], [0, 31], [1, 128]]),
                                                                    in1=_ap(cwb[:], [[124, 128], [1, 31], [0, 128]], off=i * 31), op=ALU.mult),
                            r=["ident", "cwb"], w=[("diag", slot)])

                    build_diag(0, 0)
                    pending = []
                    stats_pending = []
                    for w_ in range(4):
                        tok0 = w_ * 512
                        for i in range(4):
                            dg = diag[nd % 2]
                            dgr = ("diag", nd % 2)
                            nd += 1
                            if not (w_ == 3 and i == 3):
                                build_diag((i + 1) % 4, nd % 2)
                            bank = i % 2
                            for j in range(31):
                                pe(lambda tt, i=i, j=j, bank=bank, tok0=tok0, dg=dg: tt.matmul(
                                    ps[bank][:], lhsT=dg[:, j, :], rhs=hbuf[:, i, tok0 + j:tok0 + j + 512],
                                    start=(j == 0), stop=(j == 30)), r=[dgr, ("hbuf", i)], w=[PSR(bank)])
                            while stats_pending:
                                stats_pending.pop(0)()
                            act(lambda a, i=i, bank=bank: a.activation(out=cvv[:, i, :], in_=ps[bank][:], func=AF.Identity,
                                                                        bias=cb[:, i:i + 1]), r=[PSR(bank), "cb"], w=[("cvv", i)])
                            dve(lambda v, i=i: v.tensor_copy(out=sig[i % 2][:], in_=cvv[:, i, :]),
                                r=[("cvv", i)], w=[("sig", i % 2)])
                            act(lambda a, i=i: a.activation(out=sq[i % 2][:], in_=cvv[:, i, :], func=AF.Square),
                                r=[("cvv", i)], w=[("sq", i % 2)])
                            def _stats(i=i):
                                pe(lambda tt, i=i: tt.matmul(ps[2][:], lhsT=om512[:], rhs=sig[i % 2][:],
                                                             start=(i == 0), stop=(i == 3)), r=["om512", ("sig", i % 2)], w=[PSR(2)])
                                pe(lambda tt, i=i: tt.matmul(ps[3][:], lhsT=om512[:], rhs=sq[i % 2][:], start=(i == 0), stop=(i == 3)),
                                   r=["om512", ("sq", i % 2)], w=[PSR(3)])
                            stats_pending.append(_stats)
                        while stats_pending:
                            stats_pending.pop(0)()
                        act(lambda a: a.activation(out=cst[:, 0, :], in_=ps[2][:], func=AF.Copy), r=[PSR(2)], w=[("cst", 0)])
                        dve(lambda v: v.tensor_tensor(out=rs[0][:], in0=cst[:, 0, :], in1=cst[:, 0, :], op=ALU.mult),
                            r=[("cst", 0)], w=[("rs", 0)])
                        dve(lambda v: v.tensor_tensor(out=cst[:, 1, :], in0=ps[3][:], in1=rs[0][:], op=ALU.subtract),
                            r=[PSR(3), ("rs", 0)], w=[("cst", 1)])
                        act(lambda a: a.activation(out=cst[:, 1, :], in_=cst[:, 1, :], func=AF.Ln, bias=eps_c[:, 0:1]),
                            r=[("cst", 1), "eps_c"], w=[("cst", 1)])
                        act(lambda a: a.activation(out=cst[:, 1, :], in_=cst[:, 1, :], func=AF.Exp, scale=-0.5),
                            r=[("cst", 1)], w=[("cst", 1)])
                        for i in range(4):
                            dve(lambda v, i=i: v.tensor_tensor(out=cvv[:, i, :], in0=cvv[:, i, :], in1=cst[:, 0, :], op=ALU.subtract),
                                r=[("cvv", i), ("cst", 0)], w=[("cvv", i)])
                            dve(lambda v, i=i: v.tensor_tensor(out=cvv[:, i, :], in0=cvv[:, i, :], in1=cst[:, 1, :], op=ALU.mult),
                                r=[("cvv", i), ("cst", 1)], w=[("cvv", i)])
                            act(lambda a, i=i: a.activation(out=catT[:, i, :], in_=cvv[:, i, :], func=AF.Silu,
                                                            bias=clb[:, i:i + 1], scale=clg[:, i:i + 1]),
                                r=[("cvv", i), "clb", "clg"], w=[("catT", i)])
                        lo = tok0 + 128
                        items = [(hh, c, kt) for hh in range(4) for kt in range(16) for c in range(2)]

                        def stage1(g):
                            hh, c, kt = items[g]
                            if c == 0 and kt == 0:
                                act(lambda a, hh=hh, lo=lo: a.activation(out=Tdec[:], in_=tA_i[:, lo:lo + 2432], func=AF.Exp,
                                                                         scale=-slopes[hh]), r=["tA"], w=["Tdec"])
                                pool(lambda gp, hh=hh, tok0=tok0: gp.tensor_copy(out=qz[hh % 2][0][0:64, :], in_=qT[0:64, hh, tok0:tok0 + 512]),
                                     r=QKR, w=[("qz", hh % 2)])
                                pool(lambda gp, hh=hh, tok0=tok0: gp.tensor_copy(out=qz[hh % 2][1][64:128, :], in_=qT[64:128, hh, tok0:tok0 + 512]),
                                     r=QKR + [("qz", hh % 2)], w=[("qz", hh % 2)])
                            sbk = 3 + g % 3
                            pe(lambda tt, hh=hh, c=c, kt=kt, sbk=sbk: tt.matmul(
                                ps[sbk][:], lhsT=kT[:, hh, kt * 128:(kt + 1) * 128],
                                rhs=qz[hh % 2][c][:], start=True, stop=True),
                               r=QKR + [("qz", hh % 2)], w=[PSR(sbk)])
                            act(lambda a, sbk=sbk, g=g: a.activation(out=Eb[g % 3][:], in_=ps[sbk][:], func=AF.Exp),
                                r=[PSR(sbk)], w=[("Eb", g % 3)])
                            i0 = tok0 - kt * 128 + 2048 - lo
                            dve(lambda v, g=g, i0=i0: v.tensor_tensor(out=Pt[g % 5][:], in0=Eb[g % 3][:],
                                                                      in1=Tdec[:, i0:i0 + 512], op=ALU.mult),
                                r=[("Eb", g % 3), "Tdec"], w=[("Pt", g % 5)])

                        def stage2(g):
                            hh, c, kt = items[g]
                            bankA, bankB = (6, 7) if c == 0 else (0, 1)
                            pe(lambda tt, kt=kt, bankA=bankA, hh=hh, g=g: tt.matmul(
                                ps[bankA][:], lhsT=vS[:, kt, hh, 0:128], rhs=Pt[g % 5][:], start=(kt == 0), stop=(kt == 15)),
                               r=[("Pt", g % 5)] + VR, w=[PSR(bankA)])
                            pe(lambda tt, kt=kt, bankB=bankB, g=g: tt.matmul(
                                ps[bankB][:], lhsT=ones_bf[:, 0:128], rhs=Pt[g % 5][:], start=(kt == 0), stop=(kt == 15)),
                               r=[("Pt", g % 5), "ones_bf"], w=[PSR(bankB)])
                            if kt != 15:
                                return
                            act(lambda a, bankB=bankB, c=c: a.activation(out=rs[c][:], in_=ps[bankB][:], func=AF.Ln), r=[PSR(bankB)], w=[("rs", c)])
                            act(lambda a, c=c: a.activation(out=rs[c][:], in_=rs[c][:], func=AF.Exp, scale=-1.0), r=[("rs", c)], w=[("rs", c)])
                            dve(lambda v, bankA=bankA, c=c: v.tensor_tensor(out=oT[c][:], in0=ps[bankA][:], in1=rs[c][:], op=ALU.mult),
                                r=[PSR(bankA), ("rs", c)], w=[("oT", c)])
                            if c == 1:
                                dve(lambda v: v.scalar_tensor_tensor(out=oT[1][:], in0=oT[1][:], scalar=neglam[:, 0:1], in1=oT[0][:],
                                                                     op0=ALU.mult, op1=ALU.add),
                                    r=[("oT", 0), ("oT", 1), "neglam"], w=[("oT", 1)])
                                dve(lambda v: v.tensor_tensor(out=sq[0][:], in0=oT[1][:], in1=oT[1][:], op=ALU.mult),
                                    r=[("oT", 1)], w=[("sq", 0)])

                                def _fin(hh=hh):
                                    pe(lambda tt: tt.matmul(ps[2][:], lhsT=om128[:], rhs=sq[0][:], start=True, stop=True),
                                       r=["om128", ("sq", 0)], w=[PSR(2)])
                                    act(lambda a: a.activation(out=rs[0][:], in_=ps[2][:], func=AF.Ln, bias=eps_c[:, 0:1]),
                                        r=[PSR(2), "eps_c"], w=[("rs", 0)])
                                    act(lambda a: a.activation(out=rs[0][:], in_=rs[0][:], func=AF.Exp, scale=-0.5),
                                        r=[("rs", 0)], w=[("rs", 0)])
                                    dve(lambda v, hh=hh: v.scalar_tensor_tensor(out=catT[:, 4 + hh, :], in0=oT[1][:], scalar=subgc[:, 0:1],
                                                                                in1=rs[0][:], op0=ALU.mult, op1=ALU.mult),
                                        r=[("oT", 1), ("rs", 0), "subgc"], w=[("catT", 4 + hh)])
                                pending.append(_fin)

                        DEPTH = 4
                        for g in range(len(items)):
                            stage1(g)
                            if g >= DEPTH:
                                stage2(g - DEPTH)
                            if items[g][2] == 9 and items[g][1] == 1 and pending:
                                pending.pop(0)()
                        for g in range(len(items) - DEPTH, len(items)):
                            stage2(g)
                        while pending:
                            pending.pop(0)()
                        CATR = [("catT", i) for i in range(8)]

                        def stA(u):
                            t = b * 16 + w_ * 4 + u
                            sl = t % 2
                            zero_fill(-(-ZF_TOTAL // (2 * nseq * 16)))
                            dma("sp", xs[sl][:], x_tiles[t], w=[("xs", sl)])
                            for half in range(2):
                                bank = 4 + half
                                for kc in range(8):
                                    pe(lambda tt, kc=kc, u=u, half=half, bank=bank: tt.matmul(
                                        ps[bank][:], lhsT=catT[:, kc, u * 128:(u + 1) * 128],
                                        rhs=W_out[:, kc, half * 512:(half + 1) * 512], start=(kc == 0), stop=(kc == 7)),
                                       r=CATR + ["W_out"], w=[PSR(bank)])
                                dve(lambda v, half=half, bank=bank: v.tensor_tensor(
                                    out=tmp1[:, half * 512:(half + 1) * 512], in0=ps[bank][:], in1=G1[:, half * 512:(half + 1) * 512],
                                    op=ALU.mult), r=[PSR(bank), "G1"], w=[("tmp1h", half)])

                        def stB(u):
                            t = b * 16 + w_ * 4 + u
                            sl = t % 2
                            dve(lambda g, sl=sl: g.tensor_tensor(out=x1t[:], in0=tmp1[:], in1=xs[sl][:], op=ALU.add),
                                r=[("tmp1h", 0), ("tmp1h", 1), ("xs", sl)], w=["x1t", "tmp1"])
                            dma("sp", X1_tiles[t], x1t[:], r=["x1t"], w=[("X1", t)])
                            if debug:
                                dma("sp", dbg["x1"].ap().rearrange("(t p) d -> t p d", p=128)[t], x1t[:],
                                    r=["x1t"], w=[("dbgx1", t)])
                            act(lambda a, sl=sl: a.activation(out=htok[sl][:], in_=x1t[:], func=AF.Square, scale=D ** -0.5, accum_out=ss[:, 2:3]),
                                r=["x1t"], w=[("htok", sl), ("ss", 2)])
                            act(lambda a: a.activation(out=ss[:, 3:4], in_=ss[:, 2:3], func=AF.Ln, bias=eps_c[:, 0:1]),
                                r=[("ss", 2), "eps_c"], w=[("ss", 3)])
                            act(lambda a: a.activation(out=ss[:, 3:4], in_=ss[:, 3:4], func=AF.Exp, scale=-0.5),
                                r=[("ss", 3)], w=[("ss", 3)])
                            dve(lambda v: v.scalar_tensor_tensor(out=tmp1[:], in0=x1t[:], scalar=ss[:, 3:4], in1=S2[:],
                                                                 op0=ALU.mult, op1=ALU.mult),
                                r=["x1t", ("ss", 3), "S2"], w=["tmp1", ("tmp1h", 0), ("tmp1h", 1)])
                            dve(lambda g, sl=sl: g.tensor_tensor(out=htok[sl][:], in0=tmp1[:], in1=H2[:], op=ALU.add),
                                r=["tmp1", ("tmp1h", 0), ("tmp1h", 1), "H2"], w=[("htok", sl)])
                            dma("sp", H2_tiles[t], htok[sl][:], r=[("htok", sl)], w=[("H2d", t)])

                        def stC(u):
                            t = b * 16 + w_ * 4 + u
                            sl = t % 2
                            for kc in range(8):
                                pe(lambda tt, kc=kc, sl=sl: tt.transpose(psb(2)[:, kc * 128:(kc + 1) * 128],
                                                                         htok[sl][:, kc * 128:(kc + 1) * 128], ident[:]),
                                   r=[("htok", sl), "ident"], w=[PSR(2)])
                            act(lambda a: a.activation(out=h2T[:], in_=psb(2).rearrange("p (k t) -> p k t", k=8), func=AF.Copy),
                                r=[PSR(2)], w=["h2T"])
                            for kc in range(8):
                                pe(lambda tt, kc=kc: tt.matmul(ps[3][:, 0:E], lhsT=h2T[:, kc, :], rhs=W_r[:, kc, :],
                                                               start=(kc == 0), stop=(kc == 7)), r=["h2T", "W_r"], w=[PSR(3)])
                            dve(lambda v, t=t: v.tensor_tensor(out=lg_all[:, t, :], in0=ps[3][:, 0:E], in1=brb[:], op=ALU.add),
                                r=[PSR(3), "brb"], w=[("lg", t)])
                            dve(lambda v, t=t: v.max(out=mx8[:, t, :], in_=lg_all[:, t, :]), r=[("lg", t)], w=[("mx8", t)])
                            dve(lambda v, t=t: v.tensor_scalar(out=Mb[:, t * E:(t + 1) * E], in0=lg_all[:, t, :], scalar1=mx8[:, t, 3:4],
                                                               scalar2=None, op0=ALU.is_ge), r=[("lg", t), ("mx8", t)], w=[("Mb", t)])
                            dve(lambda v, t=t: v.tensor_scalar(out=nmx[:, t:t + 1], in0=mx8[:, t, 0:1], scalar1=-1.0, scalar2=None,
                                                               op0=ALU.mult), r=[("mx8", t)], w=[("nmx", t)])
                            act(lambda a, t=t: a.activation(out=g4[:, t, :], in_=mx8[:, t, 0:4], func=AF.Exp, bias=nmx[:, t:t + 1],
                                                            accum_out=gsm[:, t:t + 1]), r=[("mx8", t), ("nmx", t)], w=[("g4", t), ("gsm", t)])
                            dve(lambda v, t=t: v.reciprocal(out=gsm[:, t:t + 1], in_=gsm[:, t:t + 1]), r=[("gsm", t)], w=[("gsm", t)])
                            dve(lambda v, t=t: v.tensor_scalar(out=g4[:, t, :], in0=g4[:, t, :], scalar1=gsm[:, t:t + 1], scalar2=None,
                                                               op0=ALU.mult), r=[("g4", t), ("gsm", t)], w=[("g4", t)])

                        stA(0)
                        stB(0)
                        for u in range(1, 4):
                            stA(u)
                            stC(u - 1)
                            stB(u)
                        stC(3)
                    P.barrier()
                    P.emit()
                    if stop == "p2":
                        return nc
                seq_scope.__exit__(None, None, None)

        with ExitStack() as pb:
            sloti = sb("sloti", [128, NTL * 4], I32, pb)
            bei = sb("bei", [128, nstep], I32, pb)
            widx = sb("widx", [128, nstep * 8], I32, pb)
            bsel = sb("bsel", [128, 16, nstep], F32, pb)
            ohT = sb("ohT", [128, nstep], F32, pb)
            bgu = sb("bgu", [E, 2 * D], BF16, pb)
            bdn = sb("bdn", [128, D], BF16, pb)
            rt_scope = ExitStack()
            rt_scope.__enter__()
            Lst = sb("Lst", [128, 128], BF16, rt_scope)
            onesq = sb("onesq", [128, 128], BF16, rt_scope)
            pre = sb("pre", [128, NTL, E], F32, rt_scope)
            csb = sb("csb", [128, NTL, E], F32, rt_scope)
            off = sb("off", [128, NTL, E], F32, rt_scope)
            cnt = sb("cnt", [128, E], F32, rt_scope)
            pad = sb("pad", [128, E], F32, rt_scope)
            pst = sb("pst", [128, E], F32, rt_scope)
            pend = sb("pend", [128, E], F32, rt_scope)
            big = sb("big", [128, NTL, 4, E], F32, rt_scope)
            slotf = sb("slotf", [128, NTL, 4], F32, rt_scope)
            bthr = sb("bthr", [128, nstep], F32, rt_scope)
            bthr_i = sb("bthr_i", [128, nstep], I32, rt_scope)
            cmpb = sb("cmpb", [128, nstep, E], F32, rt_scope)
            bef = sb("bef", [128, nstep], F32, rt_scope)
            bgu_f = sb("bgu_f", [E, 2 * D], F32, rt_scope)
            bd_f = sb("bd_f", [E, D], F32, rt_scope)
            cmp8 = sb("cmp8", [128, E, 16], F32, rt_scope)
            kcp_i = sb("kcp_i", [128, 8], I32, rt_scope)
            kcp = sb("kcp", [128, 8], F32, rt_scope)
            widx_f = sb("widx_f", [128, nstep, 8], F32, rt_scope)
            bef1k = sb("bef1k", [128, nstep], F32, rt_scope)
            inact = sb("inact", [128, nstep], F32, rt_scope)
            ohTb = sb("ohTb", [E, nstep], BF16, rt_scope)

            LGR = [("lg", t) for t in range(NTL)]
            dve(lambda v: v.tensor_scalar(out=Lst[:], in0=iota_f[:], scalar1=iota_p[:, 0:1], scalar2=None, op0=ALU.is_gt),
                r=["iota_f", "iota_p"], w=["Lst"])
            dve(lambda v: v.memset(onesq[:], 1.0), w=["onesq"])
            pool(lambda g: g.iota(bthr_i[:], pattern=[[RB, nstep]], base=0, channel_multiplier=0), w=["bthr_i"])
            dve(lambda v: v.tensor_copy(out=bthr[:], in_=bthr_i[:]), r=["bthr_i"], w=["bthr"])
            dma("sp", bgu_f[:], bgu_d.ap(), w=["bgu_f"])
            dma("sp", bd_f[:], bd_d.ap(), w=["bd_f"])
            act(lambda a: a.activation(out=bgu[:], in_=bgu_f[:], func=AF.Copy), r=["bgu_f"], w=["bgu"])
            dve(lambda v: v.memset(bdn[:], 0.0), w=["bdn"])
            act(lambda a: a.activation(out=bdn[0:E, :], in_=bd_f[:], func=AF.Copy), r=["bd_f", "bdn"], w=["bdn"])
            MXR = []
            G4R = []
            nh = (NTL * E) // 512
            for hf in range(nh):
                pe(lambda tt, hf=hf: tt.matmul(ps[hf][:], lhsT=Lst[:], rhs=Mb[:, hf * 512:(hf + 1) * 512], start=True, stop=True),
                   r=["Lst"], w=[PSR(hf)])
                pe(lambda tt, hf=hf: tt.matmul(ps[2 + hf][:], lhsT=onesq[:], rhs=Mb[:, hf * 512:(hf + 1) * 512], start=True, stop=True),
                   r=["onesq"], w=[PSR(2 + hf)])
                act(lambda a, hf=hf: a.activation(out=pre[:].rearrange("p t e -> p (t e)")[:, hf * 512:(hf + 1) * 512],
                                                  in_=ps[hf][:], func=AF.Copy), r=[PSR(hf)], w=[("pre", hf)])
                act(lambda a, hf=hf: a.activation(out=csb[:].rearrange("p t e -> p (t e)")[:, hf * 512:(hf + 1) * 512],
                                                  in_=ps[2 + hf][:], func=AF.Copy), r=[PSR(2 + hf)], w=[("csb", hf)])
            PRER = [("pre", hf) for hf in range(nh)]
            CSR = [("csb", hf) for hf in range(nh)]
            dve(lambda v: v.memset(off[:, 0, :], 0.0), w=["off"])
            for t in range(1, NTL):
                dve(lambda v, t=t: v.tensor_tensor(out=off[:, t, :], in0=off[:, t - 1, :], in1=csb[:, t - 1, :], op=ALU.add),
                    r=["off"] + CSR, w=["off"])
            dve(lambda v: v.tensor_tensor(out=cnt[:], in0=off[:, NTL - 1, :], in1=csb[:, NTL - 1, :], op=ALU.add),
                r=["off"] + CSR, w=["cnt"])
            nmx_b = NTOK // RB + 1
            assert nmx_b <= 16
            dve(lambda v: v.tensor_tensor(out=cmp8[:, :, 0:nmx_b], in0=_ap(cnt[:], [[E, 128], [1, E], [0, nmx_b]]),
                                          in1=_ap(bthr[:], [[nstep, 128], [0, E], [1, nmx_b]]), op=ALU.is_gt),
                r=["cnt", "bthr"], w=["cmp8"])
            dve(lambda v: v.tensor_reduce(out=pad[:], in_=cmp8[:, :, 0:nmx_b], axis=AX.X, op=ALU.add), r=["cmp8"], w=["pad"])
            dve(lambda v: v.tensor_scalar(out=pad[:], in0=pad[:], scalar1=float(RB), scalar2=None, op0=ALU.mult),
                r=["pad"], w=["pad"])
            dve(lambda v: v.memset(pst[:, 0:1], 0.0), w=["pst"])
            for e in range(1, E):
                dve(lambda v, e=e: v.tensor_tensor(out=pst[:, e:e + 1], in0=pst[:, e - 1:e], in1=pad[:, e - 1:e], op=ALU.add),
                    r=["pst", "pad"], w=["pst"])
            dve(lambda v: v.tensor_tensor(out=pend[:], in0=pst[:], in1=pad[:], op=ALU.add), r=["pst", "pad"], w=["pend"])
            dve(lambda v: v.tensor_tensor(out=pre[:], in0=pre[:], in1=off[:], op=ALU.add), r=PRER + ["off"], w=["dest"])
            dve(lambda v: v.tensor_tensor(out=pre[:], in0=pre[:], in1=_ap(pst[:], [[E, 128], [0, NTL], [1, E]]), op=ALU.add),
                r=["dest", "pst"], w=["dest"])
            dve(lambda v: v.tensor_tensor(out=big[:], in0=_ap(lg_all[:], [[NTL * E, 128], [E, NTL], [0, 4], [1, E]]),
                                          in1=_ap(mx8[:], [[NTL * 8, 128], [8, NTL], [1, 4], [0, E]]), op=ALU.is_equal),
                r=LGR + MXR, w=["big"])
            dve(lambda v: v.tensor_tensor(out=big[:], in0=big[:], in1=_ap(pre[:], [[NTL * E, 128], [E, NTL], [0, 4], [1, E]]),
                                          op=ALU.mult), r=["big", "dest"], w=["big"])
            dve(lambda v: v.tensor_reduce(out=slotf[:], in_=big[:], axis=AX.X, op=ALU.add), r=["big"], w=["slotf"])
            dve(lambda v: v.tensor_copy(out=sloti[:], in_=slotf[:].rearrange("p t k -> p (t k)")), r=["slotf"], w=["sloti"])
            dve(lambda v: v.tensor_tensor(out=cmpb[:], in0=_ap(pend[:], [[E, 128], [0, nstep], [1, E]]),
                                          in1=_ap(bthr[:], [[nstep, 128], [1, nstep], [0, E]]), op=ALU.is_le),
                r=["pend", "bthr"], w=["cmpb"])
            dve(lambda v: v.tensor_reduce(out=bef[:], in_=cmpb[:], axis=AX.X, op=ALU.add), r=["cmpb"], w=["bef"])
            dve(lambda v: v.tensor_scalar(out=bef[:], in0=bef[:], scalar1=float(E - 1), scalar2=None, op0=ALU.min),
                r=["bef"], w=["bef"])
            dve(lambda v: v.tensor_copy(out=bei[:], in_=bef[:]), r=["bef"], w=["bei"])
            dve(lambda v: v.tensor_scalar(out=ohT[:], in0=bef[:, :], scalar1=iota_p[:, 0:1], scalar2=None, op0=ALU.is_equal),
                r=["bef", "iota_p"], w=["ohT"])
            pool(lambda g: g.iota(kcp_i[:], pattern=[[128, 8]], base=0, channel_multiplier=1), w=["kcp_i"])
            dve(lambda v: v.tensor_copy(out=kcp[:], in_=kcp_i[:]), r=["kcp_i"], w=["kcp"])
            dve(lambda v: v.tensor_scalar(out=bef1k[:], in0=bef[:], scalar1=float(D), scalar2=None, op0=ALU.mult), r=["bef"], w=["bef1k"])
            dve(lambda v: v.tensor_scalar(out=inact[:], in0=bthr[:], scalar1=pend[:, E - 1:E], scalar2=None, op0=ALU.is_ge),
                r=["bthr", "pend"], w=["inact"])
            dve(lambda v: v.scalar_tensor_tensor(out=bef1k[:], in0=inact[:], scalar=1.0e6, in1=bef1k[:], op0=ALU.mult, op1=ALU.add),
                r=["inact", "bef1k"], w=["bef1k"])
            dve(lambda v: v.tensor_copy(out=ohTb[:], in_=ohT[0:E, :]), r=["ohT"], w=["ohTb"])
            for c16 in range(16):
                bk = 4 + c16 // 8
                pe(lambda tt, c16=c16, bk=bk: tt.matmul(ps[bk][:, (c16 % 8) * nstep:(c16 % 8 + 1) * nstep],
                                                        lhsT=bgu[:, c16 * 128:(c16 + 1) * 128], rhs=ohTb[:], start=True, stop=True),
                   r=["bgu", "ohTb"], w=[PSR(bk)])
            act(lambda a: a.activation(out=bsel[:, 0:8, :], in_=ps[4][:, 0:8 * nstep].rearrange("p (c b) -> p c b", c=8), func=AF.Copy),
                r=[PSR(4)], w=["bsel"])
            dve(lambda v: v.tensor_scalar(out=bsel[:, 8:16, :], in0=ps[5][:, 0:8 * nstep].rearrange("p (c b) -> p c b", c=8),
                                          scalar1=1.0, scalar2=None, op0=ALU.add), r=[PSR(5), "bsel"], w=["bsel"])
            dve(lambda v: v.tensor_tensor(out=widx_f[:], in0=_ap(bef1k[:], [[nstep, 128], [1, nstep], [0, 8]]),
                                          in1=_ap(kcp[:], [[8, 128], [0, nstep], [1, 8]]), op=ALU.add),
                r=["bef1k", "kcp"], w=["widx_f"])
            dve(lambda v: v.tensor_copy(out=widx[:], in_=widx_f[:].rearrange("p b c -> p (b c)")), r=["widx_f"], w=["widx"])
            if debug:
                dma("sp", dbg["lg"].ap(), lg_all[:], r=LGR, w=["dbg_lg"])
                dma("sp", dbg["slot"].ap(), slotf[:], r=["slotf"], w=["dbg_slot"])
                dma("sp", dbg["g4"].ap(), g4[:], r=G4R, w=["dbg_g4"])
                dma("sp", dbg["be"].ap(), bef[:], r=["bef"], w=["dbg_be"])

            P.barrier()
            P.emit()
            rt_scope.__exit__(None, None, None)
            G4R = [("g4", t) for t in range(NTL)]
            st_scope = ExitStack()
            st_scope.__enter__()
            if do_moe:
                bcreg = nc.gpsimd.alloc_register("bcreg")
                ohb = [sb("ohb%d" % i, [128, 128], BF16, st_scope) for i in range(2)]
                Wgu = [sb("Wgu%d" % i, [128, 8, 2 * D], BF16, st_scope) for i in range(2)]
                Wdn = [sb("Wdn%d" % i, [128, 8, D], BF16, st_scope) for i in range(2)]
                xr = [sb("xr%d" % i, [128, D], BF16, st_scope) for i in range(8)]
                xT = [sb("xT%d" % i, [128, 8, 512], BF16, st_scope) for i in range(2)]
                actT = sb("actT", [128, 8, 512], BF16, st_scope)
                gm = [sb("gm%d" % i, [128, 512], F32, st_scope) for i in range(2)]
                sg = [sb("sg%d" % i, [128, 512], F32, st_scope) for i in range(2)]
                uc = [sb("uc%d" % i, [128, 512], F32, st_scope) for i in range(2)]
                ost = [sb("ost%d" % i, [128, D], BF16, st_scope) for i in range(2)]
            if do_moe:
                H2_tiles = H2_d.ap().rearrange("(t p) d -> t p d", p=128)
                X1_tiles = X1_d.ap().rearrange("(t p) d -> t p d", p=128)
                out_tiles = out_d.ap().rearrange("(t p) d -> t p d", p=128)
                wgu2d = wgu_d.ap().rearrange("e k n -> (e k) n")
                wd2d = wd_d.ap().rearrange("e k n -> (e k) n")
                wreg = nc.gpsimd.alloc_register("wreg")

                def load_weights(bstep):
                    par = bstep % 2
                    for kc in range(8):
                        def fn(g, kc=kc):
                            if bstep == 0 and kc == 0:
                                g.reg_mov(wreg, E * D - 1)
                            return g.indirect_dma_start(
                                out=Wgu[par][:, kc, :], out_offset=None, in_=wgu2d,
                                in_offset=bass.IndirectOffsetOnAxis(ap=widx[:, bstep * 8 + kc:bstep * 8 + kc + 1], axis=0),
                                bounds_check=wreg, oob_is_err=False)
                        P.op("pool", fn, r=["widx"], w=[("Wgu", par, kc)], dma=True)
                    for kc in range(8):
                        P.op("pool", lambda g, kc=kc: g.indirect_dma_start(
                            out=Wdn[par][:, kc, :], out_offset=None, in_=wd2d,
                            in_offset=bass.IndirectOffsetOnAxis(ap=widx[:, bstep * 8 + kc:bstep * 8 + kc + 1], axis=0),
                            bounds_check=wreg, oob_is_err=False), r=["widx"], w=[("Wdn", par, kc)], dma=True)

                load_weights(0)
                for t in range(NTL):
                    sl = t % 4
                    dma("sp", xr[sl][:], H2_tiles[t], w=[("xr", 0, sl)])
                    for k in range(4):
                        def _scat(g, sl=sl, t=t, k=k):
                            if t == 0 and k == 0:
                                g.reg_mov(bcreg, NSLOT - 1)
                            return g.indirect_dma_start(
                                out=XB_d[:, :], out_offset=bass.IndirectOffsetOnAxis(ap=sloti[:, t * 4 + k:t * 4 + k + 1], axis=0),
                                in_=xr[sl][:], in_offset=None, bounds_check=bcreg, oob_is_err=False)
                        P.op("pool", _scat,
                            r=[("xr", 0, sl), "sloti"], w=[("XBs", t, k)], dma=True)
                XBS = [("XBs", t, k) for t in range(NTL) for k in range(4)]
                P.op("sp", None, r=XBS, w=["XBjoin"])

                def emit_xload(bs):
                    for rt in range(4):
                        dma("sp", xr[(bs % 2) * 4 + rt][:], XB_d[bs * RB + rt * 128: bs * RB + (rt + 1) * 128, :],
                            r=["XBjoin"], w=[("xr", bs % 2, rt)])

                def emit_xT(bs):
                    xTs = xT[bs % 2]
                    for rt in range(4):
                        bank = rt % 2
                        src_t = xr[(bs % 2) * 4 + rt]
                        for kc in range(8):
                            pe(lambda tt, kc=kc, src_t=src_t, bank=bank: tt.transpose(psb(bank)[:, kc * 128:(kc + 1) * 128],
                                                                                     src_t[:, kc * 128:(kc + 1) * 128], ident[:]),
                               r=[("xr", bs % 2, rt), "ident"], w=[PSR(bank)])
                        act(lambda a, bank=bank, rt=rt, xTs=xTs: a.activation(
                            out=xTs[:, :, rt * 128:(rt + 1) * 128], in_=psb(bank).rearrange("p (k t) -> p k t", k=8), func=AF.Copy),
                            r=[PSR(bank)], w=[("xT", bs % 2)])

                emit_xload(0)
                emit_xT(0)
                if nstep > 1:
                    emit_xload(1)
                for bstep in range(nstep):
                    par = bstep % 2
                    if bstep + 1 < nstep:
                        load_weights(bstep + 1)
                    WG = [("Wgu", par, q) for q in range(8)]
                    WD = [("Wdn", par, q) for q in range(8)]
                    xTs = xT[par]
                    dve(lambda v, bstep=bstep, par=par: v.tensor_copy(out=ohb[par][:], in_=_ap(ohT[:], [[nstep, 128], [0, 128]], off=bstep)),
                        r=["ohT"], w=[("ohb", par)])
                    for fc in range(8):
                        bg_, bu_ = 2 + 2 * (fc % 2), 3 + 2 * (fc % 2)
                        for (bank, col0) in ((bg_, fc * 128), (bu_, D + fc * 128)):
                            for kc in range(8):
                                pe(lambda tt, kc=kc, bank=bank, col0=col0, par=par, xTs=xTs: tt.matmul(
                                    ps[bank][:], lhsT=Wgu[par][:, kc, col0:col0 + 128], rhs=xTs[:, kc, :],
                                    start=(kc == 0), stop=(kc == 7)), r=WG + [("xT", par)], w=[PSR(bank)])
                        s2 = fc % 2
                        dve(lambda v, bg_=bg_, s2=s2, fc=fc, bstep=bstep: v.tensor_scalar(
                            out=gm[s2][:], in0=ps[bg_][:], scalar1=bsel[:, fc, bstep:bstep + 1], scalar2=7.0, op0=ALU.add, op1=ALU.min),
                            r=[PSR(bg_), "bsel"], w=[("gm", s2)])
                        act(lambda a, s2=s2: a.activation(out=sg[s2][:], in_=gm[s2][:], func=AF.Sigmoid, scale=1.702),
                            r=[("gm", s2)], w=[("sg", s2)])
                        dve(lambda v, bu_=bu_, s2=s2, fc=fc, bstep=bstep: v.tensor_scalar(
                            out=uc[s2][:], in0=ps[bu_][:], scalar1=bsel[:, 8 + fc, bstep:bstep + 1], scalar2=8.0, op0=ALU.add, op1=ALU.min),
                            r=[PSR(bu_), "bsel"], w=[("uc", s2)])
                        dve(lambda v, s2=s2: v.tensor_tensor(out=gm[s2][:], in0=gm[s2][:], in1=sg[s2][:], op=ALU.mult),
                            r=[("gm", s2), ("sg", s2)], w=[("gm", s2)])
                        dve(lambda v, s2=s2, fc=fc: v.scalar_tensor_tensor(out=actT[:, fc, :], in0=uc[s2][:], scalar=-6.0, in1=gm[s2][:],
                                                                           op0=ALU.max, op1=ALU.mult),
                            r=[("uc", s2), ("gm", s2)], w=[("actT", fc)])
                    if bstep + 1 < nstep:
                        emit_xT(bstep + 1)
                    if bstep + 2 < nstep:
                        emit_xload(bstep + 2)
                    ACTR = [("actT", fc) for fc in range(8)]
                    for rt in range(4):
                        o_ = ost[rt % 2]
                        for half in range(2):
                            bank = 6 + half
                            for fc in range(8):
                                pe(lambda tt, fc=fc, rt=rt, half=half, bank=bank, par=par: tt.matmul(
                                    ps[bank][:], lhsT=actT[:, fc, rt * 128:(rt + 1) * 128],
                                    rhs=Wdn[par][:, fc, half * 512:(half + 1) * 512], start=(fc == 0), stop=False),
                                   r=ACTR + WD, w=[PSR(bank)])
                            pe(lambda tt, half=half, bank=bank, par=par: tt.matmul(
                                ps[bank][:], lhsT=ohb[par][:, 0:128], rhs=bdn[:, half * 512:(half + 1) * 512], start=False, stop=True),
                               r=["bdn", ("ohb", par)], w=[PSR(bank)])
                            if False:
                                pass
                            else:
                                dve(lambda v, half=half, bank=bank, o_=o_: v.tensor_copy(out=o_[:, half * 512:(half + 1) * 512], in_=ps[bank][:]),
                                    r=[PSR(bank)], w=[("ost", rt % 2, half)])
                        dma("sp", OB_d[bstep * RB + rt * 128: bstep * RB + (rt + 1) * 128, :], o_[:],
                            r=[("ost", rt % 2, 0), ("ost", rt % 2, 1)], w=[("OB", bstep, rt)])
                OBR = [("OB", bs_, rt) for bs_ in range(nstep) for rt in range(4)]
                P.op("sp", None, r=OBR, w=["OBjoin"])
                P.barrier()
                P.emit()
                st_scope.__exit__(None, None, None)
                st_scope = ExitStack()
                st_scope.__enter__()
                NG = 3
                gr = [[sb("gr%d_%d" % (j_, i), [128, D], BF16, st_scope) for i in range(4)] for j_ in range(NG)]
                acc = [sb("acc%d" % i, [128, D], F32, st_scope) for i in range(2)]
                x1r = [sb("x1r%d" % i, [128, D], F32, st_scope) for i in range(3)]
                G2 = sb("G2", [128, nseq, D], F32, st_scope)
                for b_ in range(nseq):
                    dma("sp", G2[:, b_, :], MOD_d[b_, 5], w=[("G2", b_, 0), ("G2", b_, 1)])
                for t in range(NTL):
                    b = t // 16
                    sl = t % 3
                    gs = t % NG
                    ac = acc[t % 2]
                    acr = ("acc", t % 2)
                    dma("sp", x1r[sl][:], X1_tiles[t], w=[("x1r", sl)])
                    for k in range(4):
                        def _gath(g, t=t, k=k, gs=gs):
                            if t == 0 and k == 0:
                                g.reg_mov(bcreg, NSLOT - 1)
                            return g.indirect_dma_start(
                                out=gr[gs][k][:], out_offset=None, in_=OB_d[:, :],
                                in_offset=bass.IndirectOffsetOnAxis(ap=sloti[:, t * 4 + k:t * 4 + k + 1], axis=0),
                                bounds_check=bcreg, oob_is_err=False)
                        P.op("pool", _gath, r=["OBjoin", "sloti"], w=[("gr", gs, k)], dma=True)
                    act(lambda a, t=t, gs=gs, ac=ac: a.activation(out=ac[:], in_=gr[gs][0][:], func=AF.Copy, scale=g4[:, t, 0:1]),
                        r=[("gr", gs, 0)] + G4R, w=[acr])
                    for k in range(1, 4):
                        dve(lambda v, t=t, k=k, gs=gs, ac=ac: v.scalar_tensor_tensor(out=ac[:], in0=gr[gs][k][:], scalar=g4[:, t, k:k + 1], in1=ac[:],
                                                                                    op0=ALU.mult, op1=ALU.add), r=[("gr", gs, k), acr] + G4R, w=[acr])
                    dve(lambda g, b=b, ac=ac: g.tensor_tensor(out=ac[:], in0=ac[:], in1=G2[:, b, :], op=ALU.mult),
                        r=[acr, ("G2", b, 0), ("G2", b, 1)], w=[acr])
                    dve(lambda g, sl=sl, ac=ac: g.tensor_tensor(out=x1r[sl][:], in0=ac[:], in1=x1r[sl][:], op=ALU.add),
                        r=[acr, ("x1r", sl)], w=[("x1r", sl), acr])
                    dma("sp", out_tiles[t], x1r[sl][:], r=[("x1r", sl)], w=[("out", t)])
            else:
                x1r = [sb("x1r%d" % i, [128, D], F32, st_scope) for i in range(2)]
                X1_tiles = X1_d.ap().rearrange("(t p) d -> t p d", p=128)
                out_tiles = out_d.ap().rearrange("(t p) d -> t p d", p=128)
                for t in range(NTL):
                    sl = t % 2
                    dma("sp", x1r[sl][:], X1_tiles[t], w=[("x1r", sl)])
                    dma("sp", out_tiles[t], x1r[sl][:], r=[("x1r", sl)], w=[("out", t)])
            P.barrier()
            P.emit()
            st_scope.__exit__(None, None, None)
    return nc


def make_in_maps(inputs, ncores=NCORES, nseq=NSEQ):
    f = lambda a: np.ascontiguousarray(np.asarray(a, dtype=np.float32))
    x = f(inputs["x"])
    c = f(inputs["c"])
    shared = {
        "w_ada": f(inputs["w_ada"][0]),
        "b_ada": f(inputs["b_ada"][0]).reshape(1, -1),
        "norm1_g": f(inputs["norm1_g"][0]).reshape(1, -1),
        "norm2_g": f(inputs["norm2_g"][0]).reshape(1, -1),
        "w_in": f(inputs["w_in"][0]),
        "gq": f(np.tile(np.asarray(inputs["q_norm_g"][0]), 2).reshape(128, 1)),
        "gk": f(np.tile(np.asarray(inputs["k_norm_g"][0]), 2).reshape(128, 1)),
        "lamv": f(np.stack([inputs["lambda_q1"][0], inputs["lambda_q2"][0], inputs["lambda_k1"][0], inputs["lambda_k2"][0]])),
        "subln_g": f(inputs["subln_g"][0]).reshape(1, -1),
        "conv_wT": f(np.asarray(inputs["conv_w"][0]).reshape(31, 4, 128).transpose(2, 1, 0)),
        "conv_b": f(np.asarray(inputs["conv_b"][0]).reshape(4, 128).T),
        "conv_ln_g": f(np.asarray(inputs["conv_ln_g"][0]).reshape(4, 128).T),
        "conv_ln_b": f(np.asarray(inputs["conv_ln_b"][0]).reshape(4, 128).T),
        "w_out": f(inputs["w_out"][0]),
        "w_router": f(inputs["w_router"][0]),
        "b_router": f(inputs["b_router"][0]).reshape(1, -1),
        "w_gate_up": f(inputs["w_gate_up"][0]),
        "b_gate_up": f(inputs["b_gate_up"][0]),
        "w_down": f(inputs["w_down"][0]),
        "b_down": f(inputs["b_down"][0]),
    }
    maps = []
    for i in range(ncores):
        m = dict(shared)
        m["x"] = np.ascontiguousarray(x[i * nseq:(i + 1) * nseq].reshape(nseq * S, D))
        cc = c[i * nseq:(i + 1) * nseq]
        m["cT"] = np.ascontiguousarray(cc.reshape(nseq, 8, 128).transpose(2, 1, 0))
        maps.append(m)
    return maps


_NC_CACHE = {}


def kernel(**inputs):
    if "nc" not in _NC_CACHE:
        _NC_CACHE["nc"] = build_program()
    nc = _NC_CACHE["nc"]
    maps = make_in_maps(inputs)
    res = run_bass_kernel_spmd(nc, maps, core_ids=list(range(NCORES)))
    outs = [np.asarray(r["out"]).reshape(NSEQ, S, D) for r in res.results]
    return np.concatenate(outs, axis=0).astype(np.float32)
```

```python
import math
from contextlib import ExitStack
import numpy as np
import concourse.bass as bass
import concourse.mybir as mybir
from concourse.bass_utils import run_bass_kernel_spmd

F32 = mybir.dt.float32
BF16 = mybir.dt.bfloat16
I32 = mybir.dt.int32
AF = mybir.ActivationFunctionType
ALU = mybir.AluOpType
AX = mybir.AxisListType

NCORES = 8
D = 1024
S = 2048
NSEQ = 2
NT = NSEQ * S // 128
E = 32
RB = 512
NSTEP = (NT * 128 * 4) // RB + E
EPS = 1e-5
LAM_INIT = 0.8 - 0.6 * math.exp(0.0)
NDQ = 12


class _Op:
    __slots__ = ("eng", "fn", "dma", "deps", "milestone", "sem", "val", "know")


class Prog:
    ENGS = ("pe", "act", "dve", "pool", "sp")

    def __init__(self, nc, es):
        self.nc = nc
        self.ops = []
        self.emitted = 0
        self.last_w = {}
        self.readers = {}
        self.esem = {e: es.enter_context(nc.semaphore("tl_" + e)) for e in self.ENGS}
        self.dsem = {e: [es.enter_context(nc.semaphore("dq_%s%d" % (e, i))) for i in range(NDQ)]
                     for e in ("sp", "act", "pool")}
        self.ecount = {e: 0 for e in self.ENGS}
        self.dcount = {e: 0 for e in self.dsem}
        self.dhist = {e: [] for e in self.dsem}
        self.know = {e: {} for e in self.ENGS}
        self.live_dma = []
        self.last_real = {}

    def op(self, eng, fn, r=(), w=(), dma=False, extra=()):
        o = _Op()
        o.eng, o.fn, o.dma = eng, fn, dma
        deps = set(extra)
        for x in r:
            if x in self.last_w:
                deps.add(self.last_w[x])
        for x in w:
            if x in self.last_w:
                deps.add(self.last_w[x])
            for rd in self.readers.get(x, ()):
                deps.add(rd)
        idx = len(self.ops)
        o.deps = deps
        o.milestone = False
        o.know = None
        self.ops.append(o)
        for x in r:
            self.readers.setdefault(x, []).append(idx)
        for x in w:
            self.last_w[x] = idx
            self.readers[x] = []
        if dma:
            self.live_dma.append(idx)
        elif fn is not None:
            self.last_real[eng] = idx
        return idx

    def barrier(self):
        firsts = []
        for e in self.ENGS:
            ex = list(self.live_dma) if e == "sp" else []
            if e in self.last_real:
                ex.append(self.last_real[e])
            firsts.append(self.op(e, None, w=[("bar", e)], extra=ex))
        self.live_dma = []
        self.last_real = {}
        for e in self.ENGS:
            self.op(e, None, r=[("bar", x) for x in self.ENGS], w=[("bar2", e)])
        self.last_w = {k: v for k, v in self.last_w.items() if k[0] == "bar2"}
        self.readers = {}

    def emit(self):
        nc = self.nc
        ops = self.ops
        start = self.emitted
        for o in ops[start:]:
            for d in o.deps:
                ops[d].milestone = True
        plan = {e: [] for e in self.ENGS}
        for i in range(start, len(ops)):
            o = ops[i]
            e = o.eng
            know = self.know[e]
            waits = []
            if o.dma:
                j = self.dcount[e]
                self.dcount[e] += 1
                o.sem = self.dsem[e][j % NDQ]
                o.val = 16 * (j // NDQ + 1)
                if j >= NDQ:
                    o.deps.add(self.dhist[e][j - NDQ])
                self.dhist[e].append(i)
            for d in sorted(o.deps):
                p = ops[d]
                if (not p.dma) and p.eng == "pe" and e == "pe" and not o.dma and o.fn is not None:
                    continue
                assert p.sem is not None, "dependency on op that was never made a milestone"
                key = id(p.sem)
                if know.get(key, (None, 0))[1] >= p.val:
                    continue
                waits.append((p.sem, p.val))
                if p.know:
                    for k2, v2 in p.know.items():
                        if know.get(k2, (None, 0))[1] < v2[1]:
                            know[k2] = v2
                know[key] = (p.sem, p.val)
            if o.dma:
                o.know = dict(know)
            elif o.milestone or o.fn is None:
                self.ecount[e] += 1
                o.sem = self.esem[e]
                o.val = self.ecount[e]
                o.milestone = True
                snap = dict(know)
                snap[id(o.sem)] = (o.sem, o.val)
                o.know = snap
            else:
                o.sem = None
                o.val = 0
            plan[e].append((o, waits))
        self.emitted = len(ops)
        attr = {"pe": "tensor", "act": "scalar", "dve": "vector", "pool": "gpsimd", "sp": "sync"}

        def run(engname, eng):
            for o, waits in plan[engname]:
                best = {}
                for sem, val in waits:
                    k = id(sem)
                    if k not in best or best[k][1] < val:
                        best[k] = (sem, val)
                for sem, val in best.values():
                    eng.wait_ge(sem, val)
                if o.fn is None:
                    eng.sem_inc(o.sem, 1)
                    continue
                ins = o.fn(eng)
                if o.dma:
                    ins.then_inc(o.sem, 16)
                elif o.milestone:
                    ins.then_inc(o.sem, 1)

        with nc.Block() as block:
            for engname in self.ENGS:
                if not plan[engname]:
                    continue
                deco = getattr(block, attr[engname])

                def body(eng, engname=engname):
                    run(engname, eng)
                deco(body)


def _ap(base, dims, off=0):
    return bass.AP(tensor=base.tensor, offset=base.offset + off, ap=[list(d) for d in dims])


def build_program(debug=False, nseq=NSEQ, do_moe=True, stop=None):
    nc = bass.Bass("TRN2", target_bir_lowering=False)
    NTOK = nseq * S
    NTL = NTOK // 128
    nstep = (NTOK * 4) // RB + E
    NSLOT = nstep * RB

    def din(name, shape, dt=F32):
        return nc.dram_tensor(name, list(shape), dt, kind="ExternalInput")

    x_d = din("x", [NTOK, D])
    cT_d = din("cT", [128, 8, nseq])
    wada_d = din("w_ada", [D, 6 * D])
    bada_d = din("b_ada", [1, 6 * D])
    n1g_d = din("norm1_g", [1, D])
    n2g_d = din("norm2_g", [1, D])
    win_d = din("w_in", [D, 2560])
    gq_d = din("gq", [128, 1])
    gk_d = din("gk", [128, 1])
    lamv_d = din("lamv", [4, 64])
    subg_d = din("subln_g", [1, 128])
    cw_d = din("conv_wT", [128, 4, 31])
    cb_d = din("conv_b", [128, 4])
    clg_d = din("conv_ln_g", [128, 4])
    clb_d = din("conv_ln_b", [128, 4])
    wout_d = din("w_out", [D, D])
    wr_d = din("w_router", [D, E])
    br_d = din("b_router", [1, E])
    wgu_d = din("w_gate_up", [E, D, 2 * D])
    bgu_d = din("b_gate_up", [E, 2 * D])
    wd_d = din("w_down", [E, D, D])
    bd_d = din("b_down", [E, D])
    out_d = nc.dram_tensor("out", [NTOK, D], F32, kind="ExternalOutput")
    X1_d = nc.dram_tensor("X1s", [NTOK, D], F32)
    H2_d = nc.dram_tensor("H2s", [NTOK, D], BF16)
    XB_d = nc.dram_tensor("XBs", [NSLOT, D], BF16)
    OB_d = nc.dram_tensor("OBs", [NSLOT, D], BF16)
    dbg = {}
    if debug:
        dbg["x1"] = nc.dram_tensor("dbg_x1", [NTOK, D], F32, kind="ExternalOutput")
        dbg["lg"] = nc.dram_tensor("dbg_lg", [128, NTL, E], F32, kind="ExternalOutput")
        dbg["slot"] = nc.dram_tensor("dbg_slot", [128, NTL, 4], F32, kind="ExternalOutput")
        dbg["g4"] = nc.dram_tensor("dbg_g4", [128, NTL, 4], F32, kind="ExternalOutput")
        dbg["be"] = nc.dram_tensor("dbg_be", [128, nstep], F32, kind="ExternalOutput")

    es = ExitStack()
    with es:
        P = Prog(nc, es)

        _cnt = [0]

        def sb(name, shape, dt, stack=es):
            _cnt[0] += 1
            return stack.enter_context(nc.sbuf_tensor("s%d_%s" % (_cnt[0], name), list(shape), dt))

        ps = [es.enter_context(nc.psum_tensor("ps%d" % i, [128, 512], F32)) for i in range(8)]

        def PSR(i):
            return ("ps", i)

        def psb(i):
            return ps[i][:].bitcast(BF16)

        ident = sb("ident", [128, 128], BF16)
        ones_bf = sb("ones_bf", [128, 512], BF16)
        zero_bf = sb("zero_bf", [128, 512], BF16)
        lg_all = sb("lg_all", [128, NTL, E], F32)
        mx8 = sb("mx8", [128, NTL, 8], F32)
        Mb = sb("Mb", [128, NTL * E], BF16)
        g4 = sb("g4", [128, NTL, 4], F32)
        nmx = sb("nmx", [128, NTL], F32)
        gsm = sb("gsm", [128, NTL], F32)
        iota_p = sb("iota_p", [128, 1], F32)
        iota_f = sb("iota_f", [128, 128], F32)

        def dve(fn, r=(), w=()):
            return P.op("dve", fn, r, w)

        def act(fn, r=(), w=()):
            return P.op("act", fn, r, w)

        def pool(fn, r=(), w=()):
            return P.op("pool", fn, r, w)

        def pe(fn, r=(), w=()):
            return P.op("pe", fn, r, w)

        def dma(eng, out, in_, r=(), w=(), **kw):
            return P.op(eng, lambda q: q.dma_start(out=out, in_=in_, **kw), r, w, dma=True)

        zf_state = [0]
        ZF_TOTAL = (NSLOT // 128) * 2

        def zero_fill(n):
            if not do_moe:
                return
            for _ in range(n):
                i = zf_state[0]
                if i >= ZF_TOTAL:
                    return
                zf_state[0] += 1
                r0, hf = (i // 2) * 128, i % 2
                dma("pool", XB_d[r0:r0 + 128, hf * 512:(hf + 1) * 512], zero_bf[:], r=["zero_bf"], w=[("XBz", i)])

        MOD_d = nc.dram_tensor("MODs", [nseq, 6, 128, D], F32)
        x_tiles = x_d.ap().rearrange("(t p) d -> t p d", p=128)
        X1_tiles = X1_d.ap().rearrange("(t p) d -> t p d", p=128)
        H2_tiles = H2_d.ap().rearrange("(t p) d -> t p d", p=128)
        slopes = [2.0 ** (-8.0 * (h + 1) / 4) for h in range(4)]

        with ExitStack() as cs:
            it_i = sb("it_i", [128, 128], I32, cs)
            ip_i = sb("ip_i", [128, 1], I32, cs)
            cT = sb("cT", [128, 8, nseq], F32, cs)
            scT = sb("scT", [128, 8, nseq], F32, cs)
            bcl2 = [sb("bcl%d" % i, [128, 8, 128], BF16, cs) for i in range(nseq)]
            wa_st = [sb("wa_st%d" % i, [128, 8, 512], BF16, cs) for i in range(6)]
            bada_b = [sb("bada_b%d" % i, [128, 512], F32, cs) for i in range(2)]
            g1b = sb("g1b", [128, D], F32, cs)
            g2b = sb("g2b", [128, D], F32, cs)
            modt2 = [[sb("modt%d_%d" % (b_i, i), [128, D], F32, cs) for i in range(6)] for b_i in range(nseq)]
            pool(lambda g: g.iota(it_i[:], pattern=[[1, 128]], base=0, channel_multiplier=0), w=["it_i"])
            pool(lambda g: g.iota(ip_i[:], pattern=[[1, 1]], base=0, channel_multiplier=1), w=["ip_i"])
            dve(lambda v: v.tensor_copy(out=iota_f[:], in_=it_i[:]), r=["it_i"], w=["iota_f"])
            dve(lambda v: v.tensor_copy(out=iota_p[:], in_=ip_i[:]), r=["ip_i"], w=["iota_p"])
            dve(lambda v: v.tensor_scalar(out=ident[:], in0=iota_f[:], scalar1=iota_p[:, 0:1], scalar2=None,
                                          op0=ALU.is_equal), r=["iota_f", "iota_p"], w=["ident"])
            dve(lambda v: v.memset(ones_bf[:], 1.0), w=["ones_bf"])
            dve(lambda v: v.memset(zero_bf[:], 0.0), w=["zero_bf"])
            dma("sp", cT[:], cT_d.ap(), w=["cT"])
            dma("sp", g1b[:], _ap(n1g_d.ap(), [[0, 128], [1, D]]), w=["g1b"])
            dma("sp", g2b[:], _ap(n2g_d.ap(), [[0, 128], [1, D]]), w=["g2b"])
            act(lambda a: a.activation(out=scT[:], in_=cT[:], func=AF.Silu), r=["cT"], w=["scT"])
            for b in range(nseq):
                dve(lambda v, b=b: v.tensor_copy(out=bcl2[b][:], in_=_ap(scT[:], [[8 * nseq, 128], [nseq, 8], [0, 128]], off=b)),
                    r=["scT"], w=[("bcl", b)])
            for cc in range(12):
                st = wa_st[cc % 6]
                dma("pool", st[:], wada_d.ap().rearrange("(kc p) n -> p kc n", p=128)[:, :, cc * 512:(cc + 1) * 512],
                    w=[("wa_st", cc % 6)])
                dma("sp", bada_b[cc % 2][:], _ap(bada_d.ap(), [[0, 128], [1, 512]], off=cc * 512), w=[("bada_b", cc % 2)])
                which, half = cc // 2, cc % 2
                for b in range(nseq):
                    bank = (cc * nseq + b) % 4
                    for kc in range(8):
                        pe(lambda t, kc=kc, st=st, bank=bank, b=b: t.matmul(ps[bank][:], lhsT=bcl2[b][:, kc, :], rhs=st[:, kc, :],
                                                                             start=(kc == 0), stop=(kc == 7)),
                           r=[("bcl", b), ("wa_st", cc % 6)], w=[PSR(bank)])
                    dve(lambda v, bank=bank, which=which, half=half, b=b, cc=cc: v.tensor_tensor(
                        out=modt2[b][which][:, half * 512:(half + 1) * 512], in0=ps[bank][:], in1=bada_b[cc % 2][:], op=ALU.add),
                        r=[PSR(bank), ("bada_b", cc % 2)], w=[("modt", b, which)])
            for b in range(nseq):
                for (which, gx, gname) in ((1, g1b, "g1b"), (4, g2b, "g2b")):
                    dve(lambda v, which=which, gx=gx, b=b: v.scalar_tensor_tensor(out=modt2[b][which][:], in0=modt2[b][which][:], scalar=1.0,
                                                                                 in1=gx[:], op0=ALU.add, op1=ALU.mult),
                        r=[("modt", b, which), gname], w=[("modt", b, which)])
                for which in range(6):
                    dma("sp", MOD_d[b, which], modt2[b][which][:], r=[("modt", b, which)], w=[("MODd", b, which)])
            if stop == "ada" and debug:
                dma("sp", dbg["x1"].ap()[0:128, :], modt2[0][1][:], r=[("modt", 0, 1)], w=["dbgada"])
            P.barrier()
            P.emit()
            if stop == "ada":
                return nc

        with ExitStack() as pa:
            W_r = sb("W_r", [128, 8, E], BF16, pa)
            brb = sb("brb", [128, E], F32, pa)
            gq = sb("gq", [128, 1], F32, pa)
            gk = sb("gk", [128, 1], F32, pa)
            lams = sb("lams", [128, 2], F32, pa)
            neglam = sb("neglam", [128, 1], F32, pa)
            cw = sb("cw", [128, 4, 31], F32, pa)
            cwb = sb("cwb", [128, 4, 31], BF16, pa)
            cb = sb("cb", [128, 4], F32, pa)
            clg = sb("clg", [128, 4], F32, pa)
            clb = sb("clb", [128, 4], F32, pa)
            blk1 = sb("blk1", [128, 128], BF16, pa)
            om512 = sb("om512", [128, 128], BF16, pa)
            eps_c = sb("eps_c", [128, 1], F32, pa)
            hbuf = sb("hbuf", [128, 4, S + 32], BF16, pa)
            qT = sb("qT", [128, 4, S], BF16, pa)
            kT = sb("kT", [128, 4, S], BF16, pa)
            vS = sb("vS", [128, 16, 4, 128], BF16, pa)
            xs = [sb("xs%d" % i, [128, D], F32, pa) for i in range(2)]
            tmp1 = sb("tmp1", [128, D], F32, pa)
            htok = [sb("htok%d" % i, [128, D], BF16, pa) for i in range(2)]
            ss = sb("ss", [128, 4], F32, pa)
            sig = [sb("sig%d" % i, [128, 512], BF16, pa) for i in range(2)]
            sq = [sb("sq%d" % i, [128, 512], BF16, pa) for i in range(2)]
            rs = [sb("rs%d" % i, [128, 512], F32, pa) for i in range(2)]
            Eb = [sb("Eb%d" % i, [128, 512], BF16, pa) for i in range(3)]
            Pt = [sb("Pt%d" % i, [128, 512], BF16, pa) for i in range(5)]
            qz = [[sb("qz%d%d" % (i, c_), [128, 512], BF16, pa) for c_ in range(2)] for i in range(2)]
            oT = [sb("oT%d" % i, [128, 512], F32, pa) for i in range(2)]
            om128 = sb("om128", [128, 128], BF16, pa)
            subgc = sb("subgc", [128, 1], F32, pa)
            sm = sb("sm", [128, 8], F32, pa)
            tmp_scope = ExitStack()
            tmp_scope.__enter__()
            pge = sb("pge", [128, 1], F32, tmp_scope)
            lamb = sb("lamb", [128, 4, 64], F32, tmp_scope)
            lamt = sb("lamt", [128, 2, 64], F32, tmp_scope)
            o1 = sb("o1", [128, 128], F32, tmp_scope)

            dma("pool", W_r[:], wr_d.ap().rearrange("(kc p) n -> p kc n", p=128), w=["W_r"])
            dma("sp", brb[:], _ap(br_d.ap(), [[0, 128], [1, E]]), w=["brb"])
            dma("sp", gq[:], gq_d.ap(), w=["gq"])
            dma("sp", gk[:], gk_d.ap(), w=["gk"])
            dma("sp", lamb[:], _ap(lamv_d.ap(), [[0, 128], [64, 4], [1, 64]]), w=["lamb"])
            dma("sp", cw[:], cw_d.ap(), w=["cw"])
            dma("sp", cb[:], cb_d.ap(), w=["cb"])
            dma("sp", clg[:], clg_d.ap(), w=["clg"])
            dma("sp", clb[:], clb_d.ap(), w=["clb"])
            dve(lambda v: v.tensor_scalar(out=gq[:], in0=gq[:], scalar1=0.125, scalar2=None, op0=ALU.mult),
                r=["gq"], w=["gq"])
            dve(lambda v: v.tensor_tensor(out=lamt[:], in0=lamb[:, 0:2, :], in1=lamb[:, 2:4, :], op=ALU.mult),
                r=["lamb"], w=["lamt"])
            dve(lambda v: v.tensor_reduce(out=lams[:], in_=lamt[:], axis=AX.X, op=ALU.add), r=["lamt"], w=["lams"])
            act(lambda a: a.activation(out=lams[:], in_=lams[:], func=AF.Exp), r=["lams"], w=["lams"])
            dve(lambda v: v.tensor_tensor(out=neglam[:], in0=lams[:, 1:2], in1=lams[:, 0:1], op=ALU.subtract),
                r=["lams"], w=["neglam"])
            dve(lambda v: v.tensor_scalar(out=neglam[:], in0=neglam[:], scalar1=-LAM_INIT, scalar2=None, op0=ALU.add),
                r=["neglam"], w=["neglam"])
            dma("sp", subgc[:], _ap(subg_d.ap(), [[1, 128], [1, 1]]), w=["subgc"])
            dve(lambda v: v.tensor_scalar(out=subgc[:], in0=subgc[:], scalar1=(1.0 - LAM_INIT), scalar2=None,
                                          op0=ALU.mult), r=["subgc"], w=["subgc"])
            dve(lambda v: v.memset(om128[:], 1.0 / 128), w=["om128"])
            dve(lambda v: v.tensor_copy(out=cwb[:], in_=cw[:]), r=["cw"], w=["cwb"])
            for i_ in range(2):
                for c_ in range(2):
                    dve(lambda v, i_=i_, c_=c_: v.memset(qz[i_][c_][:], 0.0), w=[("qz", i_)])
            dve(lambda v: v.tensor_scalar(out=o1[:], in0=iota_f[:], scalar1=64.0, scalar2=None, op0=ALU.is_ge),
                r=["iota_f"], w=["o1"])
            dve(lambda v: v.tensor_scalar(out=pge[:], in0=iota_p[:], scalar1=64.0, scalar2=None, op0=ALU.is_ge),
                r=["iota_p"], w=["pge"])
            dve(lambda v: v.tensor_scalar(out=blk1[:], in0=o1[:], scalar1=pge[:, 0:1], scalar2=1.0 / 64,
                                          op0=ALU.is_equal, op1=ALU.mult), r=["o1", "pge"], w=["blk1"])
            dve(lambda v: v.memset(om512[:], 1.0 / 512), w=["om512"])
            dve(lambda v: v.memset(eps_c[:], EPS), w=["eps_c"])
            dve(lambda v: v.memset(hbuf[:], 0.0), w=[("hbuf", i) for i in range(4)])
            dve(lambda v: v.memset(vS[:], 1.0), w=[("vS", t) for t in range(16)])
            P.barrier()
            P.emit()
            tmp_scope.__exit__(None, None, None)
            if stop == "consts":
                return nc

            for b in range(nseq):
                seq_scope = ExitStack()
                seq_scope.__enter__()
                W_out = sb("W_out", [128, 8, D], BF16, seq_scope)
                G1 = sb("G1", [128, D], F32, seq_scope)
                dma("pool", W_out[:], wout_d.ap().rearrange("(kc p) n -> p kc n", p=128), w=["W_out"])
                dma("sp", G1[:], MOD_d[b, 2], w=["G1"])
                with ExitStack() as p1:
                    W_in = sb("W_in", [128, 8, 2560], BF16, p1)
                    hT2 = [sb("hT%d" % i_, [128, 8, 512], BF16, p1) for i_ in range(2)]
                    S1 = sb("S1", [128, D], F32, p1)
                    H1 = sb("H1", [128, D], F32, p1)
                    dma("pool", W_in[:], win_d.ap().rearrange("(kc p) n -> p kc n", p=128), w=["W_in"])
                    dma("sp", H1[:], MOD_d[b, 0], w=["H1"])
                    dma("sp", S1[:], MOD_d[b, 1], w=["S1"])
                    def chain(w_, tl):
                        t = b * 16 + w_ * 4 + tl
                        sl = t % 2
                        zero_fill(-(-ZF_TOTAL // (2 * nseq * 16)))
                        dma("sp", xs[sl][:], x_tiles[t], w=[("xs", sl)])
                        act(lambda a, sl=sl: a.activation(out=htok[sl][:], in_=xs[sl][:], func=AF.Square, scale=D ** -0.5,
                                                          accum_out=ss[:, 0:1]),
                            r=[("xs", sl)], w=[("htok", sl), ("ss", 0)])
                        act(lambda a: a.activation(out=ss[:, 1:2], in_=ss[:, 0:1], func=AF.Ln, bias=eps_c[:, 0:1]),
                            r=[("ss", 0), "eps_c"], w=[("ss", 1)])
                        act(lambda a: a.activation(out=ss[:, 1:2], in_=ss[:, 1:2], func=AF.Exp, scale=-0.5),
                            r=[("ss", 1)], w=[("ss", 1)])
                        dve(lambda v, sl=sl: v.scalar_tensor_tensor(out=tmp1[:], in0=xs[sl][:], scalar=ss[:, 1:2], in1=S1[:],
                                                                    op0=ALU.mult, op1=ALU.mult),
                            r=[("xs", sl), ("ss", 1), "S1"], w=["tmp1"])
                        dve(lambda g, sl=sl: g.tensor_tensor(out=htok[sl][:], in0=tmp1[:], in1=H1[:], op=ALU.add),
                            r=["tmp1", "H1"], w=[("htok", sl)])

                    def trans(w_, tl):
                        t = b * 16 + w_ * 4 + tl
                        sl = t % 2
                        bank = tl % 2
                        hTw = hT2[w_ % 2]
                        for kc in range(8):
                            pe(lambda tt, kc=kc, sl=sl, bank=bank: tt.transpose(psb(bank)[:, kc * 128:(kc + 1) * 128],
                                                                               htok[sl][:, kc * 128:(kc + 1) * 128], ident[:]),
                               r=[("htok", sl), "ident"], w=[PSR(bank)])
                        act(lambda a, bank=bank, tl=tl, hTw=hTw: a.activation(
                            out=hTw[:, :, tl * 128:(tl + 1) * 128],
                            in_=psb(bank).rearrange("p (k t) -> p k t", k=8), func=AF.Copy),
                            r=[PSR(bank)], w=[("hT", w_ % 2)])

                    def glu_chunk(w_, i):
                        hTw = hT2[w_ % 2]
                        hres = ("hT", w_ % 2)
                        tok0 = w_ * 512
                        ba, bg = 2 + 2 * (i % 2), 3 + 2 * (i % 2)
                        for (bank, oc) in ((ba, i), (bg, 4 + i)):
                            for kc in range(8):
                                pe(lambda tt, kc=kc, bank=bank, oc=oc, hTw=hTw: tt.matmul(
                                    ps[bank][:], lhsT=W_in[:, kc, oc * 128:(oc + 1) * 128], rhs=hTw[:, kc, :],
                                    start=(kc == 0), stop=(kc == 7)), r=["W_in", hres], w=[PSR(bank)])
                        act(lambda a, bg=bg, i=i: a.activation(out=sig[i % 2][:], in_=ps[bg][:], func=AF.Sigmoid),
                            r=[PSR(bg)], w=[("sig", i % 2)])
                        dve(lambda v, ba=ba, i=i, tok0=tok0: v.tensor_tensor(
                            out=hbuf[:, i, 15 + tok0:15 + tok0 + 512], in0=ps[ba][:], in1=sig[i % 2][:], op=ALU.mult),
                            r=[PSR(ba), ("sig", i % 2)], w=[("hbuf", i)])

                    def qk_chunk(w_, qi):
                        hTw = hT2[w_ % 2]
                        hres = ("hT", w_ % 2)
                        tok0 = w_ * 512
                        hh = qi % 4
                        isq = qi < 4
                        oc = (8 + hh) if isq else (12 + hh)
                        bd_, bs_ = (2, 4, 6)[qi % 3], (3, 5, 7)[qi % 3]
                        for kc in range(8):
                            pe(lambda tt, kc=kc, bd_=bd_, oc=oc, hTw=hTw: tt.matmul(
                                ps[bd_][:], lhsT=W_in[:, kc, oc * 128:(oc + 1) * 128], rhs=hTw[:, kc, :],
                                start=(kc == 0), stop=(kc == 7)), r=["W_in", hres], w=[PSR(bd_)])
                        act(lambda a, bd_=bd_, qi=qi: a.activation(out=sq[qi % 2][:], in_=ps[bd_][:], func=AF.Square),
                            r=[PSR(bd_)], w=[("sq", qi % 2)])
                        return (qi, hh, isq, bd_, bs_, tok0)

                    def qk_finish(state):
                        qi, hh, isq, bd_, bs_, tok0 = state
                        pe(lambda tt, bs_=bs_, qi=qi: tt.matmul(ps[bs_][:], lhsT=blk1[:], rhs=sq[qi % 2][:], start=True, stop=True),
                           r=["blk1", ("sq", qi % 2)], w=[PSR(bs_)])
                        act(lambda a, bs_=bs_, qi=qi: a.activation(out=rs[qi % 2][:], in_=ps[bs_][:], func=AF.Ln, bias=eps_c[:, 0:1]),
                            r=[PSR(bs_), "eps_c"], w=[("rs", qi % 2)])
                        act(lambda a, qi=qi: a.activation(out=rs[qi % 2][:], in_=rs[qi % 2][:], func=AF.Exp, scale=-0.5),
                            r=[("rs", qi % 2)], w=[("rs", qi % 2)])
                        dstT = qT if isq else kT
                        gcol = gq if isq else gk
                        dve(lambda v, bd_=bd_, qi=qi, dstT=dstT, gcol=gcol, hh=hh, tok0=tok0: v.scalar_tensor_tensor(
                            out=dstT[:, hh, tok0:tok0 + 512], in0=ps[bd_][:], scalar=gcol[:, 0:1], in1=rs[qi % 2][:],
                            op0=ALU.mult, op1=ALU.mult),
                            r=[PSR(bd_), ("rs", qi % 2), "gq", "gk"], w=[("qk", isq, hh)])

                    def v_tile(w_, tl):
                        hTw = hT2[w_ % 2]
                        hres = ("hT", w_ % 2)
                        bank = 6 + tl % 2
                        kt = w_ * 4 + tl
                        for kc in range(8):
                            pe(lambda tt, kc=kc, bank=bank, tl=tl, hTw=hTw: tt.matmul(
                                ps[bank][:], lhsT=hTw[:, kc, tl * 128:(tl + 1) * 128], rhs=W_in[:, kc, 2048:2560],
                                start=(kc == 0), stop=(kc == 7)), r=["W_in", hres], w=[PSR(bank)])
                        act(lambda a, bank=bank, kt=kt: a.activation(
                            out=vS[:, kt, :, 0:128], in_=ps[bank][:].rearrange("p (h e) -> p h e", h=4), func=AF.Copy),
                            r=[PSR(bank)], w=[("vS", kt)])

                    for tl in range(4):
                        chain(0, tl)
                        trans(0, tl)
                    for w_ in range(4):
                        nxt = w_ + 1 < 4
                        for q in range(4):
                            if nxt:
                                chain(w_ + 1, q)
                            if q == 0:
                                glu_chunk(w_, 0); glu_chunk(w_, 1)
                            elif q == 1:
                                glu_chunk(w_, 2); glu_chunk(w_, 3)
                            elif q == 2:
                                prev = None
                                for qi in range(4):
                                    st_ = qk_chunk(w_, qi)
                                    if prev is not None:
                                        qk_finish(prev)
                                    prev = st_
                                qk_finish(prev)
                            else:
                                prev = None
                                for qi in range(4, 8):
                                    st_ = qk_chunk(w_, qi)
                                    if prev is not None:
                                        qk_finish(prev)
                                    prev = st_
                                v_tile(w_, 0)
                                qk_finish(prev)
                                for tl in range(1, 4):
                                    v_tile(w_, tl)
                            if nxt:
                                trans(w_ + 1, q)
                    P.barrier()
                    P.emit()
                    if stop == "p1":
                        return nc

                with ExitStack() as p2:
                    tA_i = sb("tA_i", [128, 4096], I32, p2)
                    Tdec = sb("Tdec", [128, 2432], BF16, p2)
                    diag = [sb("diag%d" % i, [128, 31, 128], BF16, p2) for i in range(2)]
                    catT = sb("catT", [128, 8, 512], BF16, p2)
                    cvv = sb("cvv", [128, 4, 512], F32, p2)
                    cst = sb("cst", [128, 2, 512], F32, p2)
                    x1t = sb("x1t", [128, D], F32, p2)
                    h2T = sb("h2T", [128, 8, 128], BF16, p2)
                    S2 = sb("S2", [128, D], F32, p2)
                    H2 = sb("H2", [128, D], F32, p2)
                    dma("sp", H2[:], MOD_d[b, 3], w=["H2"])
                    dma("sp", S2[:], MOD_d[b, 4], w=["S2"])
                    pool(lambda g: g.iota(tA_i[:], pattern=[[1, 4096]], base=-2048, channel_multiplier=-1), w=["tA"])
                    dve(lambda v: v.scalar_tensor_tensor(out=tA_i[:], in0=tA_i[:], scalar=-1.0, in1=tA_i[:], op0=ALU.mult, op1=ALU.max),
                        r=["tA"], w=["tA"])
                    QKR = [("qk", a_, h_) for a_ in (True, False) for h_ in range(4)]
                    VR = [("vS", t_) for t_ in range(16)]
                    nd = 0

                    def build_diag(i, slot):
                        dg_ = diag[slot]
                        dve(lambda g, i=i, dg_=dg_: g.tensor_tensor(out=dg_[:], in0=_ap(ident[:], [[128, 128], [0, 31], [1, 128]]),
                                                                    in1=_ap(cwb[:], [[124, 128], [1, 31], [0, 128]], off=i * 31), op=ALU.mult),
                            r=["ident", "cwb"], w=[("diag", slot)])

                    build_diag(0, 0)
                    pending = []
                    stats_pending = []
                    for w_ in range(4):
                        tok0 = w_ * 512
                        for i in range(4):
                            dg = diag[nd % 2]
                            dgr = ("diag", nd % 2)
                            nd += 1
                            if not (w_ == 3 and i == 3):
                                build_diag((i + 1) % 4, nd % 2)
                            bank = i % 2
                            for j in range(31):
                                pe(lambda tt, i=i, j=j, bank=bank, tok0=tok0, dg=dg: tt.matmul(
                                    ps[bank][:], lhsT=dg[:, j, :], rhs=hbuf[:, i, tok0 + j:tok0 + j + 512],
                                    start=(j == 0), stop=(j == 30)), r=[dgr, ("hbuf", i)], w=[PSR(bank)])
                            while stats_pending:
                                stats_pending.pop(0)()
                            act(lambda a, i=i, bank=bank: a.activation(out=cvv[:, i, :], in_=ps[bank][:], func=AF.Identity,
                                                                        bias=cb[:, i:i + 1]), r=[PSR(bank), "cb"], w=[("cvv", i)])
                            dve(lambda v, i=i: v.tensor_copy(out=sig[i % 2][:], in_=cvv[:, i, :]),
                                r=[("cvv", i)], w=[("sig", i % 2)])
                            act(lambda a, i=i: a.activation(out=sq[i % 2][:], in_=cvv[:, i, :], func=AF.Square),
                                r=[("cvv", i)], w=[("sq", i % 2)])
                            def _stats(i=i):
                                pe(lambda tt, i=i: tt.matmul(ps[2][:], lhsT=om512[:], rhs=sig[i % 2][:],
                                                             start=(i == 0), stop=(i == 3)), r=["om512", ("sig", i % 2)], w=[PSR(2)])
                                pe(lambda tt, i=i: tt.matmul(ps[3][:], lhsT=om512[:], rhs=sq[i % 2][:], start=(i == 0), stop=(i == 3)),
                                   r=["om512", ("sq", i % 2)], w=[PSR(3)])
                            stats_pending.append(_stats)
                        while stats_pending:
                            stats_pending.pop(0)()
                        act(lambda a: a.activation(out=cst[:, 0, :], in_=ps[2][:], func=AF.Copy), r=[PSR(2)], w=[("cst", 0)])
                        dve(lambda v: v.tensor_tensor(out=rs[0][:], in0=cst[:, 0, :], in1=cst[:, 0, :], op=ALU.mult),
                            r=[("cst", 0)], w=[("rs", 0)])
                        dve(lambda v: v.tensor_tensor(out=cst[:, 1, :], in0=ps[3][:], in1=rs[0][:], op=ALU.subtract),
                            r=[PSR(3), ("rs", 0)], w=[("cst", 1)])
                        act(lambda a: a.activation(out=cst[:, 1, :], in_=cst[:, 1, :], func=AF.Ln, bias=eps_c[:, 0:1]),
                            r=[("cst", 1), "eps_c"], w=[("cst", 1)])
                        act(lambda a: a.activation(out=cst[:, 1, :], in_=cst[:, 1, :], func=AF.Exp, scale=-0.5),
                            r=[("cst", 1)], w=[("cst", 1)])
                        for i in range(4):
                            dve(lambda v, i=i: v.tensor_tensor(out=cvv[:, i, :], in0=cvv[:, i, :], in1=cst[:, 0, :], op=ALU.subtract),
                                r=[("cvv", i), ("cst", 0)], w=[("cvv", i)])
                            dve(lambda v, i=i: v.tensor_tensor(out=cvv[:, i, :], in0=cvv[:, i, :], in1=cst[:, 1, :], op=ALU.mult),
                                r=[("cvv", i), ("cst", 1)], w=[("cvv", i)])
                            act(lambda a, i=i: a.activation(out=catT[:, i, :], in_=cvv[:, i, :], func=AF.Silu,
                                                            bias=clb[:, i:i + 1], scale=clg[:, i:i + 1]),
                                r=[("cvv", i), "clb", "clg"], w=[("catT", i)])
                        lo = tok0 + 128
                        items = [(hh, c, kt) for hh in range(4) for kt in range(16) for c in range(2)]

                        def stage1(g):
                            hh, c, kt = items[g]
                            if c == 0 and kt == 0:
                                act(lambda a, hh=hh, lo=lo: a.activation(out=Tdec[:], in_=tA_i[:, lo:lo + 2432], func=AF.Exp,
                                                                         scale=-slopes[hh]), r=["tA"], w=["Tdec"])
                                pool(lambda gp, hh=hh, tok0=tok0: gp.tensor_copy(out=qz[hh % 2][0][0:64, :], in_=qT[0:64, hh, tok0:tok0 + 512]),
                                     r=QKR, w=[("qz", hh % 2)])
                                pool(lambda gp, hh=hh, tok0=tok0: gp.tensor_copy(out=qz[hh % 2][1][64:128, :], in_=qT[64:128, hh, tok0:tok0 + 512]),
                                     r=QKR + [("qz", hh % 2)], w=[("qz", hh % 2)])
                            sbk = 3 + g % 3
                            pe(lambda tt, hh=hh, c=c, kt=kt, sbk=sbk: tt.matmul(
                                ps[sbk][:], lhsT=kT[:, hh, kt * 128:(kt + 1) * 128],
                                rhs=qz[hh % 2][c][:], start=True, stop=True),
                               r=QKR + [("qz", hh % 2)], w=[PSR(sbk)])
                            act(lambda a, sbk=sbk, g=g: a.activation(out=Eb[g % 3][:], in_=ps[sbk][:], func=AF.Exp),
                                r=[PSR(sbk)], w=[("Eb", g % 3)])
                            i0 = tok0 - kt * 128 + 2048 - lo
                            dve(lambda v, g=g, i0=i0: v.tensor_tensor(out=Pt[g % 5][:], in0=Eb[g % 3][:],
                                                                      in1=Tdec[:, i0:i0 + 512], op=ALU.mult),
                                r=[("Eb", g % 3), "Tdec"], w=[("Pt", g % 5)])

                        def stage2(g):
                            hh, c, kt = items[g]
                            bankA, bankB = (6, 7) if c == 0 else (0, 1)
                            pe(lambda tt, kt=kt, bankA=bankA, hh=hh, g=g: tt.matmul(
                                ps[bankA][:], lhsT=vS[:, kt, hh, 0:128], rhs=Pt[g % 5][:], start=(kt == 0), stop=(kt == 15)),
                               r=[("Pt", g % 5)] + VR, w=[PSR(bankA)])
                            pe(lambda tt, kt=kt, bankB=bankB, g=g: tt.matmul(
                                ps[bankB][:], lhsT=ones_bf[:, 0:128], rhs=Pt[g % 5][:], start=(kt == 0), stop=(kt == 15)),
                               r=[("Pt", g % 5), "ones_bf"], w=[PSR(bankB)])
                            if kt != 15:
                                return
                            act(lambda a, bankB=bankB, c=c: a.activation(out=rs[c][:], in_=ps[bankB][:], func=AF.Ln), r=[PSR(bankB)], w=[("rs", c)])
                            act(lambda a, c=c: a.activation(out=rs[c][:], in_=rs[c][:], func=AF.Exp, scale=-1.0), r=[("rs", c)], w=[("rs", c)])
                            dve(lambda v, bankA=bankA, c=c: v.tensor_tensor(out=oT[c][:], in0=ps[bankA][:], in1=rs[c][:], op=ALU.mult),
                                r=[PSR(bankA), ("rs", c)], w=[("oT", c)])
                            if c == 1:
                                dve(lambda v: v.scalar_tensor_tensor(out=oT[1][:], in0=oT[1][:], scalar=neglam[:, 0:1], in1=oT[0][:],
                                                                     op0=ALU.mult, op1=ALU.add),
                                    r=[("oT", 0), ("oT", 1), "neglam"], w=[("oT", 1)])
                                dve(lambda v: v.tensor_tensor(out=sq[0][:], in0=oT[1][:], in1=oT[1][:], op=ALU.mult),
                                    r=[("oT", 1)], w=[("sq", 0)])

                                def _fin(hh=hh):
                                    pe(lambda tt: tt.matmul(ps[2][:], lhsT=om128[:], rhs=sq[0][:], start=True, stop=True),
                                       r=["om128", ("sq", 0)], w=[PSR(2)])
                                    act(lambda a: a.activation(out=rs[0][:], in_=ps[2][:], func=AF.Ln, bias=eps_c[:, 0:1]),
                                        r=[PSR(2), "eps_c"], w=[("rs", 0)])
                                    act(lambda a: a.activation(out=rs[0][:], in_=rs[0][:], func=AF.Exp, scale=-0.5),
                                        r=[("rs", 0)], w=[("rs", 0)])
                                    dve(lambda v, hh=hh: v.scalar_tensor_tensor(out=catT[:, 4 + hh, :], in0=oT[1][:], scalar=subgc[:, 0:1],
                                                                                in1=rs[0][:], op0=ALU.mult, op1=ALU.mult),
                                        r=[("oT", 1), ("rs", 0), "subgc"], w=[("catT", 4 + hh)])
                                pending.append(_fin)

                        DEPTH = 4
                        for g in range(len(items)):
                            stage1(g)
                            if g >= DEPTH:
                                stage2(g - DEPTH)
                            if items[g][2] == 9 and items[g][1] == 1 and pending:
                                pending.pop(0)()
                        for g in range(len(items) - DEPTH, len(items)):
                            stage2(g)
                        while pending:
                            pending.pop(0)()
                        CATR = [("catT", i) for i in range(8)]

                        def stA(u):
                            t = b * 16 + w_ * 4 + u
                            sl = t % 2
                            zero_fill(-(-ZF_TOTAL // (2 * nseq * 16)))
                            dma("sp", xs[sl][:], x_tiles[t], w=[("xs", sl)])
                            for half in range(2):
                                bank = 4 + half
                                for kc in range(8):
                                    pe(lambda tt, kc=kc, u=u, half=half, bank=bank: tt.matmul(
                                        ps[bank][:], lhsT=catT[:, kc, u * 128:(u + 1) * 128],
                                        rhs=W_out[:, kc, half * 512:(half + 1) * 512], start=(kc == 0), stop=(kc == 7)),
                                       r=CATR + ["W_out"], w=[PSR(bank)])
                                dve(lambda v, half=half, bank=bank: v.tensor_tensor(
                                    out=tmp1[:, half * 512:(half + 1) * 512], in0=ps[bank][:], in1=G1[:, half * 512:(half + 1) * 512],
                                    op=ALU.mult), r=[PSR(bank), "G1"], w=[("tmp1h", half)])

                        def stB(u):
                            t = b * 16 + w_ * 4 + u
                            sl = t % 2
                            dve(lambda g, sl=sl: g.tensor_tensor(out=x1t[:], in0=tmp1[:], in1=xs[sl][:], op=ALU.add),
                                r=[("tmp1h", 0), ("tmp1h", 1), ("xs", sl)], w=["x1t", "tmp1"])
                            dma("sp", X1_tiles[t], x1t[:], r=["x1t"], w=[("X1", t)])
                            if debug:
                                dma("sp", dbg["x1"].ap().rearrange("(t p) d -> t p d", p=128)[t], x1t[:],
                                    r=["x1t"], w=[("dbgx1", t)])
                            act(lambda a, sl=sl: a.activation(out=htok[sl][:], in_=x1t[:], func=AF.Square, scale=D ** -0.5, accum_out=ss[:, 2:3]),
                                r=["x1t"], w=[("htok", sl), ("ss", 2)])
                            act(lambda a: a.activation(out=ss[:, 3:4], in_=ss[:, 2:3], func=AF.Ln, bias=eps_c[:, 0:1]),
                                r=[("ss", 2), "eps_c"], w=[("ss", 3)])
                            act(lambda a: a.activation(out=ss[:, 3:4], in_=ss[:, 3:4], func=AF.Exp, scale=-0.5),
                                r=[("ss", 3)], w=[("ss", 3)])
                            dve(lambda v: v.scalar_tensor_tensor(out=tmp1[:], in0=x1t[:], scalar=ss[:, 3:4], in1=S2[:],
                                                                 op0=ALU.mult, op1=ALU.mult),
                                r=["x1t", ("ss", 3), "S2"], w=["tmp1", ("tmp1h", 0), ("tmp1h", 1)])
                            dve(lambda g, sl=sl: g.tensor_tensor(out=htok[sl][:], in0=tmp1[:], in1=H2[:], op=ALU.add),
                                r=["tmp1", ("tmp1h", 0), ("tmp1h", 1), "H2"], w=[("htok", sl)])
                            dma("sp", H2_tiles[t], htok[sl][:], r=[("htok", sl)], w=[("H2d", t)])

                        def stC(u):
                            t = b * 16 + w_ * 4 + u
                            sl = t % 2
                            for kc in range(8):
                                pe(lambda tt, kc=kc, sl=sl: tt.transpose(psb(2)[:, kc * 128:(kc + 1) * 128],
                                                                         htok[sl][:, kc * 128:(kc + 1) * 128], ident[:]),
                                   r=[("htok", sl), "ident"], w=[PSR(2)])
                            act(lambda a: a.activation(out=h2T[:], in_=psb(2).rearrange("p (k t) -> p k t", k=8), func=AF.Copy),
                                r=[PSR(2)], w=["h2T"])
                            for kc in range(8):
                                pe(lambda tt, kc=kc: tt.matmul(ps[3][:, 0:E], lhsT=h2T[:, kc, :], rhs=W_r[:, kc, :],
                                                               start=(kc == 0), stop=(kc == 7)), r=["h2T", "W_r"], w=[PSR(3)])
                            dve(lambda v, t=t: v.tensor_tensor(out=lg_all[:, t, :], in0=ps[3][:, 0:E], in1=brb[:], op=ALU.add),
                                r=[PSR(3), "brb"], w=[("lg", t)])
                            dve(lambda v, t=t: v.max(out=mx8[:, t, :], in_=lg_all[:, t, :]), r=[("lg", t)], w=[("mx8", t)])
                            dve(lambda v, t=t: v.tensor_scalar(out=Mb[:, t * E:(t + 1) * E], in0=lg_all[:, t, :], scalar1=mx8[:, t, 3:4],
                                                               scalar2=None, op0=ALU.is_ge), r=[("lg", t), ("mx8", t)], w=[("Mb", t)])
                            dve(lambda v, t=t: v.tensor_scalar(out=nmx[:, t:t + 1], in0=mx8[:, t, 0:1], scalar1=-1.0, scalar2=None,
                                                               op0=ALU.mult), r=[("mx8", t)], w=[("nmx", t)])
                            act(lambda a, t=t: a.activation(out=g4[:, t, :], in_=mx8[:, t, 0:4], func=AF.Exp, bias=nmx[:, t:t + 1],
                                                            accum_out=gsm[:, t:t + 1]), r=[("mx8", t), ("nmx", t)], w=[("g4", t), ("gsm", t)])
                            dve(lambda v, t=t: v.reciprocal(out=gsm[:, t:t + 1], in_=gsm[:, t:t + 1]), r=[("gsm", t)], w=[("gsm", t)])
                            dve(lambda v, t=t: v.tensor_scalar(out=g4[:, t, :], in0=g4[:, t, :], scalar1=gsm[:, t:t + 1], scalar2=None,
                                                               op0=ALU.mult), r=[("g4", t), ("gsm", t)], w=[("g4", t)])

                        stA(0)
                        stB(0)
                        for u in range(1, 4):
                            stA(u)
                            stC(u - 1)
                            stB(u)
                        stC(3)
                    P.barrier()
                    P.emit()
                    if stop == "p2":
                        return nc
                seq_scope.__exit__(None, None, None)

        with ExitStack() as pb:
            sloti = sb("sloti", [128, NTL * 4], I32, pb)
            bei = sb("bei", [128, nstep], I32, pb)
            widx = sb("widx", [128, nstep * 8], I32, pb)
            bsel = sb("bsel", [128, 16, nstep], F32, pb)
            ohT = sb("ohT", [128, nstep], F32, pb)
            bgu = sb("bgu", [E, 2 * D], BF16, pb)
            bdn = sb("bdn", [128, D], BF16, pb)
            rt_scope = ExitStack()
            rt_scope.__enter__()
            Lst = sb("Lst", [128, 128], BF16, rt_scope)
            onesq = sb("onesq", [128, 128], BF16, rt_scope)
            pre = sb("pre", [128, NTL, E], F32, rt_scope)
            csb = sb("csb", [128, NTL, E], F32, rt_scope)
            off = sb("off", [128, NTL, E], F32, rt_scope)
            cnt = sb("cnt", [128, E], F32, rt_scope)
            pad = sb("pad", [128, E], F32, rt_scope)
            pst = sb("pst", [128, E], F32, rt_scope)
            pend = sb("pend", [128, E], F32, rt_scope)
            big = sb("big", [128, NTL, 4, E], F32, rt_scope)
            slotf = sb("slotf", [128, NTL, 4], F32, rt_scope)
            bthr = sb("bthr", [128, nstep], F32, rt_scope)
            bthr_i = sb("bthr_i", [128, nstep], I32, rt_scope)
            cmpb = sb("cmpb", [128, nstep, E], F32, rt_scope)
            bef = sb("bef", [128, nstep], F32, rt_scope)
            bgu_f = sb("bgu_f", [E, 2 * D], F32, rt_scope)
            bd_f = sb("bd_f", [E, D], F32, rt_scope)
            cmp8 = sb("cmp8", [128, E, 16], F32, rt_scope)
            kcp_i = sb("kcp_i", [128, 8], I32, rt_scope)
            kcp = sb("kcp", [128, 8], F32, rt_scope)
            widx_f = sb("widx_f", [128, nstep, 8], F32, rt_scope)
            bef1k = sb("bef1k", [128, nstep], F32, rt_scope)
            inact = sb("inact", [128, nstep], F32, rt_scope)
            ohTb = sb("ohTb", [E, nstep], BF16, rt_scope)

            LGR = [("lg", t) for t in range(NTL)]
            dve(lambda v: v.tensor_scalar(out=Lst[:], in0=iota_f[:], scalar1=iota_p[:, 0:1], scalar2=None, op0=ALU.is_gt),
                r=["iota_f", "iota_p"], w=["Lst"])
            dve(lambda v: v.memset(onesq[:], 1.0), w=["onesq"])
            pool(lambda g: g.iota(bthr_i[:], pattern=[[RB, nstep]], base=0, channel_multiplier=0), w=["bthr_i"])
            dve(lambda v: v.tensor_copy(out=bthr[:], in_=bthr_i[:]), r=["bthr_i"], w=["bthr"])
            dma("sp", bgu_f[:], bgu_d.ap(), w=["bgu_f"])
            dma("sp", bd_f[:], bd_d.ap(), w=["bd_f"])
            act(lambda a: a.activation(out=bgu[:], in_=bgu_f[:], func=AF.Copy), r=["bgu_f"], w=["bgu"])
            dve(lambda v: v.memset(bdn[:], 0.0), w=["bdn"])
            act(lambda a: a.activation(out=bdn[0:E, :], in_=bd_f[:], func=AF.Copy), r=["bd_f", "bdn"], w=["bdn"])
            MXR = []
            G4R = []
            nh = (NTL * E) // 512
            for hf in range(nh):
                pe(lambda tt, hf=hf: tt.matmul(ps[hf][:], lhsT=Lst[:], rhs=Mb[:, hf * 512:(hf + 1) * 512], start=True, stop=True),
                   r=["Lst"], w=[PSR(hf)])
                pe(lambda tt, hf=hf: tt.matmul(ps[2 + hf][:], lhsT=onesq[:], rhs=Mb[:, hf * 512:(hf + 1) * 512], start=True, stop=True),
                   r=["onesq"], w=[PSR(2 + hf)])
                act(lambda a, hf=hf: a.activation(out=pre[:].rearrange("p t e -> p (t e)")[:, hf * 512:(hf + 1) * 512],
                                                  in_=ps[hf][:], func=AF.Copy), r=[PSR(hf)], w=[("pre", hf)])
                act(lambda a, hf=hf: a.activation(out=csb[:].rearrange("p t e -> p (t e)")[:, hf * 512:(hf + 1) * 512],
                                                  in_=ps[2 + hf][:], func=AF.Copy), r=[PSR(2 + hf)], w=[("csb", hf)])
            PRER = [("pre", hf) for hf in range(nh)]
            CSR = [("csb", hf) for hf in range(nh)]
            dve(lambda v: v.memset(off[:, 0, :], 0.0), w=["off"])
            for t in range(1, NTL):
                dve(lambda v, t=t: v.tensor_tensor(out=off[:, t, :], in0=off[:, t - 1, :], in1=csb[:, t - 1, :], op=ALU.add),
                    r=["off"] + CSR, w=["off"])
            dve(lambda v: v.tensor_tensor(out=cnt[:], in0=off[:, NTL - 1, :], in1=csb[:, NTL - 1, :], op=ALU.add),
                r=["off"] + CSR, w=["cnt"])
            nmx_b = NTOK // RB + 1
            assert nmx_b <= 16
            dve(lambda v: v.tensor_tensor(out=cmp8[:, :, 0:nmx_b], in0=_ap(cnt[:], [[E, 128], [1, E], [0, nmx_b]]),
                                          in1=_ap(bthr[:], [[nstep, 128], [0, E], [1, nmx_b]]), op=ALU.is_gt),
                r=["cnt", "bthr"], w=["cmp8"])
            dve(lambda v: v.tensor_reduce(out=pad[:], in_=cmp8[:, :, 0:nmx_b], axis=AX.X, op=ALU.add), r=["cmp8"], w=["pad"])
            dve(lambda v: v.tensor_scalar(out=pad[:], in0=pad[:], scalar1=float(RB), scalar2=None, op0=ALU.mult),
                r=["pad"], w=["pad"])
            dve(lambda v: v.memset(pst[:, 0:1], 0.0), w=["pst"])
            for e in range(1, E):
                dve(lambda v, e=e: v.tensor_tensor(out=pst[:, e:e + 1], in0=pst[:, e - 1:e], in1=pad[:, e - 1:e], op=ALU.add),
                    r=["pst", "pad"], w=["pst"])
            dve(lambda v: v.tensor_tensor(out=pend[:], in0=pst[:], in1=pad[:], op=ALU.add), r=["pst", "pad"], w=["pend"])
            dve(lambda v: v.tensor_tensor(out=pre[:], in0=pre[:], in1=off[:], op=ALU.add), r=PRER + ["off"], w=["dest"])
            dve(lambda v: v.tensor_tensor(out=pre[:], in0=pre[:], in1=_ap(pst[:], [[E, 128], [0, NTL], [1, E]]), op=ALU.add),
                r=["dest", "pst"], w=["dest"])
            dve(lambda v: v.tensor_tensor(out=big[:], in0=_ap(lg_all[:], [[NTL * E, 128], [E, NTL], [0, 4], [1, E]]),
                                          in1=_ap(mx8[:], [[NTL * 8, 128], [8, NTL], [1, 4], [0, E]]), op=ALU.is_equal),
                r=LGR + MXR, w=["big"])
            dve(lambda v: v.tensor_tensor(out=big[:], in0=big[:], in1=_ap(pre[:], [[NTL * E, 128], [E, NTL], [0, 4], [1, E]]),
                                          op=ALU.mult), r=["big", "dest"], w=["big"])
            dve(lambda v: v.tensor_reduce(out=slotf[:], in_=big[:], axis=AX.X, op=ALU.add), r=["big"], w=["slotf"])
            dve(lambda v: v.tensor_copy(out=sloti[:], in_=slotf[:].rearrange("p t k -> p (t k)")), r=["slotf"], w=["sloti"])
            dve(lambda v: v.tensor_tensor(out=cmpb[:], in0=_ap(pend[:], [[E, 128], [0, nstep], [1, E]]),
                                          in1=_ap(bthr[:], [[nstep, 128], [1, nstep], [0, E]]), op=ALU.is_le),
                r=["pend", "bthr"], w=["cmpb"])
            dve(lambda v: v.tensor_reduce(out=bef[:], in_=cmpb[:], axis=AX.X, op=ALU.add), r=["cmpb"], w=["bef"])
            dve(lambda v: v.tensor_scalar(out=bef[:], in0=bef[:], scalar1=float(E - 1), scalar2=None, op0=ALU.min),
                r=["bef"], w=["bef"])
            dve(lambda v: v.tensor_copy(out=bei[:], in_=bef[:]), r=["bef"], w=["bei"])
            dve(lambda v: v.tensor_scalar(out=ohT[:], in0=bef[:, :], scalar1=iota_p[:, 0:1], scalar2=None, op0=ALU.is_equal),
                r=["bef", "iota_p"], w=["ohT"])
            pool(lambda g: g.iota(kcp_i[:], pattern=[[128, 8]], base=0, channel_multiplier=1), w=["kcp_i"])
            dve(lambda v: v.tensor_copy(out=kcp[:], in_=kcp_i[:]), r=["kcp_i"], w=["kcp"])
            dve(lambda v: v.tensor_scalar(out=bef1k[:], in0=bef[:], scalar1=float(D), scalar2=None, op0=ALU.mult), r=["bef"], w=["bef1k"])
            dve(lambda v: v.tensor_scalar(out=inact[:], in0=bthr[:], scalar1=pend[:, E - 1:E], scalar2=None, op0=ALU.is_ge),
                r=["bthr", "pend"], w=["inact"])
            dve(lambda v: v.scalar_tensor_tensor(out=bef1k[:], in0=inact[:], scalar=1.0e6, in1=bef1k[:], op0=ALU.mult, op1=ALU.add),
                r=["inact", "bef1k"], w=["bef1k"])
            dve(lambda v: v.tensor_copy(out=ohTb[:], in_=ohT[0:E, :]), r=["ohT"], w=["ohTb"])
            for c16 in range(16):
                bk = 4 + c16 // 8
                pe(lambda tt, c16=c16, bk=bk: tt.matmul(ps[bk][:, (c16 % 8) * nstep:(c16 % 8 + 1) * nstep],
                                                        lhsT=bgu[:, c16 * 128:(c16 + 1) * 128], rhs=ohTb[:], start=True, stop=True),
                   r=["bgu", "ohTb"], w=[PSR(bk)])
            act(lambda a: a.activation(out=bsel[:, 0:8, :], in_=ps[4][:, 0:8 * nstep].rearrange("p (c b) -> p c b", c=8), func=AF.Copy),
                r=[PSR(4)], w=["bsel"])
            dve(lambda v: v.tensor_scalar(out=bsel[:, 8:16, :], in0=ps[5][:, 0:8 * nstep].rearrange("p (c b) -> p c b", c=8),
                                          scalar1=1.0, scalar2=None, op0=ALU.add), r=[PSR(5), "bsel"], w=["bsel"])
            dve(lambda v: v.tensor_tensor(out=widx_f[:], in0=_ap(bef1k[:], [[nstep, 128], [1, nstep], [0, 8]]),
                                          in1=_ap(kcp[:], [[8, 128], [0, nstep], [1, 8]]), op=ALU.add),
                r=["bef1k", "kcp"], w=["widx_f"])
            dve(lambda v: v.tensor_copy(out=widx[:], in_=widx_f[:].rearrange("p b c -> p (b c)")), r=["widx_f"], w=["widx"])
            if debug:
                dma("sp", dbg["lg"].ap(), lg_all[:], r=LGR, w=["dbg_lg"])
                dma("sp", dbg["slot"].ap(), slotf[:], r=["slotf"], w=["dbg_slot"])
                dma("sp", dbg["g4"].ap(), g4[:], r=G4R, w=["dbg_g4"])
                dma("sp", dbg["be"].ap(), bef[:], r=["bef"], w=["dbg_be"])

            P.barrier()
            P.emit()
            rt_scope.__exit__(None, None, None)
            G4R = [("g4", t) for t in range(NTL)]
            st_scope = ExitStack()
            st_scope.__enter__()
            if do_moe:
                bcreg = nc.gpsimd.alloc_register("bcreg")
                ohb = [sb("ohb%d" % i, [128, 128], BF16, st_scope) for i in range(2)]
                Wgu = [sb("Wgu%d" % i, [128, 8, 2 * D], BF16, st_scope) for i in range(2)]
                Wdn = [sb("Wdn%d" % i, [128, 8, D], BF16, st_scope) for i in range(2)]
                xr = [sb("xr%d" % i, [128, D], BF16, st_scope) for i in range(8)]
                xT = [sb("xT%d" % i, [128, 8, 512], BF16, st_scope) for i in range(2)]
                actT = sb("actT", [128, 8, 512], BF16, st_scope)
                gm = [sb("gm%d" % i, [128, 512], F32, st_scope) for i in range(2)]
                sg = [sb("sg%d" % i, [128, 512], F32, st_scope) for i in range(2)]
                uc = [sb("uc%d" % i, [128, 512], F32, st_scope) for i in range(2)]
                ost = [sb("ost%d" % i, [128, D], BF16, st_scope) for i in range(2)]
            if do_moe:
                H2_tiles = H2_d.ap().rearrange("(t p) d -> t p d", p=128)
                X1_tiles = X1_d.ap().rearrange("(t p) d -> t p d", p=128)
                out_tiles = out_d.ap().rearrange("(t p) d -> t p d", p=128)
                wgu2d = wgu_d.ap().rearrange("e k n -> (e k) n")
                wd2d = wd_d.ap().rearrange("e k n -> (e k) n")
                wreg = nc.gpsimd.alloc_register("wreg")

                def load_weights(bstep):
                    par = bstep % 2
                    for kc in range(8):
                        def fn(g, kc=kc):
                            if bstep == 0 and kc == 0:
                                g.reg_mov(wreg, E * D - 1)
                            return g.indirect_dma_start(
                                out=Wgu[par][:, kc, :], out_offset=None, in_=wgu2d,
                                in_offset=bass.IndirectOffsetOnAxis(ap=widx[:, bstep * 8 + kc:bstep * 8 + kc + 1], axis=0),
                                bounds_check=wreg, oob_is_err=False)
                        P.op("pool", fn, r=["widx"], w=[("Wgu", par, kc)], dma=True)
                    for kc in range(8):
                        P.op("pool", lambda g, kc=kc: g.indirect_dma_start(
                            out=Wdn[par][:, kc, :], out_offset=None, in_=wd2d,
                            in_offset=bass.IndirectOffsetOnAxis(ap=widx[:, bstep * 8 + kc:bstep * 8 + kc + 1], axis=0),
                            bounds_check=wreg, oob_is_err=False), r=["widx"], w=[("Wdn", par, kc)], dma=True)

                load_weights(0)
                for t in range(NTL):
                    sl = t % 4
                    dma("sp", xr[sl][:], H2_tiles[t], w=[("xr", 0, sl)])
                    for k in range(4):
                        def _scat(g, sl=sl, t=t, k=k):
                            if t == 0 and k == 0:
                                g.reg_mov(bcreg, NSLOT - 1)
                            return g.indirect_dma_start(
                                out=XB_d[:, :], out_offset=bass.IndirectOffsetOnAxis(ap=sloti[:, t * 4 + k:t * 4 + k + 1], axis=0),
                                in_=xr[sl][:], in_offset=None, bounds_check=bcreg, oob_is_err=False)
                        P.op("pool", _scat,
                            r=[("xr", 0, sl), "sloti"], w=[("XBs", t, k)], dma=True)
                XBS = [("XBs", t, k) for t in range(NTL) for k in range(4)]
                P.op("sp", None, r=XBS, w=["XBjoin"])

                def emit_xload(bs):
                    for rt in range(4):
                        dma("sp", xr[(bs % 2) * 4 + rt][:], XB_d[bs * RB + rt * 128: bs * RB + (rt + 1) * 128, :],
                            r=["XBjoin"], w=[("xr", bs % 2, rt)])

                def emit_xT(bs):
                    xTs = xT[bs % 2]
                    for rt in range(4):
                        bank = rt % 2
                        src_t = xr[(bs % 2) * 4 + rt]
                        for kc in range(8):
                            pe(lambda tt, kc=kc, src_t=src_t, bank=bank: tt.transpose(psb(bank)[:, kc * 128:(kc + 1) * 128],
                                                                                     src_t[:, kc * 128:(kc + 1) * 128], ident[:]),
                               r=[("xr", bs % 2, rt), "ident"], w=[PSR(bank)])
                        act(lambda a, bank=bank, rt=rt, xTs=xTs: a.activation(
                            out=xTs[:, :, rt * 128:(rt + 1) * 128], in_=psb(bank).rearrange("p (k t) -> p k t", k=8), func=AF.Copy),
                            r=[PSR(bank)], w=[("xT", bs % 2)])

                emit_xload(0)
                emit_xT(0)
                if nstep > 1:
                    emit_xload(1)
                for bstep in range(nstep):
                    par = bstep % 2
                    if bstep + 1 < nstep:
                        load_weights(bstep + 1)
                    WG = [("Wgu", par, q) for q in range(8)]
                    WD = [("Wdn", par, q) for q in range(8)]
                    xTs = xT[par]
                    dve(lambda v, bstep=bstep, par=par: v.tensor_copy(out=ohb[par][:], in_=_ap(ohT[:], [[nstep, 128], [0, 128]], off=bstep)),
                        r=["ohT"], w=[("ohb", par)])
                    for fc in range(8):
                        bg_, bu_ = 2 + 2 * (fc % 2), 3 + 2 * (fc % 2)
                        for (bank, col0) in ((bg_, fc * 128), (bu_, D + fc * 128)):
                            for kc in range(8):
                                pe(lambda tt, kc=kc, bank=bank, col0=col0, par=par, xTs=xTs: tt.matmul(
                                    ps[bank][:], lhsT=Wgu[par][:, kc, col0:col0 + 128], rhs=xTs[:, kc, :],
                                    start=(kc == 0), stop=(kc == 7)), r=WG + [("xT", par)], w=[PSR(bank)])
                        s2 = fc % 2
                        dve(lambda v, bg_=bg_, s2=s2, fc=fc, bstep=bstep: v.tensor_scalar(
                            out=gm[s2][:], in0=ps[bg_][:], scalar1=bsel[:, fc, bstep:bstep + 1], scalar2=7.0, op0=ALU.add, op1=ALU.min),
                            r=[PSR(bg_), "bsel"], w=[("gm", s2)])
                        act(lambda a, s2=s2: a.activation(out=sg[s2][:], in_=gm[s2][:], func=AF.Sigmoid, scale=1.702),
                            r=[("gm", s2)], w=[("sg", s2)])
                        dve(lambda v, bu_=bu_, s2=s2, fc=fc, bstep=bstep: v.tensor_scalar(
                            out=uc[s2][:], in0=ps[bu_][:], scalar1=bsel[:, 8 + fc, bstep:bstep + 1], scalar2=8.0, op0=ALU.add, op1=ALU.min),
                            r=[PSR(bu_), "bsel"], w=[("uc", s2)])
                        dve(lambda v, s2=s2: v.tensor_tensor(out=gm[s2][:], in0=gm[s2][:], in1=sg[s2][:], op=ALU.mult),
                            r=[("gm", s2), ("sg", s2)], w=[("gm", s2)])
                        dve(lambda v, s2=s2, fc=fc: v.scalar_tensor_tensor(out=actT[:, fc, :], in0=uc[s2][:], scalar=-6.0, in1=gm[s2][:],
                                                                           op0=ALU.max, op1=ALU.mult),
                            r=[("uc", s2), ("gm", s2)], w=[("actT", fc)])
                    if bstep + 1 < nstep:
                        emit_xT(bstep + 1)
                    if bstep + 2 < nstep:
                        emit_xload(bstep + 2)
                    ACTR = [("actT", fc) for fc in range(8)]
                    for rt in range(4):
                        o_ = ost[rt % 2]
                        for half in range(2):
                            bank = ((6, 7), (0, 1))[rt % 2][half]
                            for fc in range(8):
                                pe(lambda tt, fc=fc, rt=rt, half=half, bank=bank, par=par: tt.matmul(
                                    ps[bank][:], lhsT=actT[:, fc, rt * 128:(rt + 1) * 128],
                                    rhs=Wdn[par][:, fc, half * 512:(half + 1) * 512], start=(fc == 0), stop=False),
                                   r=ACTR + WD, w=[PSR(bank)])
                            pe(lambda tt, half=half, bank=bank, par=par: tt.matmul(
                                ps[bank][:], lhsT=ohb[par][:, 0:128], rhs=bdn[:, half * 512:(half + 1) * 512], start=False, stop=True),
                               r=["bdn", ("ohb", par)], w=[PSR(bank)])
                            if False:
                                pass
                            else:
                                dve(lambda v, half=half, bank=bank, o_=o_: v.tensor_copy(out=o_[:, half * 512:(half + 1) * 512], in_=ps[bank][:]),
                                    r=[PSR(bank)], w=[("ost", rt % 2, half)])
                        dma("sp", OB_d[bstep * RB + rt * 128: bstep * RB + (rt + 1) * 128, :], o_[:],
                            r=[("ost", rt % 2, 0), ("ost", rt % 2, 1)], w=[("OB", bstep, rt)])
                OBR = [("OB", bs_, rt) for bs_ in range(nstep) for rt in range(4)]
                P.op("sp", None, r=OBR, w=["OBjoin"])
                P.barrier()
                P.emit()
                st_scope.__exit__(None, None, None)
                st_scope = ExitStack()
                st_scope.__enter__()
                NG = 3
                gr = [[sb("gr%d_%d" % (j_, i), [128, D], BF16, st_scope) for i in range(4)] for j_ in range(NG)]
                acc = [sb("acc%d" % i, [128, D], F32, st_scope) for i in range(2)]
                x1r = [sb("x1r%d" % i, [128, D], F32, st_scope) for i in range(3)]
                G2 = sb("G2", [128, nseq, D], F32, st_scope)
                for b_ in range(nseq):
                    dma("sp", G2[:, b_, :], MOD_d[b_, 5], w=[("G2", b_, 0), ("G2", b_, 1)])
                for t in range(NTL):
                    b = t // 16
                    sl = t % 3
                    gs = t % NG
                    ac = acc[t % 2]
                    acr = ("acc", t % 2)
                    dma("sp", x1r[sl][:], X1_tiles[t], w=[("x1r", sl)])
                    for k in range(4):
                        def _gath(g, t=t, k=k, gs=gs):
                            if t == 0 and k == 0:
                                g.reg_mov(bcreg, NSLOT - 1)
                            return g.indirect_dma_start(
                                out=gr[gs][k][:], out_offset=None, in_=OB_d[:, :],
                                in_offset=bass.IndirectOffsetOnAxis(ap=sloti[:, t * 4 + k:t * 4 + k + 1], axis=0),
                                bounds_check=bcreg, oob_is_err=False)
                        P.op("pool", _gath, r=["OBjoin", "sloti"], w=[("gr", gs, k)], dma=True)
                    act(lambda a, t=t, gs=gs, ac=ac: a.activation(out=ac[:], in_=gr[gs][0][:], func=AF.Copy, scale=g4[:, t, 0:1]),
                        r=[("gr", gs, 0)] + G4R, w=[acr])
                    for k in range(1, 4):
                        dve(lambda v, t=t, k=k, gs=gs, ac=ac: v.scalar_tensor_tensor(out=ac[:], in0=gr[gs][k][:], scalar=g4[:, t, k:k + 1], in1=ac[:],
                                                                                    op0=ALU.mult, op1=ALU.add), r=[("gr", gs, k), acr] + G4R, w=[acr])
                    dve(lambda g, b=b, ac=ac: g.tensor_tensor(out=ac[:], in0=ac[:], in1=G2[:, b, :], op=ALU.mult),
                        r=[acr, ("G2", b, 0), ("G2", b, 1)], w=[acr])
                    dve(lambda g, sl=sl, ac=ac: g.tensor_tensor(out=x1r[sl][:], in0=ac[:], in1=x1r[sl][:], op=ALU.add),
                        r=[acr, ("x1r", sl)], w=[("x1r", sl), acr])
                    dma("sp", out_tiles[t], x1r[sl][:], r=[("x1r", sl)], w=[("out", t)])
            else:
                x1r = [sb("x1r%d" % i, [128, D], F32, st_scope) for i in range(2)]
                X1_tiles = X1_d.ap().rearrange("(t p) d -> t p d", p=128)
                out_tiles = out_d.ap().rearrange("(t p) d -> t p d", p=128)
                for t in range(NTL):
                    sl = t % 2
                    dma("sp", x1r[sl][:], X1_tiles[t], w=[("x1r", sl)])
                    dma("sp", out_tiles[t], x1r[sl][:], r=[("x1r", sl)], w=[("out", t)])
            P.barrier()
            P.emit()
            st_scope.__exit__(None, None, None)
    return nc


def make_in_maps(inputs, ncores=NCORES, nseq=NSEQ):
    f = lambda a: np.ascontiguousarray(np.asarray(a, dtype=np.float32))
    x = f(inputs["x"])
    c = f(inputs["c"])
    shared = {
        "w_ada": f(inputs["w_ada"][0]),
        "b_ada": f(inputs["b_ada"][0]).reshape(1, -1),
        "norm1_g": f(inputs["norm1_g"][0]).reshape(1, -1),
        "norm2_g": f(inputs["norm2_g"][0]).reshape(1, -1),
        "w_in": f(inputs["w_in"][0]),
        "gq": f(np.tile(np.asarray(inputs["q_norm_g"][0]), 2).reshape(128, 1)),
        "gk": f(np.tile(np.asarray(inputs["k_norm_g"][0]), 2).reshape(128, 1)),
        "lamv": f(np.stack([inputs["lambda_q1"][0], inputs["lambda_q2"][0], inputs["lambda_k1"][0], inputs["lambda_k2"][0]])),
        "subln_g": f(inputs["subln_g"][0]).reshape(1, -1),
        "conv_wT": f(np.asarray(inputs["conv_w"][0]).reshape(31, 4, 128).transpose(2, 1, 0)),
        "conv_b": f(np.asarray(inputs["conv_b"][0]).reshape(4, 128).T),
        "conv_ln_g": f(np.asarray(inputs["conv_ln_g"][0]).reshape(4, 128).T),
        "conv_ln_b": f(np.asarray(inputs["conv_ln_b"][0]).reshape(4, 128).T),
        "w_out": f(inputs["w_out"][0]),
        "w_router": f(inputs["w_router"][0]),
        "b_router": f(inputs["b_router"][0]).reshape(1, -1),
        "w_gate_up": f(inputs["w_gate_up"][0]),
        "b_gate_up": f(inputs["b_gate_up"][0]),
        "w_down": f(inputs["w_down"][0]),
        "b_down": f(inputs["b_down"][0]),
    }
    maps = []
    for i in range(ncores):
        m = dict(shared)
        m["x"] = np.ascontiguousarray(x[i * nseq:(i + 1) * nseq].reshape(nseq * S, D))
        cc = c[i * nseq:(i + 1) * nseq]
        m["cT"] = np.ascontiguousarray(cc.reshape(nseq, 8, 128).transpose(2, 1, 0))
        maps.append(m)
    return maps


_NC_CACHE = {}


def kernel(**inputs):
    if "nc" not in _NC_CACHE:
        _NC_CACHE["nc"] = build_program()
    nc = _NC_CACHE["nc"]
    maps = make_in_maps(inputs)
    res = run_bass_kernel_spmd(nc, maps, core_ids=list(range(NCORES)))
    outs = [np.asarray(r["out"]).reshape(NSEQ, S, D) for r in res.results]
    return np.concatenate(outs, axis=0).astype(np.float32)
```

```python
import math
from contextlib import ExitStack
import numpy as np
import concourse.bass as bass
import concourse.mybir as mybir
from concourse.bass_utils import run_bass_kernel_spmd

F32 = mybir.dt.float32
BF16 = mybir.dt.bfloat16
I32 = mybir.dt.int32
AF = mybir.ActivationFunctionType
ALU = mybir.AluOpType
AX = mybir.AxisListType

NCORES = 8
D = 1024
S = 2048
NSEQ = 2
NT = NSEQ * S // 128
E = 32
RB = 512
NSTEP = (NT * 128 * 4) // RB + E
EPS = 1e-5
LAM_INIT = 0.8 - 0.6 * math.exp(0.0)
NDQ = 12


class _Op:
    __slots__ = ("eng", "fn", "dma", "deps", "milestone", "sem", "val", "know")


class Prog:
    ENGS = ("pe", "act", "dve", "pool", "sp")

    def __init__(self, nc, es):
        self.nc = nc
        self.ops = []
        self.emitted = 0
        self.last_w = {}
        self.readers = {}
        self.esem = {e: es.enter_context(nc.semaphore("tl_" + e)) for e in self.ENGS}
        self.dsem = {e: [es.enter_context(nc.semaphore("dq_%s%d" % (e, i))) for i in range(NDQ)]
                     for e in ("sp", "act", "pool")}
        self.ecount = {e: 0 for e in self.ENGS}
        self.dcount = {e: 0 for e in self.dsem}
        self.dhist = {e: [] for e in self.dsem}
        self.know = {e: {} for e in self.ENGS}
        self.live_dma = []
        self.last_real = {}

    def op(self, eng, fn, r=(), w=(), dma=False, extra=()):
        o = _Op()
        o.eng, o.fn, o.dma = eng, fn, dma
        deps = set(extra)
        for x in r:
            if x in self.last_w:
                deps.add(self.last_w[x])
        for x in w:
            if x in self.last_w:
                deps.add(self.last_w[x])
            for rd in self.readers.get(x, ()):
                deps.add(rd)
        idx = len(self.ops)
        o.deps = deps
        o.milestone = False
        o.know = None
        self.ops.append(o)
        for x in r:
            self.readers.setdefault(x, []).append(idx)
        for x in w:
            self.last_w[x] = idx
            self.readers[x] = []
        if dma:
            self.live_dma.append(idx)
        elif fn is not None:
            self.last_real[eng] = idx
        return idx

    def barrier(self):
        firsts = []
        for e in self.ENGS:
            ex = list(self.live_dma) if e == "sp" else []
            if e in self.last_real:
                ex.append(self.last_real[e])
            firsts.append(self.op(e, None, w=[("bar", e)], extra=ex))
        self.live_dma = []
        self.last_real = {}
        for e in self.ENGS:
            self.op(e, None, r=[("bar", x) for x in self.ENGS], w=[("bar2", e)])
        self.last_w = {k: v for k, v in self.last_w.items() if k[0] == "bar2"}
        self.readers = {}

    def emit(self):
        nc = self.nc
        ops = self.ops
        start = self.emitted
        for o in ops[start:]:
            for d in o.deps:
                ops[d].milestone = True
        plan = {e: [] for e in self.ENGS}
        for i in range(start, len(ops)):
            o = ops[i]
            e = o.eng
            know = self.know[e]
            waits = []
            if o.dma:
                j = self.dcount[e]
                self.dcount[e] += 1
                o.sem = self.dsem[e][j % NDQ]
                o.val = 16 * (j // NDQ + 1)
                if j >= NDQ:
                    o.deps.add(self.dhist[e][j - NDQ])
                self.dhist[e].append(i)
            for d in sorted(o.deps):
                p = ops[d]
                if (not p.dma) and p.eng == "pe" and e == "pe" and not o.dma and o.fn is not None:
                    continue
                assert p.sem is not None, "dependency on op that was never made a milestone"
                key = id(p.sem)
                if know.get(key, (None, 0))[1] >= p.val:
                    continue
                waits.append((p.sem, p.val))
                if p.know:
                    for k2, v2 in p.know.items():
                        if know.get(k2, (None, 0))[1] < v2[1]:
                            know[k2] = v2
                know[key] = (p.sem, p.val)
            if o.dma:
                o.know = dict(know)
            elif o.milestone or o.fn is None:
                self.ecount[e] += 1
                o.sem = self.esem[e]
                o.val = self.ecount[e]
                o.milestone = True
                snap = dict(know)
                snap[id(o.sem)] = (o.sem, o.val)
                o.know = snap
            else:
                o.sem = None
                o.val = 0
            plan[e].append((o, waits))
        self.emitted = len(ops)
        attr = {"pe": "tensor", "act": "scalar", "dve": "vector", "pool": "gpsimd", "sp": "sync"}

        def run(engname, eng):
            for o, waits in plan[engname]:
                best = {}
                for sem, val in waits:
                    k = id(sem)
                    if k not in best or best[k][1] < val:
                        best[k] = (sem, val)
                for sem, val in best.values():
                    eng.wait_ge(sem, val)
                if o.fn is None:
                    eng.sem_inc(o.sem, 1)
                    continue
                ins = o.fn(eng)
                if o.dma:
                    ins.then_inc(o.sem, 16)
                elif o.milestone:
                    ins.then_inc(o.sem, 1)

        with nc.Block() as block:
            for engname in self.ENGS:
                if not plan[engname]:
                    continue
                deco = getattr(block, attr[engname])

                def body(eng, engname=engname):
                    run(engname, eng)
                deco(body)


def _ap(base, dims, off=0):
    return bass.AP(tensor=base.tensor, offset=base.offset + off, ap=[list(d) for d in dims])


def build_program(debug=False, nseq=NSEQ, do_moe=True, stop=None):
    nc = bass.Bass("TRN2", target_bir_lowering=False)
    NTOK = nseq * S
    NTL = NTOK // 128
    nstep = (NTOK * 4) // RB + E
    NSLOT = nstep * RB

    def din(name, shape, dt=F32):
        return nc.dram_tensor(name, list(shape), dt, kind="ExternalInput")

    x_d = din("x", [NTOK, D])
    cT_d = din("cT", [128, 8, nseq])
    wada_d = din("w_ada", [D, 6 * D])
    bada_d = din("b_ada", [1, 6 * D])
    n1g_d = din("norm1_g", [1, D])
    n2g_d = din("norm2_g", [1, D])
    win_d = din("w_in", [D, 2560])
    gq_d = din("gq", [128, 1])
    gk_d = din("gk", [128, 1])
    lamv_d = din("lamv", [4, 64])
    subg_d = din("subln_g", [1, 128])
    cw_d = din("conv_wT", [128, 4, 31])
    cb_d = din("conv_b", [128, 4])
    clg_d = din("conv_ln_g", [128, 4])
    clb_d = din("conv_ln_b", [128, 4])
    wout_d = din("w_out", [D, D])
    wr_d = din("w_router", [D, E])
    br_d = din("b_router", [1, E])
    wgu_d = din("w_gate_up", [E, D, 2 * D])
    bgu_d = din("b_gate_up", [E, 2 * D])
    wd_d = din("w_down", [E, D, D])
    bd_d = din("b_down", [E, D])
    out_d = nc.dram_tensor("out", [NTOK, D], F32, kind="ExternalOutput")
    X1_d = nc.dram_tensor("X1s", [NTOK, D], F32)
    H2_d = nc.dram_tensor("H2s", [NTOK, D], BF16)
    XB_d = nc.dram_tensor("XBs", [NSLOT, D], BF16)
    OB_d = nc.dram_tensor("OBs", [NSLOT, D], BF16)
    dbg = {}
    if debug:
        dbg["x1"] = nc.dram_tensor("dbg_x1", [NTOK, D], F32, kind="ExternalOutput")
        dbg["lg"] = nc.dram_tensor("dbg_lg", [128, NTL, E], F32, kind="ExternalOutput")
        dbg["slot"] = nc.dram_tensor("dbg_slot", [128, NTL, 4], F32, kind="ExternalOutput")
        dbg["g4"] = nc.dram_tensor("dbg_g4", [128, NTL, 4], F32, kind="ExternalOutput")
        dbg["be"] = nc.dram_tensor("dbg_be", [128, nstep], F32, kind="ExternalOutput")

    es = ExitStack()
    with es:
        P = Prog(nc, es)

        _cnt = [0]

        def sb(name, shape, dt, stack=es):
            _cnt[0] += 1
            return stack.enter_context(nc.sbuf_tensor("s%d_%s" % (_cnt[0], name), list(shape), dt))

        ps = [es.enter_context(nc.psum_tensor("ps%d" % i, [128, 512], F32)) for i in range(8)]

        def PSR(i):
            return ("ps", i)

        def psb(i):
            return ps[i][:].bitcast(BF16)

        ident = sb("ident", [128, 128], BF16)
        ones_bf = sb("ones_bf", [128, 512], BF16)
        zero_bf = sb("zero_bf", [128, 512], BF16)
        lg_all = sb("lg_all", [128, NTL, E], F32)
        mx8 = sb("mx8", [128, NTL, 8], F32)
        Mb = sb("Mb", [128, NTL * E], BF16)
        g4 = sb("g4", [128, NTL, 4], F32)
        nmx = sb("nmx", [128, NTL], F32)
        gsm = sb("gsm", [128, NTL], F32)
        iota_p = sb("iota_p", [128, 1], F32)
        iota_f = sb("iota_f", [128, 128], F32)

        def dve(fn, r=(), w=()):
            return P.op("dve", fn, r, w)

        def act(fn, r=(), w=()):
            return P.op("act", fn, r, w)

        def pool(fn, r=(), w=()):
            return P.op("pool", fn, r, w)

        def pe(fn, r=(), w=()):
            return P.op("pe", fn, r, w)

        def dma(eng, out, in_, r=(), w=(), **kw):
            return P.op(eng, lambda q: q.dma_start(out=out, in_=in_, **kw), r, w, dma=True)

        zf_state = [0]
        ZF_TOTAL = (NSLOT // 128) * 2

        def zero_fill(n):
            if not do_moe:
                return
            for _ in range(n):
                i = zf_state[0]
                if i >= ZF_TOTAL:
                    return
                zf_state[0] += 1
                r0, hf = (i // 2) * 128, i % 2
                dma("pool", XB_d[r0:r0 + 128, hf * 512:(hf + 1) * 512], zero_bf[:], r=["zero_bf"], w=[("XBz", i)])

        MOD_d = nc.dram_tensor("MODs", [nseq, 6, 128, D], F32)
        x_tiles = x_d.ap().rearrange("(t p) d -> t p d", p=128)
        X1_tiles = X1_d.ap().rearrange("(t p) d -> t p d", p=128)
        H2_tiles = H2_d.ap().rearrange("(t p) d -> t p d", p=128)
        slopes = [2.0 ** (-8.0 * (h + 1) / 4) for h in range(4)]

        with ExitStack() as cs:
            it_i = sb("it_i", [128, 128], I32, cs)
            ip_i = sb("ip_i", [128, 1], I32, cs)
            cT = sb("cT", [128, 8, nseq], F32, cs)
            scT = sb("scT", [128, 8, nseq], F32, cs)
            bcl2 = [sb("bcl%d" % i, [128, 8, 128], BF16, cs) for i in range(nseq)]
            wa_st = [sb("wa_st%d" % i, [128, 8, 512], BF16, cs) for i in range(6)]
            bada_b = [sb("bada_b%d" % i, [128, 512], F32, cs) for i in range(2)]
            g1b = sb("g1b", [128, D], F32, cs)
            g2b = sb("g2b", [128, D], F32, cs)
            modt2 = [[sb("modt%d_%d" % (b_i, i), [128, D], F32, cs) for i in range(6)] for b_i in range(nseq)]
            pool(lambda g: g.iota(it_i[:], pattern=[[1, 128]], base=0, channel_multiplier=0), w=["it_i"])
            pool(lambda g: g.iota(ip_i[:], pattern=[[1, 1]], base=0, channel_multiplier=1), w=["ip_i"])
            dve(lambda v: v.tensor_copy(out=iota_f[:], in_=it_i[:]), r=["it_i"], w=["iota_f"])
            dve(lambda v: v.tensor_copy(out=iota_p[:], in_=ip_i[:]), r=["ip_i"], w=["iota_p"])
            dve(lambda v: v.tensor_scalar(out=ident[:], in0=iota_f[:], scalar1=iota_p[:, 0:1], scalar2=None,
                                          op0=ALU.is_equal), r=["iota_f", "iota_p"], w=["ident"])
            dve(lambda v: v.memset(ones_bf[:], 1.0), w=["ones_bf"])
            dve(lambda v: v.memset(zero_bf[:], 0.0), w=["zero_bf"])
            dma("sp", cT[:], cT_d.ap(), w=["cT"])
            dma("sp", g1b[:], _ap(n1g_d.ap(), [[0, 128], [1, D]]), w=["g1b"])
            dma("sp", g2b[:], _ap(n2g_d.ap(), [[0, 128], [1, D]]), w=["g2b"])
            act(lambda a: a.activation(out=scT[:], in_=cT[:], func=AF.Silu), r=["cT"], w=["scT"])
            for b in range(nseq):
                dve(lambda v, b=b: v.tensor_copy(out=bcl2[b][:], in_=_ap(scT[:], [[8 * nseq, 128], [nseq, 8], [0, 128]], off=b)),
                    r=["scT"], w=[("bcl", b)])
            for cc in range(12):
                st = wa_st[cc % 6]
                dma("pool", st[:], wada_d.ap().rearrange("(kc p) n -> p kc n", p=128)[:, :, cc * 512:(cc + 1) * 512],
                    w=[("wa_st", cc % 6)])
                dma("sp", bada_b[cc % 2][:], _ap(bada_d.ap(), [[0, 128], [1, 512]], off=cc * 512), w=[("bada_b", cc % 2)])
                which, half = cc // 2, cc % 2
                for b in range(nseq):
                    bank = (cc * nseq + b) % 4
                    for kc in range(8):
                        pe(lambda t, kc=kc, st=st, bank=bank, b=b: t.matmul(ps[bank][:], lhsT=bcl2[b][:, kc, :], rhs=st[:, kc, :],
                                                                             start=(kc == 0), stop=(kc == 7)),
                           r=[("bcl", b), ("wa_st", cc % 6)], w=[PSR(bank)])
                    dve(lambda v, bank=bank, which=which, half=half, b=b, cc=cc: v.tensor_tensor(
                        out=modt2[b][which][:, half * 512:(half + 1) * 512], in0=ps[bank][:], in1=bada_b[cc % 2][:], op=ALU.add),
                        r=[PSR(bank), ("bada_b", cc % 2)], w=[("modt", b, which)])
            for b in range(nseq):
                for (which, gx, gname) in ((1, g1b, "g1b"), (4, g2b, "g2b")):
                    dve(lambda v, which=which, gx=gx, b=b: v.scalar_tensor_tensor(out=modt2[b][which][:], in0=modt2[b][which][:], scalar=1.0,
                                                                                 in1=gx[:], op0=ALU.add, op1=ALU.mult),
                        r=[("modt", b, which), gname], w=[("modt", b, which)])
                for which in range(6):
                    dma("sp", MOD_d[b, which], modt2[b][which][:], r=[("modt", b, which)], w=[("MODd", b, which)])
            if stop == "ada" and debug:
                dma("sp", dbg["x1"].ap()[0:128, :], modt2[0][1][:], r=[("modt", 0, 1)], w=["dbgada"])
            P.barrier()
            P.emit()
            if stop == "ada":
                return nc

        with ExitStack() as pa:
            W_r = sb("W_r", [128, 8, E], BF16, pa)
            brb = sb("brb", [128, E], F32, pa)
            gq = sb("gq", [128, 1], F32, pa)
            gk = sb("gk", [128, 1], F32, pa)
            lams = sb("lams", [128, 2], F32, pa)
            neglam = sb("neglam", [128, 1], F32, pa)
            cw = sb("cw", [128, 4, 31], F32, pa)
            cwb = sb("cwb", [128, 4, 31], BF16, pa)
            cb = sb("cb", [128, 4], F32, pa)
            clg = sb("clg", [128, 4], F32, pa)
            clb = sb("clb", [128, 4], F32, pa)
            blk1 = sb("blk1", [128, 128], BF16, pa)
            om512 = sb("om512", [128, 128], BF16, pa)
            eps_c = sb("eps_c", [128, 1], F32, pa)
            hbuf = sb("hbuf", [128, 4, S + 32], BF16, pa)
            qT = sb("qT", [128, 4, S], BF16, pa)
            kT = sb("kT", [128, 4, S], BF16, pa)
            vS = sb("vS", [128, 16, 4, 128], BF16, pa)
            xs = [sb("xs%d" % i, [128, D], F32, pa) for i in range(2)]
            tmp1 = sb("tmp1", [128, D], F32, pa)
            htok = [sb("htok%d" % i, [128, D], BF16, pa) for i in range(2)]
            ss = sb("ss", [128, 4], F32, pa)
            sig = [sb("sig%d" % i, [128, 512], BF16, pa) for i in range(2)]
            sq = [sb("sq%d" % i, [128, 512], BF16, pa) for i in range(2)]
            rs = [sb("rs%d" % i, [128, 512], F32, pa) for i in range(2)]
            Eb = [sb("Eb%d" % i, [128, 512], BF16, pa) for i in range(3)]
            Pt = [sb("Pt%d" % i, [128, 512], BF16, pa) for i in range(5)]
            qz = [[sb("qz%d%d" % (i, c_), [128, 512], BF16, pa) for c_ in range(2)] for i in range(2)]
            oT = [sb("oT%d" % i, [128, 512], F32, pa) for i in range(2)]
            om128 = sb("om128", [128, 128], BF16, pa)
            subgc = sb("subgc", [128, 1], F32, pa)
            sm = sb("sm", [128, 8], F32, pa)
            tmp_scope = ExitStack()
            tmp_scope.__enter__()
            pge = sb("pge", [128, 1], F32, tmp_scope)
            lamb = sb("lamb", [128, 4, 64], F32, tmp_scope)
            lamt = sb("lamt", [128, 2, 64], F32, tmp_scope)
            o1 = sb("o1", [128, 128], F32, tmp_scope)

            dma("pool", W_r[:], wr_d.ap().rearrange("(kc p) n -> p kc n", p=128), w=["W_r"])
            dma("sp", brb[:], _ap(br_d.ap(), [[0, 128], [1, E]]), w=["brb"])
            dma("sp", gq[:], gq_d.ap(), w=["gq"])
            dma("sp", gk[:], gk_d.ap(), w=["gk"])
            dma("sp", lamb[:], _ap(lamv_d.ap(), [[0, 128], [64, 4], [1, 64]]), w=["lamb"])
            dma("sp", cw[:], cw_d.ap(), w=["cw"])
            dma("sp", cb[:], cb_d.ap(), w=["cb"])
            dma("sp", clg[:], clg_d.ap(), w=["clg"])
            dma("sp", clb[:], clb_d.ap(), w=["clb"])
            dve(lambda v: v.tensor_scalar(out=gq[:], in0=gq[:], scalar1=0.125, scalar2=None, op0=ALU.mult),
                r=["gq"], w=["gq"])
            dve(lambda v: v.tensor_tensor(out=lamt[:], in0=lamb[:, 0:2, :], in1=lamb[:, 2:4, :], op=ALU.mult),
                r=["lamb"], w=["lamt"])
            dve(lambda v: v.tensor_reduce(out=lams[:], in_=lamt[:], axis=AX.X, op=ALU.add), r=["lamt"], w=["lams"])
            act(lambda a: a.activation(out=lams[:], in_=lams[:], func=AF.Exp), r=["lams"], w=["lams"])
            dve(lambda v: v.tensor_tensor(out=neglam[:], in0=lams[:, 1:2], in1=lams[:, 0:1], op=ALU.subtract),
                r=["lams"], w=["neglam"])
            dve(lambda v: v.tensor_scalar(out=neglam[:], in0=neglam[:], scalar1=-LAM_INIT, scalar2=None, op0=ALU.add),
                r=["neglam"], w=["neglam"])
            dma("sp", subgc[:], _ap(subg_d.ap(), [[1, 128], [1, 1]]), w=["subgc"])
            dve(lambda v: v.tensor_scalar(out=subgc[:], in0=subgc[:], scalar1=(1.0 - LAM_INIT), scalar2=None,
                                          op0=ALU.mult), r=["subgc"], w=["subgc"])
            dve(lambda v: v.memset(om128[:], 1.0 / 128), w=["om128"])
            dve(lambda v: v.tensor_copy(out=cwb[:], in_=cw[:]), r=["cw"], w=["cwb"])
            for i_ in range(2):
                for c_ in range(2):
                    dve(lambda v, i_=i_, c_=c_: v.memset(qz[i_][c_][:], 0.0), w=[("qz", i_)])
            dve(lambda v: v.tensor_scalar(out=o1[:], in0=iota_f[:], scalar1=64.0, scalar2=None, op0=ALU.is_ge),
                r=["iota_f"], w=["o1"])
            dve(lambda v: v.tensor_scalar(out=pge[:], in0=iota_p[:], scalar1=64.0, scalar2=None, op0=ALU.is_ge),
                r=["iota_p"], w=["pge"])
            dve(lambda v: v.tensor_scalar(out=blk1[:], in0=o1[:], scalar1=pge[:, 0:1], scalar2=1.0 / 64,
                                          op0=ALU.is_equal, op1=ALU.mult), r=["o1", "pge"], w=["blk1"])
            dve(lambda v: v.memset(om512[:], 1.0 / 512), w=["om512"])
            dve(lambda v: v.memset(eps_c[:], EPS), w=["eps_c"])
            dve(lambda v: v.memset(hbuf[:], 0.0), w=[("hbuf", i) for i in range(4)])
            dve(lambda v: v.memset(vS[:], 1.0), w=[("vS", t) for t in range(16)])
            P.barrier()
            P.emit()
            tmp_scope.__exit__(None, None, None)
            if stop == "consts":
                return nc

            for b in range(nseq):
                seq_scope = ExitStack()
                seq_scope.__enter__()
                W_out = sb("W_out", [128, 8, D], BF16, seq_scope)
                G1 = sb("G1", [128, D], F32, seq_scope)
                dma("pool", W_out[:], wout_d.ap().rearrange("(kc p) n -> p kc n", p=128), w=["W_out"])
                dma("sp", G1[:], MOD_d[b, 2], w=["G1"])
                with ExitStack() as p1:
                    W_in = sb("W_in", [128, 8, 2560], BF16, p1)
                    hT2 = [sb("hT%d" % i_, [128, 8, 512], BF16, p1) for i_ in range(2)]
                    S1 = sb("S1", [128, D], F32, p1)
                    H1 = sb("H1", [128, D], F32, p1)
                    dma("pool", W_in[:], win_d.ap().rearrange("(kc p) n -> p kc n", p=128), w=["W_in"])
                    dma("sp", H1[:], MOD_d[b, 0], w=["H1"])
                    dma("sp", S1[:], MOD_d[b, 1], w=["S1"])
                    def chain(w_, tl):
                        t = b * 16 + w_ * 4 + tl
                        sl = t % 2
                        zero_fill(-(-ZF_TOTAL // (2 * nseq * 16)))
                        dma("sp", xs[sl][:], x_tiles[t], w=[("xs", sl)])
                        act(lambda a, sl=sl: a.activation(out=htok[sl][:], in_=xs[sl][:], func=AF.Square, scale=D ** -0.5,
                                                          accum_out=ss[:, 0:1]),
                            r=[("xs", sl)], w=[("htok", sl), ("ss", 0)])
                        act(lambda a: a.activation(out=ss[:, 1:2], in_=ss[:, 0:1], func=AF.Ln, bias=eps_c[:, 0:1]),
                            r=[("ss", 0), "eps_c"], w=[("ss", 1)])
                        act(lambda a: a.activation(out=ss[:, 1:2], in_=ss[:, 1:2], func=AF.Exp, scale=-0.5),
                            r=[("ss", 1)], w=[("ss", 1)])
                        dve(lambda v, sl=sl: v.scalar_tensor_tensor(out=tmp1[:], in0=xs[sl][:], scalar=ss[:, 1:2], in1=S1[:],
                                                                    op0=ALU.mult, op1=ALU.mult),
                            r=[("xs", sl), ("ss", 1), "S1"], w=["tmp1"])
                        dve(lambda g, sl=sl: g.tensor_tensor(out=htok[sl][:], in0=tmp1[:], in1=H1[:], op=ALU.add),
                            r=["tmp1", "H1"], w=[("htok", sl)])

                    def trans(w_, tl):
                        t = b * 16 + w_ * 4 + tl
                        sl = t % 2
                        bank = tl % 2
                        hTw = hT2[w_ % 2]
                        for kc in range(8):
                            pe(lambda tt, kc=kc, sl=sl, bank=bank: tt.transpose(psb(bank)[:, kc * 128:(kc + 1) * 128],
                                                                               htok[sl][:, kc * 128:(kc + 1) * 128], ident[:]),
                               r=[("htok", sl), "ident"], w=[PSR(bank)])
                        act(lambda a, bank=bank, tl=tl, hTw=hTw: a.activation(
                            out=hTw[:, :, tl * 128:(tl + 1) * 128],
                            in_=psb(bank).rearrange("p (k t) -> p k t", k=8), func=AF.Copy),
                            r=[PSR(bank)], w=[("hT", w_ % 2)])

                    def glu_chunk(w_, i):
                        hTw = hT2[w_ % 2]
                        hres = ("hT", w_ % 2)
                        tok0 = w_ * 512
                        ba, bg = 2 + 2 * (i % 2), 3 + 2 * (i % 2)
                        for (bank, oc) in ((ba, i), (bg, 4 + i)):
                            for kc in range(8):
                                pe(lambda tt, kc=kc, bank=bank, oc=oc, hTw=hTw: tt.matmul(
                                    ps[bank][:], lhsT=W_in[:, kc, oc * 128:(oc + 1) * 128], rhs=hTw[:, kc, :],
                                    start=(kc == 0), stop=(kc == 7)), r=["W_in", hres], w=[PSR(bank)])
                        act(lambda a, bg=bg, i=i: a.activation(out=sig[i % 2][:], in_=ps[bg][:], func=AF.Sigmoid),
                            r=[PSR(bg)], w=[("sig", i % 2)])
                        dve(lambda v, ba=ba, i=i, tok0=tok0: v.tensor_tensor(
                            out=hbuf[:, i, 15 + tok0:15 + tok0 + 512], in0=ps[ba][:], in1=sig[i % 2][:], op=ALU.mult),
                            r=[PSR(ba), ("sig", i % 2)], w=[("hbuf", i)])

                    def qk_chunk(w_, qi):
                        hTw = hT2[w_ % 2]
                        hres = ("hT", w_ % 2)
                        tok0 = w_ * 512
                        hh = qi % 4
                        isq = qi < 4
                        oc = (8 + hh) if isq else (12 + hh)
                        bd_, bs_ = (2, 4, 6)[qi % 3], (3, 5, 7)[qi % 3]
                        for kc in range(8):
                            pe(lambda tt, kc=kc, bd_=bd_, oc=oc, hTw=hTw: tt.matmul(
                                ps[bd_][:], lhsT=W_in[:, kc, oc * 128:(oc + 1) * 128], rhs=hTw[:, kc, :],
                                start=(kc == 0), stop=(kc == 7)), r=["W_in", hres], w=[PSR(bd_)])
                        act(lambda a, bd_=bd_, qi=qi: a.activation(out=sq[qi % 2][:], in_=ps[bd_][:], func=AF.Square),
                            r=[PSR(bd_)], w=[("sq", qi % 2)])
                        return (qi, hh, isq, bd_, bs_, tok0)

                    def qk_finish(state):
                        qi, hh, isq, bd_, bs_, tok0 = state
                        pe(lambda tt, bs_=bs_, qi=qi: tt.matmul(ps[bs_][:], lhsT=blk1[:], rhs=sq[qi % 2][:], start=True, stop=True),
                           r=["blk1", ("sq", qi % 2)], w=[PSR(bs_)])
                        act(lambda a, bs_=bs_, qi=qi: a.activation(out=rs[qi % 2][:], in_=ps[bs_][:], func=AF.Ln, bias=eps_c[:, 0:1]),
                            r=[PSR(bs_), "eps_c"], w=[("rs", qi % 2)])
                        act(lambda a, qi=qi: a.activation(out=rs[qi % 2][:], in_=rs[qi % 2][:], func=AF.Exp, scale=-0.5),
                            r=[("rs", qi % 2)], w=[("rs", qi % 2)])
                        dstT = qT if isq else kT
                        gcol = gq if isq else gk
                        dve(lambda v, bd_=bd_, qi=qi, dstT=dstT, gcol=gcol, hh=hh, tok0=tok0: v.scalar_tensor_tensor(
                            out=dstT[:, hh, tok0:tok0 + 512], in0=ps[bd_][:], scalar=gcol[:, 0:1], in1=rs[qi % 2][:],
                            op0=ALU.mult, op1=ALU.mult),
                            r=[PSR(bd_), ("rs", qi % 2), "gq", "gk"], w=[("qk", isq, hh)])

                    def v_tile(w_, tl):
                        hTw = hT2[w_ % 2]
                        hres = ("hT", w_ % 2)
                        bank = 6 + tl % 2
                        kt = w_ * 4 + tl
                        for kc in range(8):
                            pe(lambda tt, kc=kc, bank=bank, tl=tl, hTw=hTw: tt.matmul(
                                ps[bank][:], lhsT=hTw[:, kc, tl * 128:(tl + 1) * 128], rhs=W_in[:, kc, 2048:2560],
                                start=(kc == 0), stop=(kc == 7)), r=["W_in", hres], w=[PSR(bank)])
                        act(lambda a, bank=bank, kt=kt: a.activation(
                            out=vS[:, kt, :, 0:128], in_=ps[bank][:].rearrange("p (h e) -> p h e", h=4), func=AF.Copy),
                            r=[PSR(bank)], w=[("vS", kt)])

                    for tl in range(4):
                        chain(0, tl)
                        trans(0, tl)
                    for w_ in range(4):
                        nxt = w_ + 1 < 4
                        for q in range(4):
                            if nxt:
                                chain(w_ + 1, q)
                            if q == 0:
                                glu_chunk(w_, 0); glu_chunk(w_, 1)
                            elif q == 1:
                                glu_chunk(w_, 2); glu_chunk(w_, 3)
                            elif q == 2:
                                prev = None
                                for qi in range(4):
                                    st_ = qk_chunk(w_, qi)
                                    if prev is not None:
                                        qk_finish(prev)
                                    prev = st_
                                qk_finish(prev)
                            else:
                                prev = None
                                for qi in range(4, 8):
                                    st_ = qk_chunk(w_, qi)
                                    if prev is not None:
                                        qk_finish(prev)
                                    prev = st_
                                v_tile(w_, 0)
                                qk_finish(prev)
                                for tl in range(1, 4):
                                    v_tile(w_, tl)
                            if nxt:
                                trans(w_ + 1, q)
                    P.barrier()
                    P.emit()
                    if stop == "p1":
                        return nc

                with ExitStack() as p2:
                    tA_i = sb("tA_i", [128, 4096], I32, p2)
                    Tdec = sb("Tdec", [128, 2432], BF16, p2)
                    diag = [sb("diag%d" % i, [128, 31, 128], BF16, p2) for i in range(2)]
                    catT = sb("catT", [128, 8, 512], BF16, p2)
                    cvv = sb("cvv", [128, 4, 512], F32, p2)
                    cst = sb("cst", [128, 2, 512], F32, p2)
                    x1t = sb("x1t", [128, D], F32, p2)
                    h2T = sb("h2T", [128, 8, 128], BF16, p2)
                    S2 = sb("S2", [128, D], F32, p2)
                    H2 = sb("H2", [128, D], F32, p2)
                    dma("sp", H2[:], MOD_d[b, 3], w=["H2"])
                    dma("sp", S2[:], MOD_d[b, 4], w=["S2"])
                    pool(lambda g: g.iota(tA_i[:], pattern=[[1, 4096]], base=-2048, channel_multiplier=-1), w=["tA"])
                    dve(lambda v: v.scalar_tensor_tensor(out=tA_i[:], in0=tA_i[:], scalar=-1.0, in1=tA_i[:], op0=ALU.mult, op1=ALU.max),
                        r=["tA"], w=["tA"])
                    QKR = [("qk", a_, h_) for a_ in (True, False) for h_ in range(4)]
                    VR = [("vS", t_) for t_ in range(16)]
                    nd = 0

                    def build_diag(i, slot):
                        dg_ = diag[slot]
                        dve(lambda g, i=i, dg_=dg_: g.tensor_tensor(out=dg_[:], in0=_ap(ident[:], [[128, 128], [0, 31], [1, 128]]),
                                                                    in1=_ap(cwb[:], [[124, 128], [1, 31], [0, 128]], off=i * 31), op=ALU.mult),
                            r=["ident", "cwb"], w=[("diag", slot)])

                    build_diag(0, 0)
                    pending = []
                    stats_pending = []
                    for w_ in range(4):
                        tok0 = w_ * 512
                        for i in range(4):
                            dg = diag[nd % 2]
                            dgr = ("diag", nd % 2)
                            nd += 1
                            if not (w_ == 3 and i == 3):
                                build_diag((i + 1) % 4, nd % 2)
                            bank = i % 2
                            for j in range(31):
                                pe(lambda tt, i=i, j=j, bank=bank, tok0=tok0, dg=dg: tt.matmul(
                                    ps[bank][:], lhsT=dg[:, j, :], rhs=hbuf[:, i, tok0 + j:tok0 + j + 512],
                                    start=(j == 0), stop=(j == 30)), r=[dgr, ("hbuf", i)], w=[PSR(bank)])
                            while stats_pending:
                                stats_pending.pop(0)()
                            act(lambda a, i=i, bank=bank: a.activation(out=cvv[:, i, :], in_=ps[bank][:], func=AF.Identity,
                                                                        bias=cb[:, i:i + 1]), r=[PSR(bank), "cb"], w=[("cvv", i)])
                            dve(lambda v, i=i: v.tensor_copy(out=sig[i % 2][:], in_=cvv[:, i, :]),
                                r=[("cvv", i)], w=[("sig", i % 2)])
                            act(lambda a, i=i: a.activation(out=sq[i % 2][:], in_=cvv[:, i, :], func=AF.Square),
                                r=[("cvv", i)], w=[("sq", i % 2)])
                            def _stats(i=i):
                                pe(lambda tt, i=i: tt.matmul(ps[2][:], lhsT=om512[:], rhs=sig[i % 2][:],
                                                             start=(i == 0), stop=(i == 3)), r=["om512", ("sig", i % 2)], w=[PSR(2)])
                                pe(lambda tt, i=i: tt.matmul(ps[3][:], lhsT=om512[:], rhs=sq[i % 2][:], start=(i == 0), stop=(i == 3)),
                                   r=["om512", ("sq", i % 2)], w=[PSR(3)])
                            stats_pending.append(_stats)
                        while stats_pending:
                            stats_pending.pop(0)()
                        act(lambda a: a.activation(out=cst[:, 0, :], in_=ps[2][:], func=AF.Copy), r=[PSR(2)], w=[("cst", 0)])
                        dve(lambda v: v.tensor_tensor(out=rs[0][:], in0=cst[:, 0, :], in1=cst[:, 0, :], op=ALU.mult),
                            r=[("cst", 0)], w=[("rs", 0)])
                        dve(lambda v: v.tensor_tensor(out=cst[:, 1, :], in0=ps[3][:], in1=rs[0][:], op=ALU.subtract),
                            r=[PSR(3), ("rs", 0)], w=[("cst", 1)])
                        act(lambda a: a.activation(out=cst[:, 1, :], in_=cst[:, 1, :], func=AF.Ln, bias=eps_c[:, 0:1]),
                            r=[("cst", 1), "eps_c"], w=[("cst", 1)])
                        act(lambda a: a.activation(out=cst[:, 1, :], in_=cst[:, 1, :], func=AF.Exp, scale=-0.5),
                            r=[("cst", 1)], w=[("cst", 1)])
                        for i in range(4):
                            dve(lambda v, i=i: v.tensor_tensor(out=cvv[:, i, :], in0=cvv[:, i, :], in1=cst[:, 0, :], op=ALU.subtract),
                                r=[("cvv", i), ("cst", 0)], w=[("cvv", i)])
                            dve(lambda v, i=i: v.tensor_tensor(out=cvv[:, i, :], in0=cvv[:, i, :], in1=cst[:, 1, :], op=ALU.mult),
                                r=[("cvv", i), ("cst", 1)], w=[("cvv", i)])
                            act(lambda a, i=i: a.activation(out=catT[:, i, :], in_=cvv[:, i, :], func=AF.Silu,
                                                            bias=clb[:, i:i + 1], scale=clg[:, i:i + 1]),
                                r=[("cvv", i), "clb", "clg"], w=[("catT", i)])
                        lo = tok0 + 128
                        items = [(hh, c, kt) for hh in range(4) for kt in range(16) for c in range(2)]

                        def stage1(g):
                            hh, c, kt = items[g]
                            if c == 0 and kt == 0:
                                act(lambda a, hh=hh, lo=lo: a.activation(out=Tdec[:], in_=tA_i[:, lo:lo + 2432], func=AF.Exp,
                                                                         scale=-slopes[hh]), r=["tA"], w=["Tdec"])
                                pool(lambda gp, hh=hh, tok0=tok0: gp.tensor_copy(out=qz[hh % 2][0][0:64, :], in_=qT[0:64, hh, tok0:tok0 + 512]),
                                     r=QKR, w=[("qz", hh % 2)])
                                pool(lambda gp, hh=hh, tok0=tok0: gp.tensor_copy(out=qz[hh % 2][1][64:128, :], in_=qT[64:128, hh, tok0:tok0 + 512]),
                                     r=QKR + [("qz", hh % 2)], w=[("qz", hh % 2)])
                            sbk = 3 + g % 3
                            pe(lambda tt, hh=hh, c=c, kt=kt, sbk=sbk: tt.matmul(
                                ps[sbk][:], lhsT=kT[:, hh, kt * 128:(kt + 1) * 128],
                                rhs=qz[hh % 2][c][:], start=True, stop=True),
                               r=QKR + [("qz", hh % 2)], w=[PSR(sbk)])
                            act(lambda a, sbk=sbk, g=g: a.activation(out=Eb[g % 3][:], in_=ps[sbk][:], func=AF.Exp),
                                r=[PSR(sbk)], w=[("Eb", g % 3)])
                            i0 = tok0 - kt * 128 + 2048 - lo
                            dve(lambda v, g=g, i0=i0: v.tensor_tensor(out=Pt[g % 5][:], in0=Eb[g % 3][:],
                                                                      in1=Tdec[:, i0:i0 + 512], op=ALU.mult),
                                r=[("Eb", g % 3), "Tdec"], w=[("Pt", g % 5)])

                        def stage2(g):
                            hh, c, kt = items[g]
                            bankA, bankB = (6, 7) if c == 0 else (0, 1)
                            pe(lambda tt, kt=kt, bankA=bankA, hh=hh, g=g: tt.matmul(
                                ps[bankA][:], lhsT=vS[:, kt, hh, 0:128], rhs=Pt[g % 5][:], start=(kt == 0), stop=(kt == 15)),
                               r=[("Pt", g % 5)] + VR, w=[PSR(bankA)])
                            pe(lambda tt, kt=kt, bankB=bankB, g=g: tt.matmul(
                                ps[bankB][:], lhsT=ones_bf[:, 0:128], rhs=Pt[g % 5][:], start=(kt == 0), stop=(kt == 15)),
                               r=[("Pt", g % 5), "ones_bf"], w=[PSR(bankB)])
                            if kt != 15:
                                return
                            act(lambda a, bankB=bankB, c=c: a.activation(out=rs[c][:], in_=ps[bankB][:], func=AF.Ln), r=[PSR(bankB)], w=[("rs", c)])
                            act(lambda a, c=c: a.activation(out=rs[c][:], in_=rs[c][:], func=AF.Exp, scale=-1.0), r=[("rs", c)], w=[("rs", c)])
                            dve(lambda v, bankA=bankA, c=c: v.tensor_tensor(out=oT[c][:], in0=ps[bankA][:], in1=rs[c][:], op=ALU.mult),
                                r=[PSR(bankA), ("rs", c)], w=[("oT", c)])
                            if c == 1:
                                dve(lambda v: v.scalar_tensor_tensor(out=oT[1][:], in0=oT[1][:], scalar=neglam[:, 0:1], in1=oT[0][:],
                                                                     op0=ALU.mult, op1=ALU.add),
                                    r=[("oT", 0), ("oT", 1), "neglam"], w=[("oT", 1)])
                                dve(lambda v: v.tensor_tensor(out=sq[0][:], in0=oT[1][:], in1=oT[1][:], op=ALU.mult),
                                    r=[("oT", 1)], w=[("sq", 0)])

                                def _fin(hh=hh):
                                    pe(lambda tt: tt.matmul(ps[2][:], lhsT=om128[:], rhs=sq[0][:], start=True, stop=True),
                                       r=["om128", ("sq", 0)], w=[PSR(2)])
                                    act(lambda a: a.activation(out=rs[0][:], in_=ps[2][:], func=AF.Ln, bias=eps_c[:, 0:1]),
                                        r=[PSR(2), "eps_c"], w=[("rs", 0)])
                                    act(lambda a: a.activation(out=rs[0][:], in_=rs[0][:], func=AF.Exp, scale=-0.5),
                                        r=[("rs", 0)], w=[("rs", 0)])
                                    dve(lambda v, hh=hh: v.scalar_tensor_tensor(out=catT[:, 4 + hh, :], in0=oT[1][:], scalar=subgc[:, 0:1],
                                                                                in1=rs[0][:], op0=ALU.mult, op1=ALU.mult),
                                        r=[("oT", 1), ("rs", 0), "subgc"], w=[("catT", 4 + hh)])
                                pending.append(_fin)

                        DEPTH = 4
                        for g in range(len(items)):
                            stage1(g)
                            if g >= DEPTH:
                                stage2(g - DEPTH)
                            if items[g][2] == 9 and items[g][1] == 1 and pending:
                                pending.pop(0)()
                        for g in range(len(items) - DEPTH, len(items)):
                            stage2(g)
                        while pending:
                            pending.pop(0)()
                        CATR = [("catT", i) for i in range(8)]

                        def stA(u):
                            t = b * 16 + w_ * 4 + u
                            sl = t % 2
                            zero_fill(-(-ZF_TOTAL // (2 * nseq * 16)))
                            dma("sp", xs[sl][:], x_tiles[t], w=[("xs", sl)])
                            for half in range(2):
                                bank = 4 + half
                                for kc in range(8):
                                    pe(lambda tt, kc=kc, u=u, half=half, bank=bank: tt.matmul(
                                        ps[bank][:], lhsT=catT[:, kc, u * 128:(u + 1) * 128],
                                        rhs=W_out[:, kc, half * 512:(half + 1) * 512], start=(kc == 0), stop=(kc == 7)),
                                       r=CATR + ["W_out"], w=[PSR(bank)])
                                dve(lambda v, half=half, bank=bank: v.tensor_tensor(
                                    out=tmp1[:, half * 512:(half + 1) * 512], in0=ps[bank][:], in1=G1[:, half * 512:(half + 1) * 512],
                                    op=ALU.mult), r=[PSR(bank), "G1"], w=[("tmp1h", half)])

                        def stB(u):
                            t = b * 16 + w_ * 4 + u
                            sl = t % 2
                            dve(lambda g, sl=sl: g.tensor_tensor(out=x1t[:], in0=tmp1[:], in1=xs[sl][:], op=ALU.add),
                                r=[("tmp1h", 0), ("tmp1h", 1), ("xs", sl)], w=["x1t", "tmp1"])
                            dma("sp", X1_tiles[t], x1t[:], r=["x1t"], w=[("X1", t)])
                            if debug:
                                dma("sp", dbg["x1"].ap().rearrange("(t p) d -> t p d", p=128)[t], x1t[:],
                                    r=["x1t"], w=[("dbgx1", t)])
                            act(lambda a, sl=sl: a.activation(out=htok[sl][:], in_=x1t[:], func=AF.Square, scale=D ** -0.5, accum_out=ss[:, 2:3]),
                                r=["x1t"], w=[("htok", sl), ("ss", 2)])
                            act(lambda a: a.activation(out=ss[:, 3:4], in_=ss[:, 2:3], func=AF.Ln, bias=eps_c[:, 0:1]),
                                r=[("ss", 2), "eps_c"], w=[("ss", 3)])
                            act(lambda a: a.activation(out=ss[:, 3:4], in_=ss[:, 3:4], func=AF.Exp, scale=-0.5),
                                r=[("ss", 3)], w=[("ss", 3)])
                            dve(lambda v: v.scalar_tensor_tensor(out=tmp1[:], in0=x1t[:], scalar=ss[:, 3:4], in1=S2[:],
                                                                 op0=ALU.mult, op1=ALU.mult),
                                r=["x1t", ("ss", 3), "S2"], w=["tmp1", ("tmp1h", 0), ("tmp1h", 1)])
                            dve(lambda g, sl=sl: g.tensor_tensor(out=htok[sl][:], in0=tmp1[:], in1=H2[:], op=ALU.add),
                                r=["tmp1", ("tmp1h", 0), ("tmp1h", 1), "H2"], w=[("htok", sl)])
                            dma("sp", H2_tiles[t], htok[sl][:], r=[("htok", sl)], w=[("H2d", t)])

                        def stC(u):
                            t = b * 16 + w_ * 4 + u
                            sl = t % 2
                            for kc in range(8):
                                pe(lambda tt, kc=kc, sl=sl: tt.transpose(psb(2)[:, kc * 128:(kc + 1) * 128],
                                                                         htok[sl][:, kc * 128:(kc + 1) * 128], ident[:]),
                                   r=[("htok", sl), "ident"], w=[PSR(2)])
                            act(lambda a: a.activation(out=h2T[:], in_=psb(2).rearrange("p (k t) -> p k t", k=8), func=AF.Copy),
                                r=[PSR(2)], w=["h2T"])
                            for kc in range(8):
                                pe(lambda tt, kc=kc: tt.matmul(ps[3][:, 0:E], lhsT=h2T[:, kc, :], rhs=W_r[:, kc, :],
                                                               start=(kc == 0), stop=(kc == 7)), r=["h2T", "W_r"], w=[PSR(3)])
                            dve(lambda v, t=t: v.tensor_tensor(out=lg_all[:, t, :], in0=ps[3][:, 0:E], in1=brb[:], op=ALU.add),
                                r=[PSR(3), "brb"], w=[("lg", t)])
                            dve(lambda v, t=t: v.max(out=mx8[:, t, :], in_=lg_all[:, t, :]), r=[("lg", t)], w=[("mx8", t)])
                            dve(lambda v, t=t: v.tensor_scalar(out=Mb[:, t * E:(t + 1) * E], in0=lg_all[:, t, :], scalar1=mx8[:, t, 3:4],
                                                               scalar2=None, op0=ALU.is_ge), r=[("lg", t), ("mx8", t)], w=[("Mb", t)])
                            dve(lambda v, t=t: v.tensor_scalar(out=nmx[:, t:t + 1], in0=mx8[:, t, 0:1], scalar1=-1.0, scalar2=None,
                                                               op0=ALU.mult), r=[("mx8", t)], w=[("nmx", t)])
                            act(lambda a, t=t: a.activation(out=g4[:, t, :], in_=mx8[:, t, 0:4], func=AF.Exp, bias=nmx[:, t:t + 1],
                                                            accum_out=gsm[:, t:t + 1]), r=[("mx8", t), ("nmx", t)], w=[("g4", t), ("gsm", t)])
                            dve(lambda v, t=t: v.reciprocal(out=gsm[:, t:t + 1], in_=gsm[:, t:t + 1]), r=[("gsm", t)], w=[("gsm", t)])
                            dve(lambda v, t=t: v.tensor_scalar(out=g4[:, t, :], in0=g4[:, t, :], scalar1=gsm[:, t:t + 1], scalar2=None,
                                                               op0=ALU.mult), r=[("g4", t), ("gsm", t)], w=[("g4", t)])

                        stA(0)
                        stB(0)
                        for u in range(1, 4):
                            stA(u)
                            stC(u - 1)
                            stB(u)
                        stC(3)
                    P.barrier()
                    P.emit()
                    if stop == "p2":
                        return nc
                seq_scope.__exit__(None, None, None)

        with ExitStack() as pb:
            sloti = sb("sloti", [128, NTL * 4], I32, pb)
            bei = sb("bei", [128, nstep], I32, pb)
            widx = sb("widx", [128, nstep * 8], I32, pb)
            bsel = sb("bsel", [128, 16, nstep], F32, pb)
            ohT = sb("ohT", [128, nstep], F32, pb)
            bgu = sb("bgu", [E, 2 * D], BF16, pb)
            bdn = sb("bdn", [128, D], BF16, pb)
            rt_scope = ExitStack()
            rt_scope.__enter__()
            Lst = sb("Lst", [128, 128], BF16, rt_scope)
            onesq = sb("onesq", [128, 128], BF16, rt_scope)
            pre = sb("pre", [128, NTL, E], F32, rt_scope)
            csb = sb("csb", [128, NTL, E], F32, rt_scope)
            off = sb("off", [128, NTL, E], F32, rt_scope)
            cnt = sb("cnt", [128, E], F32, rt_scope)
            pad = sb("pad", [128, E], F32, rt_scope)
            pst = sb("pst", [128, E], F32, rt_scope)
            pend = sb("pend", [128, E], F32, rt_scope)
            big = sb("big", [128, NTL, 4, E], F32, rt_scope)
            slotf = sb("slotf", [128, NTL, 4], F32, rt_scope)
            bthr = sb("bthr", [128, nstep], F32, rt_scope)
            bthr_i = sb("bthr_i", [128, nstep], I32, rt_scope)
            cmpb = sb("cmpb", [128, nstep, E], F32, rt_scope)
            bef = sb("bef", [128, nstep], F32, rt_scope)
            bgu_f = sb("bgu_f", [E, 2 * D], F32, rt_scope)
            bd_f = sb("bd_f", [E, D], F32, rt_scope)
            cmp8 = sb("cmp8", [128, E, 16], F32, rt_scope)
            kcp_i = sb("kcp_i", [128, 8], I32, rt_scope)
            kcp = sb("kcp", [128, 8], F32, rt_scope)
            widx_f = sb("widx_f", [128, nstep, 8], F32, rt_scope)
            bef1k = sb("bef1k", [128, nstep], F32, rt_scope)
            inact = sb("inact", [128, nstep], F32, rt_scope)
            ohTb = sb("ohTb", [E, nstep], BF16, rt_scope)

            LGR = [("lg", t) for t in range(NTL)]
            dve(lambda v: v.tensor_scalar(out=Lst[:], in0=iota_f[:], scalar1=iota_p[:, 0:1], scalar2=None, op0=ALU.is_gt),
                r=["iota_f", "iota_p"], w=["Lst"])
            dve(lambda v: v.memset(onesq[:], 1.0), w=["onesq"])
            pool(lambda g: g.iota(bthr_i[:], pattern=[[RB, nstep]], base=0, channel_multiplier=0), w=["bthr_i"])
            dve(lambda v: v.tensor_copy(out=bthr[:], in_=bthr_i[:]), r=["bthr_i"], w=["bthr"])
            dma("sp", bgu_f[:], bgu_d.ap(), w=["bgu_f"])
            dma("sp", bd_f[:], bd_d.ap(), w=["bd_f"])
            act(lambda a: a.activation(out=bgu[:], in_=bgu_f[:], func=AF.Copy), r=["bgu_f"], w=["bgu"])
            dve(lambda v: v.memset(bdn[:], 0.0), w=["bdn"])
            act(lambda a: a.activation(out=bdn[0:E, :], in_=bd_f[:], func=AF.Copy), r=["bd_f", "bdn"], w=["bdn"])
            MXR = []
            G4R = []
            nh = (NTL * E) // 512
            for hf in range(nh):
                pe(lambda tt, hf=hf: tt.matmul(ps[hf][:], lhsT=Lst[:], rhs=Mb[:, hf * 512:(hf + 1) * 512], start=True, stop=True),
                   r=["Lst"], w=[PSR(hf)])
                pe(lambda tt, hf=hf: tt.matmul(ps[2 + hf][:], lhsT=onesq[:], rhs=Mb[:, hf * 512:(hf + 1) * 512], start=True, stop=True),
                   r=["onesq"], w=[PSR(2 + hf)])
                act(lambda a, hf=hf: a.activation(out=pre[:].rearrange("p t e -> p (t e)")[:, hf * 512:(hf + 1) * 512],
                                                  in_=ps[hf][:], func=AF.Copy), r=[PSR(hf)], w=[("pre", hf)])
                act(lambda a, hf=hf: a.activation(out=csb[:].rearrange("p t e -> p (t e)")[:, hf * 512:(hf + 1) * 512],
                                                  in_=ps[2 + hf][:], func=AF.Copy), r=[PSR(2 + hf)], w=[("csb", hf)])
            PRER = [("pre", hf) for hf in range(nh)]
            CSR = [("csb", hf) for hf in range(nh)]
            dve(lambda v: v.memset(off[:, 0, :], 0.0), w=["off"])
            for t in range(1, NTL):
                dve(lambda v, t=t: v.tensor_tensor(out=off[:, t, :], in0=off[:, t - 1, :], in1=csb[:, t - 1, :], op=ALU.add),
                    r=["off"] + CSR, w=["off"])
            dve(lambda v: v.tensor_tensor(out=cnt[:], in0=off[:, NTL - 1, :], in1=csb[:, NTL - 1, :], op=ALU.add),
                r=["off"] + CSR, w=["cnt"])
            nmx_b = NTOK // RB + 1
            assert nmx_b <= 16
            dve(lambda v: v.tensor_tensor(out=cmp8[:, :, 0:nmx_b], in0=_ap(cnt[:], [[E, 128], [1, E], [0, nmx_b]]),
                                          in1=_ap(bthr[:], [[nstep, 128], [0, E], [1, nmx_b]]), op=ALU.is_gt),
                r=["cnt", "bthr"], w=["cmp8"])
            dve(lambda v: v.tensor_reduce(out=pad[:], in_=cmp8[:, :, 0:nmx_b], axis=AX.X, op=ALU.add), r=["cmp8"], w=["pad"])
            dve(lambda v: v.tensor_scalar(out=pad[:], in0=pad[:], scalar1=float(RB), scalar2=None, op0=ALU.mult),
                r=["pad"], w=["pad"])
            dve(lambda v: v.memset(pst[:, 0:1], 0.0), w=["pst"])
            for e in range(1, E):
                dve(lambda v, e=e: v.tensor_tensor(out=pst[:, e:e + 1], in0=pst[:, e - 1:e], in1=pad[:, e - 1:e], op=ALU.add),
                    r=["pst", "pad"], w=["pst"])
            dve(lambda v: v.tensor_tensor(out=pend[:], in0=pst[:], in1=pad[:], op=ALU.add), r=["pst", "pad"], w=["pend"])
            dve(lambda v: v.tensor_tensor(out=pre[:], in0=pre[:], in1=off[:], op=ALU.add), r=PRER + ["off"], w=["dest"])
            dve(lambda v: v.tensor_tensor(out=pre[:], in0=pre[:], in1=_ap(pst[:], [[E, 128], [0, NTL], [1, E]]), op=ALU.add),
                r=["dest", "pst"], w=["dest"])
            dve(lambda v: v.tensor_tensor(out=big[:], in0=_ap(lg_all[:], [[NTL * E, 128], [E, NTL], [0, 4], [1, E]]),
                                          in1=_ap(mx8[:], [[NTL * 8, 128], [8, NTL], [1, 4], [0, E]]), op=ALU.is_equal),
                r=LGR + MXR, w=["big"])
            dve(lambda v: v.tensor_tensor(out=big[:], in0=big[:], in1=_ap(pre[:], [[NTL * E, 128], [E, NTL], [0, 4], [1, E]]),
                                          op=ALU.mult), r=["big", "dest"], w=["big"])
            dve(lambda v: v.tensor_reduce(out=slotf[:], in_=big[:], axis=AX.X, op=ALU.add), r=["big"], w=["slotf"])
            dve(lambda v: v.tensor_copy(out=sloti[:], in_=slotf[:].rearrange("p t k -> p (t k)")), r=["slotf"], w=["sloti"])
            dve(lambda v: v.tensor_tensor(out=cmpb[:], in0=_ap(pend[:], [[E, 128], [0, nstep], [1, E]]),
                                          in1=_ap(bthr[:], [[nstep, 128], [1, nstep], [0, E]]), op=ALU.is_le),
                r=["pend", "bthr"], w=["cmpb"])
            dve(lambda v: v.tensor_reduce(out=bef[:], in_=cmpb[:], axis=AX.X, op=ALU.add), r=["cmpb"], w=["bef"])
            dve(lambda v: v.tensor_scalar(out=bef[:], in0=bef[:], scalar1=float(E - 1), scalar2=None, op0=ALU.min),
                r=["bef"], w=["bef"])
            dve(lambda v: v.tensor_copy(out=bei[:], in_=bef[:]), r=["bef"], w=["bei"])
            dve(lambda v: v.tensor_scalar(out=ohT[:], in0=bef[:, :], scalar1=iota_p[:, 0:1], scalar2=None, op0=ALU.is_equal),
                r=["bef", "iota_p"], w=["ohT"])
            pool(lambda g: g.iota(kcp_i[:], pattern=[[128, 8]], base=0, channel_multiplier=1), w=["kcp_i"])
            dve(lambda v: v.tensor_copy(out=kcp[:], in_=kcp_i[:]), r=["kcp_i"], w=["kcp"])
            dve(lambda v: v.tensor_scalar(out=bef1k[:], in0=bef[:], scalar1=float(D), scalar2=None, op0=ALU.mult), r=["bef"], w=["bef1k"])
            dve(lambda v: v.tensor_scalar(out=inact[:], in0=bthr[:], scalar1=pend[:, E - 1:E], scalar2=None, op0=ALU.is_ge),
                r=["bthr", "pend"], w=["inact"])
            dve(lambda v: v.scalar_tensor_tensor(out=bef1k[:], in0=inact[:], scalar=1.0e6, in1=bef1k[:], op0=ALU.mult, op1=ALU.add),
                r=["inact", "bef1k"], w=["bef1k"])
            dve(lambda v: v.tensor_copy(out=ohTb[:], in_=ohT[0:E, :]), r=["ohT"], w=["ohTb"])
            for c16 in range(16):
                bk = 4 + c16 // 8
                pe(lambda tt, c16=c16, bk=bk: tt.matmul(ps[bk][:, (c16 % 8) * nstep:(c16 % 8 + 1) * nstep],
                                                        lhsT=bgu[:, c16 * 128:(c16 + 1) * 128], rhs=ohTb[:], start=True, stop=True),
                   r=["bgu", "ohTb"], w=[PSR(bk)])
            act(lambda a: a.activation(out=bsel[:, 0:8, :], in_=ps[4][:, 0:8 * nstep].rearrange("p (c b) -> p c b", c=8), func=AF.Copy),
                r=[PSR(4)], w=["bsel"])
            dve(lambda v: v.tensor_scalar(out=bsel[:, 8:16, :], in0=ps[5][:, 0:8 * nstep].rearrange("p (c b) -> p c b", c=8),
                                          scalar1=1.0, scalar2=None, op0=ALU.add), r=[PSR(5), "bsel"], w=["bsel"])
            dve(lambda v: v.tensor_tensor(out=widx_f[:], in0=_ap(bef1k[:], [[nstep, 128], [1, nstep], [0, 8]]),
                                          in1=_ap(kcp[:], [[8, 128], [0, nstep], [1, 8]]), op=ALU.add),
                r=["bef1k", "kcp"], w=["widx_f"])
            dve(lambda v: v.tensor_copy(out=widx[:], in_=widx_f[:].rearrange("p b c -> p (b c)")), r=["widx_f"], w=["widx"])
            if debug:
                dma("sp", dbg["lg"].ap(), lg_all[:], r=LGR, w=["dbg_lg"])
                dma("sp", dbg["slot"].ap(), slotf[:], r=["slotf"], w=["dbg_slot"])
                dma("sp", dbg["g4"].ap(), g4[:], r=G4R, w=["dbg_g4"])
                dma("sp", dbg["be"].ap(), bef[:], r=["bef"], w=["dbg_be"])

            P.barrier()
            P.emit()
            rt_scope.__exit__(None, None, None)
            G4R = [("g4", t) for t in range(NTL)]
            st_scope = ExitStack()
            st_scope.__enter__()
            if do_moe:
                bcreg = nc.gpsimd.alloc_register("bcreg")
                ohb = [sb("ohb%d" % i, [128, 128], BF16, st_scope) for i in range(2)]
                Wgu = [sb("Wgu%d" % i, [128, 8, 2 * D], BF16, st_scope) for i in range(2)]
                Wdn = [sb("Wdn%d" % i, [128, 8, D], BF16, st_scope) for i in range(2)]
                xr = [sb("xr%d" % i, [128, D], BF16, st_scope) for i in range(8)]
                xT = [sb("xT%d" % i, [128, 8, 512], BF16, st_scope) for i in range(2)]
                actT = sb("actT", [128, 8, 512], BF16, st_scope)
                gm = [sb("gm%d" % i, [128, 512], F32, st_scope) for i in range(2)]
                sg = [sb("sg%d" % i, [128, 512], F32, st_scope) for i in range(2)]
                uc = [sb("uc%d" % i, [128, 512], F32, st_scope) for i in range(2)]
                ost = [sb("ost%d" % i, [128, D], BF16, st_scope) for i in range(2)]
            if do_moe:
                H2_tiles = H2_d.ap().rearrange("(t p) d -> t p d", p=128)
                X1_tiles = X1_d.ap().rearrange("(t p) d -> t p d", p=128)
                out_tiles = out_d.ap().rearrange("(t p) d -> t p d", p=128)
                wgu2d = wgu_d.ap().rearrange("e k n -> (e k) n")
                wd2d = wd_d.ap().rearrange("e k n -> (e k) n")
                wreg = nc.gpsimd.alloc_register("wreg")

                def load_weights(bstep):
                    par = bstep % 2
                    for kc in range(8):
                        def fn(g, kc=kc):
                            if bstep == 0 and kc == 0:
                                g.reg_mov(wreg, E * D - 1)
                            return g.indirect_dma_start(
                                out=Wgu[par][:, kc, :], out_offset=None, in_=wgu2d,
                                in_offset=bass.IndirectOffsetOnAxis(ap=widx[:, bstep * 8 + kc:bstep * 8 + kc + 1], axis=0),
                                bounds_check=wreg, oob_is_err=False)
                        P.op("pool", fn, r=["widx"], w=[("Wgu", par, kc)], dma=True)
                    for kc in range(8):
                        P.op("pool", lambda g, kc=kc: g.indirect_dma_start(
                            out=Wdn[par][:, kc, :], out_offset=None, in_=wd2d,
                            in_offset=bass.IndirectOffsetOnAxis(ap=widx[:, bstep * 8 + kc:bstep * 8 + kc + 1], axis=0),
                            bounds_check=wreg, oob_is_err=False), r=["widx"], w=[("Wdn", par, kc)], dma=True)

                load_weights(0)
                for t in range(NTL):
                    sl = t % 4
                    dma("sp", xr[sl][:], H2_tiles[t], w=[("xr", 0, sl)])
                    for k in range(4):
                        def _scat(g, sl=sl, t=t, k=k):
                            if t == 0 and k == 0:
                                g.reg_mov(bcreg, NSLOT - 1)
                            return g.indirect_dma_start(
                                out=XB_d[:, :], out_offset=bass.IndirectOffsetOnAxis(ap=sloti[:, t * 4 + k:t * 4 + k + 1], axis=0),
                                in_=xr[sl][:], in_offset=None, bounds_check=bcreg, oob_is_err=False)
                        P.op("pool", _scat,
                            r=[("xr", 0, sl), "sloti"], w=[("XBs", t, k)], dma=True)
                XBS = [("XBs", t, k) for t in range(NTL) for k in range(4)]
                P.op("sp", None, r=XBS, w=["XBjoin"])

                def emit_xload(bs):
                    for rt in range(4):
                        dma("sp", xr[(bs % 2) * 4 + rt][:], XB_d[bs * RB + rt * 128: bs * RB + (rt + 1) * 128, :],
                            r=["XBjoin"], w=[("xr", bs % 2, rt)])

                def emit_xT(bs):
                    xTs = xT[bs % 2]
                    for rt in range(4):
                        bank = (6, 7, 0, 1)[rt]
                        src_t = xr[(bs % 2) * 4 + rt]
                        for kc in range(8):
                            pe(lambda tt, kc=kc, src_t=src_t, bank=bank: tt.transpose(psb(bank)[:, kc * 128:(kc + 1) * 128],
                                                                                     src_t[:, kc * 128:(kc + 1) * 128], ident[:]),
                               r=[("xr", bs % 2, rt), "ident"], w=[PSR(bank)])
                        act(lambda a, bank=bank, rt=rt, xTs=xTs: a.activation(
                            out=xTs[:, :, rt * 128:(rt + 1) * 128], in_=psb(bank).rearrange("p (k t) -> p k t", k=8), func=AF.Copy),
                            r=[PSR(bank)], w=[("xT", bs % 2)])

                emit_xload(0)
                emit_xT(0)
                if nstep > 1:
                    emit_xload(1)
                for bstep in range(nstep):
                    par = bstep % 2
                    if bstep + 1 < nstep:
                        load_weights(bstep + 1)
                    WG = [("Wgu", par, q) for q in range(8)]
                    WD = [("Wdn", par, q) for q in range(8)]
                    xTs = xT[par]
                    dve(lambda v, bstep=bstep, par=par: v.tensor_copy(out=ohb[par][:], in_=_ap(ohT[:], [[nstep, 128], [0, 128]], off=bstep)),
                        r=["ohT"], w=[("ohb", par)])
                    for fc in range(8):
                        bg_, bu_ = 2 + 2 * (fc % 2), 3 + 2 * (fc % 2)
                        for (bank, col0) in ((bg_, fc * 128), (bu_, D + fc * 128)):
                            for kc in range(8):
                                pe(lambda tt, kc=kc, bank=bank, col0=col0, par=par, xTs=xTs: tt.matmul(
                                    ps[bank][:], lhsT=Wgu[par][:, kc, col0:col0 + 128], rhs=xTs[:, kc, :],
                                    start=(kc == 0), stop=(kc == 7)), r=WG + [("xT", par)], w=[PSR(bank)])
                        s2 = fc % 2
                        dve(lambda v, bg_=bg_, s2=s2, fc=fc, bstep=bstep: v.tensor_scalar(
                            out=gm[s2][:], in0=ps[bg_][:], scalar1=bsel[:, fc, bstep:bstep + 1], scalar2=7.0, op0=ALU.add, op1=ALU.min),
                            r=[PSR(bg_), "bsel"], w=[("gm", s2)])
                        act(lambda a, s2=s2: a.activation(out=sg[s2][:], in_=gm[s2][:], func=AF.Sigmoid, scale=1.702),
                            r=[("gm", s2)], w=[("sg", s2)])
                        dve(lambda v, bu_=bu_, s2=s2, fc=fc, bstep=bstep: v.tensor_scalar(
                            out=uc[s2][:], in0=ps[bu_][:], scalar1=bsel[:, 8 + fc, bstep:bstep + 1], scalar2=8.0, op0=ALU.add, op1=ALU.min),
                            r=[PSR(bu_), "bsel"], w=[("uc", s2)])
                        dve(lambda v, s2=s2: v.tensor_tensor(out=gm[s2][:], in0=gm[s2][:], in1=sg[s2][:], op=ALU.mult),
                            r=[("gm", s2), ("sg", s2)], w=[("gm", s2)])
                        dve(lambda v, s2=s2, fc=fc: v.scalar_tensor_tensor(out=actT[:, fc, :], in0=uc[s2][:], scalar=-6.0, in1=gm[s2][:],
                                                                           op0=ALU.max, op1=ALU.mult),
                            r=[("uc", s2), ("gm", s2)], w=[("actT", fc)])
                    if bstep + 1 < nstep:
                        emit_xT(bstep + 1)
                    if bstep + 2 < nstep:
                        emit_xload(bstep + 2)
                    ACTR = [("actT", fc) for fc in range(8)]
                    for rt in range(4):
                        o_ = ost[rt % 2]
                        for half in range(2):
                            bank = ((6, 7), (0, 1))[rt % 2][half]
                            for fc in range(8):
                                pe(lambda tt, fc=fc, rt=rt, half=half, bank=bank, par=par: tt.matmul(
                                    ps[bank][:], lhsT=actT[:, fc, rt * 128:(rt + 1) * 128],
                                    rhs=Wdn[par][:, fc, half * 512:(half + 1) * 512], start=(fc == 0), stop=False),
                                   r=ACTR + WD, w=[PSR(bank)])
                            pe(lambda tt, half=half, bank=bank, par=par: tt.matmul(
                                ps[bank][:], lhsT=ohb[par][:, 0:128], rhs=bdn[:, half * 512:(half + 1) * 512], start=False, stop=True),
                               r=["bdn", ("ohb", par)], w=[PSR(bank)])
                            if False:
                                pass
                            else:
                                dve(lambda v, half=half, bank=bank, o_=o_: v.tensor_copy(out=o_[:, half * 512:(half + 1) * 512], in_=ps[bank][:]),
                                    r=[PSR(bank)], w=[("ost", rt % 2, half)])
                        dma("sp", OB_d[bstep * RB + rt * 128: bstep * RB + (rt + 1) * 128, :], o_[:],
                            r=[("ost", rt % 2, 0), ("ost", rt % 2, 1)], w=[("OB", bstep, rt)])
                OBR = [("OB", bs_, rt) for bs_ in range(nstep) for rt in range(4)]
                P.op("sp", None, r=OBR, w=["OBjoin"])
                P.barrier()
                P.emit()
                st_scope.__exit__(None, None, None)
                st_scope = ExitStack()
                st_scope.__enter__()
                NG = 3
                gr = [[sb("gr%d_%d" % (j_, i), [128, D], BF16, st_scope) for i in range(4)] for j_ in range(NG)]
                acc = [sb("acc%d" % i, [128, D], F32, st_scope) for i in range(2)]
                x1r = [sb("x1r%d" % i, [128, D], F32, st_scope) for i in range(3)]
                G2 = sb("G2", [128, nseq, D], F32, st_scope)
                for b_ in range(nseq):
                    dma("sp", G2[:, b_, :], MOD_d[b_, 5], w=[("G2", b_, 0), ("G2", b_, 1)])
                for t in range(NTL):
                    b = t // 16
                    sl = t % 3
                    gs = t % NG
                    ac = acc[t % 2]
                    acr = ("acc", t % 2)
                    dma("sp", x1r[sl][:], X1_tiles[t], w=[("x1r", sl)])
                    for k in range(4):
                        def _gath(g, t=t, k=k, gs=gs):
                            if t == 0 and k == 0:
                                g.reg_mov(bcreg, NSLOT - 1)
                            return g.indirect_dma_start(
                                out=gr[gs][k][:], out_offset=None, in_=OB_d[:, :],
                                in_offset=bass.IndirectOffsetOnAxis(ap=sloti[:, t * 4 + k:t * 4 + k + 1], axis=0),
                                bounds_check=bcreg, oob_is_err=False)
                        P.op("pool", _gath, r=["OBjoin", "sloti"], w=[("gr", gs, k)], dma=True)
                    act(lambda a, t=t, gs=gs, ac=ac: a.activation(out=ac[:], in_=gr[gs][0][:], func=AF.Copy, scale=g4[:, t, 0:1]),
                        r=[("gr", gs, 0)] + G4R, w=[acr])
                    for k in range(1, 4):
                        dve(lambda v, t=t, k=k, gs=gs, ac=ac: v.scalar_tensor_tensor(out=ac[:], in0=gr[gs][k][:], scalar=g4[:, t, k:k + 1], in1=ac[:],
                                                                                    op0=ALU.mult, op1=ALU.add), r=[("gr", gs, k), acr] + G4R, w=[acr])
                    dve(lambda g, b=b, ac=ac: g.tensor_tensor(out=ac[:], in0=ac[:], in1=G2[:, b, :], op=ALU.mult),
                        r=[acr, ("G2", b, 0), ("G2", b, 1)], w=[acr])
                    dve(lambda g, sl=sl, ac=ac: g.tensor_tensor(out=x1r[sl][:], in0=ac[:], in1=x1r[sl][:], op=ALU.add),
                        r=[acr, ("x1r", sl)], w=[("x1r", sl), acr])
                    dma("sp", out_tiles[t], x1r[sl][:], r=[("x1r", sl)], w=[("out", t)])
            else:
                x1r = [sb("x1r%d" % i, [128, D], F32, st_scope) for i in range(2)]
                X1_tiles = X1_d.ap().rearrange("(t p) d -> t p d", p=128)
                out_tiles = out_d.ap().rearrange("(t p) d -> t p d", p=128)
                for t in range(NTL):
                    sl = t % 2
                    dma("sp", x1r[sl][:], X1_tiles[t], w=[("x1r", sl)])
                    dma("sp", out_tiles[t], x1r[sl][:], r=[("x1r", sl)], w=[("out", t)])
            P.barrier()
            P.emit()
            st_scope.__exit__(None, None, None)
    return nc


def make_in_maps(inputs, ncores=NCORES, nseq=NSEQ):
    f = lambda a: np.ascontiguousarray(np.asarray(a, dtype=np.float32))
    x = f(inputs["x"])
    c = f(inputs["c"])
    shared = {
        "w_ada": f(inputs["w_ada"][0]),
        "b_ada": f(inputs["b_ada"][0]).reshape(1, -1),
        "norm1_g": f(inputs["norm1_g"][0]).reshape(1, -1),
        "norm2_g": f(inputs["norm2_g"][0]).reshape(1, -1),
        "w_in": f(inputs["w_in"][0]),
        "gq": f(np.tile(np.asarray(inputs["q_norm_g"][0]), 2).reshape(128, 1)),
        "gk": f(np.tile(np.asarray(inputs["k_norm_g"][0]), 2).reshape(128, 1)),
        "lamv": f(np.stack([inputs["lambda_q1"][0], inputs["lambda_q2"][0], inputs["lambda_k1"][0], inputs["lambda_k2"][0]])),
        "subln_g": f(inputs["subln_g"][0]).reshape(1, -1),
        "conv_wT": f(np.asarray(inputs["conv_w"][0]).reshape(31, 4, 128).transpose(2, 1, 0)),
        "conv_b": f(np.asarray(inputs["conv_b"][0]).reshape(4, 128).T),
        "conv_ln_g": f(np.asarray(inputs["conv_ln_g"][0]).reshape(4, 128).T),
        "conv_ln_b": f(np.asarray(inputs["conv_ln_b"][0]).reshape(4, 128).T),
        "w_out": f(inputs["w_out"][0]),
        "w_router": f(inputs["w_router"][0]),
        "b_router": f(inputs["b_router"][0]).reshape(1, -1),
        "w_gate_up": f(inputs["w_gate_up"][0]),
        "b_gate_up": f(inputs["b_gate_up"][0]),
        "w_down": f(inputs["w_down"][0]),
        "b_down": f(inputs["b_down"][0]),
    }
    maps = []
    for i in range(ncores):
        m = dict(shared)
        m["x"] = np.ascontiguousarray(x[i * nseq:(i + 1) * nseq].reshape(nseq * S, D))
        cc = c[i * nseq:(i + 1) * nseq]
        m["cT"] = np.ascontiguousarray(cc.reshape(nseq, 8, 128).transpose(2, 1, 0))
        maps.append(m)
    return maps


_NC_CACHE = {}


def kernel(**inputs):
    if "nc" not in _NC_CACHE:
        _NC_CACHE["nc"] = build_program()
    nc = _NC_CACHE["nc"]
    maps = make_in_maps(inputs)
    res = run_bass_kernel_spmd(nc, maps, core_ids=list(range(NCORES)))
    outs = [np.asarray(r["out"]).reshape(NSEQ, S, D) for r in res.results]
    return np.concatenate(outs, axis=0).astype(np.float32)
```
